# Optimizing a Trainium2 kernel written in Bass

```python
import math
import jax
import jax.numpy as jnp
from jax import lax
import numpy as np

D_MODEL = 1024
BATCH = 4
SEQ = 4096
DEPTH = 4

N_HEADS = 16
HEAD_DIM = 64
A_HEADS = 8
B_HEADS = 8
MOBA_BLOCK = 256
MOBA_TOPK = 3
MOBA_Q_CHUNK = 64
IDX_HEADS = 16
IDX_DIM = 64
DSA_TOPK_MAX = 256
DSA_Q_CHUNK = 128
DILATED_PATTERNS = ((128, 1), (512, 4), (2048, 16))
REL_BUCKETS = 32
REL_MAX_DIST = 128
D_FF = 2816
CONV_WIDTH = 3
RMS_EPS = 1e-6

kernel_name = 'hybrid_moba_dsa_dilated_convffn'


def rmsnorm(x, g):
    x32 = x.astype(jnp.float32)
    y = x32 * lax.rsqrt(jnp.mean(x32 * x32, axis=-1, keepdims=True) + RMS_EPS)
    return (y * g.astype(jnp.float32)).astype(x.dtype)


def t5_bucket(dist):
    n = jnp.maximum(dist, 0)
    max_exact = REL_BUCKETS // 2
    nf = jnp.maximum(n, max_exact).astype(jnp.float32)
    large = max_exact + (jnp.log(nf / max_exact) / math.log(REL_MAX_DIST / max_exact)
                         * (REL_BUCKETS - max_exact)).astype(jnp.int32)
    large = jnp.minimum(large, REL_BUCKETS - 1)
    return jnp.where(n < max_exact, n, large)


def split_cols(a, sizes):
    out, off = [], 0
    for s in sizes:
        out.append(a[..., off:off + s])
        off += s
    return out


def moba_attention(q, k, v, table_hk):
    B, H, S, Dh = q.shape
    nb = -(-S // MOBA_BLOCK)
    pad = nb * MOBA_BLOCK - S
    kb = jnp.pad(k, ((0, 0), (0, 0), (0, pad), (0, 0))).reshape(B, H, nb, MOBA_BLOCK, Dh)
    vb = jnp.pad(v, ((0, 0), (0, 0), (0, pad), (0, 0))).reshape(B, H, nb, MOBA_BLOCK, Dh)
    kmean = jnp.mean(kb.astype(jnp.float32), axis=3).astype(k.dtype)
    n_sel = min(MOBA_TOPK, nb - 1)
    scale = Dh ** -0.5
    nq = S // MOBA_Q_CHUNK
    qc = jnp.moveaxis(q.reshape(B, H, nq, MOBA_Q_CHUNK, Dh), 2, 0)
    bi = jnp.arange(B)[:, None, None, None]
    hi = jnp.arange(H)[None, :, None, None]
    offs = jnp.arange(MOBA_BLOCK)
    blk_ids = jnp.arange(nb)

    def chunk(args):
        qi, c = args
        q0 = c * MOBA_Q_CHUNK
        t = q0 + jnp.arange(MOBA_Q_CHUNK)
        own = q0 // MOBA_BLOCK
        k_own = lax.dynamic_index_in_dim(kb, own, axis=2, keepdims=False)
        v_own = lax.dynamic_index_in_dim(vb, own, axis=2, keepdims=False)
        d_own = t[:, None] - (own * MOBA_BLOCK + offs)[None, :]
        l_own = jnp.einsum('bhqd,bhkd->bhqk', qi, k_own).astype(jnp.float32) * scale
        l_own = jnp.where(d_own >= 0, l_own + table_hk[:, t5_bucket(d_own)], -jnp.inf)
        if n_sel == 0:
            p = jax.nn.softmax(l_own, axis=-1).astype(v.dtype)
            return jnp.einsum('bhqk,bhkd->bhqd', p, v_own)
        gate = jnp.einsum('bhqd,bhnd->bhqn', qi, kmean).astype(jnp.float32)
        gate = jnp.where(blk_ids < own, gate, -jnp.inf)
        _, sel = lax.top_k(gate, n_sel)
        k_sel = kb[bi, hi, sel]
        v_sel = vb[bi, hi, sel]
        d_sel = t[None, None, :, None, None] - (sel[..., None] * MOBA_BLOCK + offs)
        l_sel = jnp.einsum('bhqd,bhqnkd->bhqnk', qi, k_sel).astype(jnp.float32) * scale
        l_sel = l_sel + table_hk[hi[..., None], t5_bucket(d_sel)]
        l_sel = jnp.where((sel < own)[..., None], l_sel, -jnp.inf)
        n_k = n_sel * MOBA_BLOCK
        logits = jnp.concatenate([l_sel.reshape(B, H, MOBA_Q_CHUNK, n_k), l_own], axis=-1)
        p = jax.nn.softmax(logits, axis=-1).astype(v.dtype)
        p_sel = p[..., :n_k].reshape(B, H, MOBA_Q_CHUNK, n_sel, MOBA_BLOCK)
        p_own = p[..., n_k:]
        return (jnp.einsum('bhqnk,bhqnkd->bhqd', p_sel, v_sel)
                + jnp.einsum('bhqk,bhkd->bhqd', p_own, v_own))

    out = lax.map(chunk, (qc, jnp.arange(nq, dtype=jnp.int32)))
    return jnp.moveaxis(out, 0, 2).reshape(B, H, S, Dh)


def dsa_attention(q, k, v, q_idx, k_idx, w_idx, table_hk):
    B, S, H, Dh = q.shape
    n_keep = min(DSA_TOPK_MAX, S // 4)
    nq = S // DSA_Q_CHUNK
    scale = Dh ** -0.5
    key_pos = jnp.arange(S)
    bi = jnp.arange(B)[:, None, None]

    def chunkify(a):
        return jnp.moveaxis(a.reshape((B, nq, DSA_Q_CHUNK) + a.shape[2:]), 1, 0)

    def chunk(args):
        qi, qii, wi, c = args
        t = c * DSA_Q_CHUNK + jnp.arange(DSA_Q_CHUNK)
        rel = jax.nn.relu(jnp.einsum('bqhd,bsd->bqhs', qii, k_idx) * (IDX_DIM ** -0.5))
        score = jnp.einsum('bqhs,bqh->bqs', rel, wi).astype(jnp.float32)
        score = jnp.where(key_pos[None, :] <= t[:, None], score, -jnp.inf)
        _, sel = lax.top_k(score, n_keep)
        k_sel = k[bi, sel]
        v_sel = v[bi, sel]
        d = t[None, :, None] - sel
        logits = jnp.einsum('bqhd,bqkhd->bhqk', qi, k_sel).astype(jnp.float32) * scale
        logits = logits + jnp.moveaxis(table_hk[:, t5_bucket(d)], 0, 1)
        logits = jnp.where((d >= 0)[:, None], logits, -jnp.inf)
        p = jax.nn.softmax(logits, axis=-1).astype(v.dtype)
        return jnp.einsum('bhqk,bqkhd->bqhd', p, v_sel)

    out = lax.map(chunk, (chunkify(q), chunkify(q_idx), chunkify(w_idx),
                          jnp.arange(nq, dtype=jnp.int32)))
    return jnp.moveaxis(out, 0, 1).reshape(B, S, H, Dh)


def dilated_branch(q, k, v, table_hk, window, dilation):
    B, H, S, Dh = q.shape
    w_sub = window // dilation
    bb = w_sub
    n = S // dilation
    nbk = -(-n // bb)
    npad = nbk * bb
    scale = Dh ** -0.5

    def to_sub(a):
        a = jnp.swapaxes(a.reshape(B, H, n, dilation, Dh), 2, 3)
        return jnp.pad(a, ((0, 0), (0, 0), (0, 0), (0, npad - n), (0, 0)))

    def band(a):
        ap = jnp.pad(a, ((0, 0), (0, 0), (0, 0), (bb, 0), (0, 0)))
        prev = ap[:, :, :, :npad].reshape(B, H, dilation, nbk, bb, Dh)
        cur = a.reshape(B, H, dilation, nbk, bb, Dh)
        return jnp.concatenate([prev, cur], axis=4)

    qs = to_sub(q).reshape(B, H, dilation, nbk, bb, Dh)
    kband = band(to_sub(k))
    vband = band(to_sub(v))
    logits = jnp.einsum('bhrnqd,bhrnkd->bhrnqk', qs, kband).astype(jnp.float32) * scale
    a_i = jnp.arange(bb)
    m_i = jnp.arange(2 * bb)
    di = a_i[:, None] + bb - m_i[None, :]
    band_ok = (di >= 0) & (di <= w_sub)
    j_idx = jnp.arange(nbk)[:, None] * bb - bb + m_i[None, :]
    mask = band_ok[None] & (j_idx >= 0)[:, None, :]
    bias = table_hk[:, t5_bucket(di * dilation)][:, None, None]
    logits = jnp.where(mask, logits + bias, -jnp.inf)
    mx = jnp.max(logits, axis=-1, keepdims=True)
    e = jnp.exp(logits - mx)
    den = jnp.sum(e, axis=-1, keepdims=True)
    o = jnp.einsum('bhrnqk,bhrnkd->bhrnqd', (e / den).astype(v.dtype), vband)
    lse = (mx + jnp.log(den))[..., 0]
    o = jnp.swapaxes(o.reshape(B, H, dilation, npad, Dh)[:, :, :, :n], 2, 3).reshape(B, H, S, Dh)
    lse = jnp.swapaxes(lse.reshape(B, H, dilation, npad)[:, :, :, :n], 2, 3).reshape(B, H, S)
    return o, lse


def dilated_mixture(q, k, v, table_hk):
    outs, lses = [], []
    for window, dilation in DILATED_PATTERNS:
        o, l = dilated_branch(q, k, v, table_hk, window, dilation)
        outs.append(o)
        lses.append(l)
    alpha = jax.nn.softmax(jnp.stack(lses, axis=0), axis=0)
    return jnp.einsum('pbhs,pbhsd->bhsd', alpha.astype(v.dtype), jnp.stack(outs, axis=0))


def conv_ffn(h, w_up, conv_w, conv_b, w_down):
    S = h.shape[1]
    u = h @ w_up
    up = jnp.pad(u, ((0, 0), (CONV_WIDTH - 1, 0), (0, 0)))
    c = conv_b
    for j in range(CONV_WIDTH):
        c = c + conv_w[j] * up[:, j:j + S]
    val, gate = jnp.split(c, 2, axis=-1)
    return (jax.nn.silu(gate) * val) @ w_down


def setup_inputs(seed: int = 0) -> dict:
    key = jax.random.key(seed)
    ks = jax.random.split(key, 13)
    n_even = (DEPTH + 1) // 2
    n_odd = DEPTH // 2
    even_in = 3 * A_HEADS * HEAD_DIM + 3 * B_HEADS * HEAD_DIM + IDX_HEADS * IDX_DIM + IDX_DIM + IDX_HEADS
    odd_in = 3 * N_HEADS * HEAD_DIM
    mix = N_HEADS * HEAD_DIM

    def nrm(k, shape, s):
        return jax.random.normal(k, shape, jnp.float32) * s

    return {
        'x': nrm(ks[0], (BATCH, SEQ, D_MODEL), 1.0),
        'w_in_even': nrm(ks[1], (n_even, D_MODEL, even_in), D_MODEL ** -0.5),
        'idx_k_norm': 1.0 + nrm(ks[2], (n_even, IDX_DIM), 0.02),
        'w_in_odd': nrm(ks[3], (n_odd, D_MODEL, odd_in), D_MODEL ** -0.5),
        'w_o': nrm(ks[4], (DEPTH, mix, D_MODEL), mix ** -0.5),
        'rel_bias': nrm(ks[5], (REL_BUCKETS, N_HEADS), 0.2),
        'attn_norm': 1.0 + nrm(ks[6], (DEPTH, D_MODEL), 0.02),
        'ffn_norm': 1.0 + nrm(ks[7], (DEPTH, D_MODEL), 0.02),
        'w_up': nrm(ks[8], (DEPTH, D_MODEL, 2 * D_FF), D_MODEL ** -0.5),
        'conv_w': nrm(ks[9], (DEPTH, CONV_WIDTH, 2 * D_FF), CONV_WIDTH ** -0.5),
        'conv_b': nrm(ks[10], (DEPTH, 2 * D_FF), 0.02),
        'w_down': nrm(ks[11], (DEPTH, D_FF, D_MODEL), D_FF ** -0.5),
        'final_norm': 1.0 + nrm(ks[12], (D_MODEL,), 0.02),
    }


def reference(x, w_in_even, idx_k_norm, w_in_odd, w_o, rel_bias, attn_norm, ffn_norm,
              w_up, conv_w, conv_b, w_down, final_norm):
    B, S, _ = x.shape
    mix = N_HEADS * HEAD_DIM
    sizes_even = [A_HEADS * HEAD_DIM] * 3 + [B_HEADS * HEAD_DIM] * 3 + [IDX_HEADS * IDX_DIM, IDX_DIM, IDX_HEADS]
    sizes_odd = [N_HEADS * HEAD_DIM] * 3
    table_a = rel_bias[:, :A_HEADS].T
    table_b = rel_bias[:, A_HEADS:A_HEADS + B_HEADS].T
    table_c = rel_bias.T
    for layer in range(DEPTH):
        h = rmsnorm(x, attn_norm[layer])
        if layer % 2 == 0:
            j = layer // 2
            qa, ka, va, qb, kb, vb, qi, ki, wi = split_cols(h @ w_in_even[j], sizes_even)
            qa, ka, va = (jnp.swapaxes(a.reshape(B, S, A_HEADS, HEAD_DIM), 1, 2) for a in (qa, ka, va))
            o_a = jnp.swapaxes(moba_attention(qa, ka, va, table_a), 1, 2)
            qb, kb, vb = (a.reshape(B, S, B_HEADS, HEAD_DIM) for a in (qb, kb, vb))
            o_b = dsa_attention(qb, kb, vb,
                                qi.reshape(B, S, IDX_HEADS, IDX_DIM),
                                rmsnorm(ki, idx_k_norm[j]),
                                wi * (IDX_HEADS ** -0.5),
                                table_b)
            mixed = jnp.concatenate([o_a, o_b], axis=2).reshape(B, S, mix)
        else:
            j = layer // 2
            qc, kc, vc = (jnp.swapaxes(a.reshape(B, S, N_HEADS, HEAD_DIM), 1, 2)
                          for a in split_cols(h @ w_in_odd[j], sizes_odd))
            o_c = dilated_mixture(qc, kc, vc, table_c)
            mixed = jnp.swapaxes(o_c, 1, 2).reshape(B, S, mix)
        x = x + mixed @ w_o[layer]
        h = rmsnorm(x, ffn_norm[layer])
        x = x + conv_ffn(h, w_up[layer], conv_w[layer], conv_b[layer], w_down[layer])
    return rmsnorm(x, final_norm)
```

```python
import math
from contextlib import ExitStack

import numpy as np
import ml_dtypes
import concourse.bass as bass
import concourse.mybir as mybir
from concourse.bass_utils import run_bass_kernel_spmd

F32 = mybir.dt.float32
BF16 = mybir.dt.bfloat16
AF = mybir.ActivationFunctionType
ALU = mybir.AluOpType
AX = mybir.AxisListType

D = 1024
KC = 8
DFF = 2816
NF = 22
NEG = -30000.0
SAME_ENGINE_SYNC = True


class Buf:
    __slots__ = ("name", "w", "r")

    def __init__(self, name=""):
        self.name = name
        self.w = {}
        self.r = {}


class Eng:
    def __init__(self, K, name, e, is_pe=False):
        self.K = K
        self.name = name
        self.e = e
        self.is_pe = is_pe
        self.sem = K.new_sem("pg_" + name)
        self.n = 0
        self.waited = {}

    def wait(self, sem, val):
        key = id(sem)
        if self.waited.get(key, 0) >= val:
            return
        self.waited[key] = val
        self.e.wait_ge(sem, val)


class K:
    def __init__(self, nc):
        self.nc = nc
        self.stack = ExitStack()
        self.sems = []
        self.pe = Eng(self, "pe", nc.tensor, is_pe=True)
        self.act = Eng(self, "act", nc.scalar)
        self.dve = Eng(self, "dve", nc.vector)
        self.pool = Eng(self, "pool", nc.gpsimd)
        self.sp = Eng(self, "sp", nc.sync)
        self.engs = [self.pe, self.act, self.dve, self.pool, self.sp]
        self.dq = {}
        for q in (self.sp, self.pool, self.act):
            self.dq[q.name] = dict(sems=[self.new_sem("dq_%s%d" % (q.name, i)) for i in range(8)],
                                   tot=[0] * 8, i=0)

    def new_sem(self, name):
        s = self.stack.enter_context(self.nc.semaphore(name))
        self.sems.append(s)
        return s

    def _deps(self, X, reads, writes, acc=False):
        for b in reads:
            for sem, val in b.w.values():
                if (sem is X.sem) and (X.is_pe or not SAME_ENGINE_SYNC):
                    continue
                X.wait(sem, val)
        for b in writes:
            for sem, val in list(b.w.values()) + list(b.r.values()):
                if (sem is X.sem) and (X.is_pe or not SAME_ENGINE_SYNC):
                    continue
                X.wait(sem, val)

    def _mark(self, ev, reads, writes):
        sem, val = ev
        for b in reads:
            b.r[id(sem)] = ev
        for b in writes:
            b.w = {id(sem): ev}
            b.r = {}

    def op(self, X, ins_fn, reads=(), writes=()):
        self._deps(X, reads, writes)
        ins = ins_fn()
        ins.then_inc(X.sem, 1)
        X.n += 1
        self._mark((X.sem, X.n), reads, writes)
        return ins

    def dma(self, Q, out, in_, reads=(), writes=(), **kw):
        self._deps(Q, reads, writes)
        dq = self.dq[Q.name]
        i = dq["i"]
        dq["i"] = (i + 1) % len(dq["sems"])
        sem = dq["sems"][i]
        if dq["tot"][i] > 0:
            Q.wait(sem, dq["tot"][i])
        Q.e.dma_start(out=out, in_=in_, **kw).then_inc(sem, 16)
        dq["tot"][i] += 16
        self._mark((sem, dq["tot"][i]), reads, writes)

    def barrier(self):
        for X in self.engs:
            for Y in self.engs:
                if Y is not X and Y.n > 0:
                    X.wait(Y.sem, Y.n)
            for dq in self.dq.values():
                for sem, tot in zip(dq["sems"], dq["tot"]):
                    if tot > 0:
                        X.wait(sem, tot)

    def final_wait(self):
        X = self.sp
        for Y in self.engs:
            if Y is not X and Y.n > 0:
                X.wait(Y.sem, Y.n)
        for dq in self.dq.values():
            for sem, tot in zip(dq["sems"], dq["tot"]):
                if tot > 0:
                    X.wait(sem, tot)


class Phase:
    _uid = [0]

    def __init__(self, k, name):
        self.k = k
        Phase._uid[0] += 1
        self.name = "%s%d" % (name, Phase._uid[0])
        self.stack = ExitStack()
        self.cnt = 0

    def __enter__(self):
        return self

    def __exit__(self, *a):
        self.k.barrier()
        self.stack.close()
        return False

    def sb(self, shape, dt, name=None):
        self.cnt += 1
        t = self.stack.enter_context(
            self.k.nc.sbuf_tensor("%s_%s%d" % (self.name, name or "t", self.cnt), list(shape), dt))
        return t

    def ps(self, shape, dt, name=None):
        self.cnt += 1
        t = self.stack.enter_context(
            self.k.nc.psum_tensor("%s_%s%d" % (self.name, name or "p", self.cnt), list(shape), dt))
        return t


class Ring:
    def __init__(self, ph, n, shape, dt, name, psum=False):
        self.t = [(ph.ps if psum else ph.sb)(shape, dt, name) for _ in range(n)]
        self.b = [Buf("%s%d" % (name, i)) for i in range(n)]
        self.i = -1
        self.n = n

    def next(self):
        self.i = (self.i + 1) % self.n
        return self.t[self.i], self.b[self.i]


def t5_bucket_np(dist):
    n = np.maximum(dist, 0)
    max_exact = 16
    nf = np.maximum(n, max_exact).astype(np.float32)
    large = max_exact + (np.log(nf / max_exact) / math.log(128 / max_exact) * (32 - max_exact)).astype(np.int32)
    large = np.minimum(large, 31)
    return np.where(n < max_exact, n, large)


class Cfg:
    def __init__(self, S=4096, depth=4, debug=None, stop_after=None):
        self.S = S
        self.T = S // 128
        self.TB = S // 512
        self.depth = depth
        self.debug = debug
        self.stop_after = stop_after


NIT = 18


class NS:
    pass


def norm_rings(ph):
    return dict(
        junk=Ring(ph, 1, [128, D], BF16, "junk"),
        ss=Ring(ph, 4, [128, 4], F32, "ss"),
        hb=Ring(ph, 2, [128, D], BF16, "hb"),
        pT=Ring(ph, 2, [128, D], BF16, "pT", psum=True),
    )


def rstd_chain(k, xt, xb, ss, ssb, junk, junkb, n):
    nc = k.nc
    k.op(k.act, lambda: nc.scalar.activation(out=junk, in_=xt, func=AF.Square, accum_out=ss[:, 0:1]),
         reads=[xb], writes=[junkb, ssb])
    k.op(k.dve, lambda: nc.vector.tensor_scalar(out=ss[:, 1:2], in0=ss[:, 0:1], scalar1=1.0 / n, scalar2=1e-6,
                                                op0=ALU.mult, op1=ALU.add), reads=[ssb], writes=[ssb])
    k.op(k.act, lambda: nc.scalar.activation(out=ss[:, 2:3], in_=ss[:, 1:2], func=AF.Sqrt),
         reads=[ssb], writes=[ssb])
    k.op(k.dve, lambda: nc.vector.reciprocal(out=ss[:, 3:4], in_=ss[:, 2:3]), reads=[ssb], writes=[ssb])


def norm_tile(k, C, xt, xb, g_bc, gb, hT, hTb_tile, t, rings):
    nc = k.nc
    junk, junkb = rings["junk"].next()
    ss, ssb = rings["ss"].next()
    rstd_chain(k, xt[:], xb, ss, ssb, junk[:], junkb, D)
    hb, hbb = rings["hb"].next()
    k.op(k.dve, lambda: nc.vector.scalar_tensor_tensor(out=hb[:], in0=xt[:], scalar=ss[:, 3:4], in1=g_bc[:],
                                                       op0=ALU.mult, op1=ALU.mult),
         reads=[xb, ssb, gb], writes=[hbb])
    pT, pTb = rings["pT"].next()
    for kc in range(KC):
        k.op(k.pe, lambda kc=kc: nc.tensor.transpose(out=pT[:, kc * 128:(kc + 1) * 128],
                                                     in_=hb[:, kc * 128:(kc + 1) * 128], identity=C.ident[:]),
             reads=[hbb], writes=[pTb])
    k.op(k.act, lambda: nc.scalar.copy(out=hT[:, :, t * 128:(t + 1) * 128],
                                       in_=pT[:].rearrange("p (c n) -> p c n", c=KC)),
         reads=[pTb], writes=[hTb_tile])


def phase_A1(k, cfg, C, hT, hTb, layer):
    nc = k.nc
    with Phase(k, "A1") as ph:
        g_bc = ph.sb([128, D], F32, "g")
        gb = Buf("g")
        k.dma(k.sp, g_bc[:], C.g_attn[layer], writes=[gb])
        rings = norm_rings(ph)
        xr = Ring(ph, 3, [128, D], F32, "x")
        for t in range(cfg.T):
            xt, xb = xr.next()
            k.dma(k.sp, xt[:], C.x_in[t * 128:(t + 1) * 128, :], writes=[xb])
            norm_tile(k, C, xt, xb, g_bc, gb, hT, hTb[t], t, rings)


def phase_A2(k, cfg, C, hT, hTb, layer):
    nc = k.nc
    S, T, TB = cfg.S, cfg.T, cfg.TB
    with Phase(k, "A2") as ph:
        W = C.w_in_even[layer // 2] if layer % 2 == 0 else C.w_in_odd[layer // 2]
        Wv = W.rearrange("(c p) n -> p c n", p=128)
        if layer % 2 == 0:
            blocks = [(0, 512, "fm", ("Q", 0, 0.125)), (512, 512, "fm", ("K", 0, 1.0)),
                      (1024, 512, "tm", ("V", 0)),
                      (1536, 512, "fm", ("Q", 4, 0.125)), (2048, 512, "fm", ("K", 4, 1.0)),
                      (2560, 512, "tm", ("V", 4)),
                      (3072, 512, "fm", ("QI", 0, 1.0)), (3584, 512, "fm", ("QI", 4, 1.0)),
                      (4096, 80, "tm", ("KW", 0))]
        else:
            blocks = [(0, 512, "fm", ("Q", 0, 0.125)), (512, 512, "fm", ("Q", 4, 0.125)),
                      (1024, 512, "fm", ("K", 0, 1.0)), (1536, 512, "fm", ("K", 4, 1.0)),
                      (2048, 512, "tm", ("V", 0)), (2560, 512, "tm", ("V", 4))]
        wst = Ring(ph, 2, [128, KC, 512], F32, "wst")
        wbf = Ring(ph, 2, [128, KC, 512], BF16, "wbf")
        pacc = Ring(ph, 4, [128, 512], F32, "pacc", psum=True)
        ostg = Ring(ph, 2, [128, S], BF16, "ostg")
        vstg = Ring(ph, 3, [128, 512], BF16, "vstg")
        kwstg = Ring(ph, 3, [128, 80], F32, "kwstg")
        flip = 0
        for (c0, ncol, kind, dest) in blocks:
            ws, wsb = wst.next()
            k.dma(k.sp, ws[:, :, 0:ncol], Wv[:, :, c0:c0 + ncol], writes=[wsb])
            wb, wbb = wbf.next()
            k.op(k.pool, lambda: nc.gpsimd.tensor_copy(out=wb[:, :, 0:ncol], in_=ws[:, :, 0:ncol]),
                 reads=[wsb], writes=[wbb])
            if kind == "fm":
                name, cbase, scale = dest
                dst = {"Q": C.QT_d, "K": C.KT_d, "QI": C.QI_d}[name]
                for ci in range(ncol // 128):
                    og, ogb = ostg.next()
                    for tb in range(TB):
                        pa, pab = pacc.next()
                        for kc in range(KC):
                            k.op(k.pe, lambda kc=kc: nc.tensor.matmul(
                                pa[:], lhsT=wb[:, kc, ci * 128:(ci + 1) * 128],
                                rhs=hT[:, kc, tb * 512:(tb + 1) * 512],
                                start=(kc == 0), stop=(kc == KC - 1)),
                                reads=[wbb] + hTb[tb * 4:(tb + 1) * 4], writes=[pab])
                        flip ^= 1
                        if flip:
                            k.op(k.act, lambda: nc.scalar.mul(out=og[:, tb * 512:(tb + 1) * 512], in_=pa[:],
                                                              mul=scale), reads=[pab], writes=[ogb])
                        else:
                            k.op(k.dve, lambda: nc.vector.tensor_scalar(
                                out=og[:, tb * 512:(tb + 1) * 512], in0=pa[:], scalar1=scale, scalar2=None,
                                op0=ALU.mult), reads=[pab], writes=[ogb])
                    k.dma(k.pool, dst[cbase + ci], og[:], reads=[ogb])
            else:
                name, cbase = dest
                for t in range(T):
                    pa, pab = pacc.next()
                    for kc in range(KC):
                        k.op(k.pe, lambda kc=kc: nc.tensor.matmul(
                            pa[:, 0:ncol], lhsT=hT[:, kc, t * 128:(t + 1) * 128], rhs=wb[:, kc, 0:ncol],
                            start=(kc == 0), stop=(kc == KC - 1)),
                            reads=[wbb, hTb[t]], writes=[pab])
                    flip ^= 1
                    if name == "V":
                        vs, vsb = vstg.next()
                        if flip:
                            k.op(k.act, lambda: nc.scalar.copy(out=vs[:], in_=pa[:]), reads=[pab], writes=[vsb])
                        else:
                            k.op(k.dve, lambda: nc.vector.tensor_copy(out=vs[:], in_=pa[:]),
                                 reads=[pab], writes=[vsb])
                        k.dma(k.pool, C.V_d[cbase:cbase + 4, :, t, :].rearrange("c p n -> p c n"),
                              vs[:].rearrange("p (c n) -> p c n", c=4), reads=[vsb])
                    else:
                        vs, vsb = kwstg.next()
                        k.op(k.dve, lambda: nc.vector.tensor_copy(out=vs[:], in_=pa[:, 0:80]),
                             reads=[pab], writes=[vsb])
                        k.dma(k.pool, C.KW_d[:, t, :], vs[:], reads=[vsb])


def phase_tables(k, cfg, C):
    nc = k.nc
    with Phase(k, "T") as ph:
        negc = ph.sb([128, 16], F32, "negc")
        negb = Buf()
        k.op(k.dve, lambda: nc.vector.tensor_scalar(out=negc[:], in0=C.c31[:], scalar1=-1.0, scalar2=None,
                                                    op0=ALU.mult), reads=[C.c31b], writes=[negb])
        caus = ph.sb([128, 1024], F32, "caus")
        cdil = ph.sb([128, 1024], F32, "cdil")
        cb_, db_ = Buf(), Buf()
        k.dma(k.sp, caus[:], C.caus_in[:, :], writes=[cb_])
        k.dma(k.sp, cdil[:], C.cdil_in[:, :], writes=[db_])
        gr = Ring(ph, 2, [128, 1024], F32, "g")
        er = Ring(ph, 2, [128, 1024], F32, "e")
        mnr = Ring(ph, 2, [128, 1024], BF16, "mn")
        mdr = Ring(ph, 2, [128, 1024], BF16, "md")
        for h in range(16):
            g, gb = gr.next()
            k.dma(k.sp, g[:], C.G_in[h], writes=[gb])
            e, eb = er.next()
            k.op(k.act, lambda: nc.scalar.activation(out=e[:], in_=g[:], func=AF.Exp, bias=negc[:, h:h + 1],
                                                     scale=1.0), reads=[gb, negb], writes=[eb])
            mn, mnb = mnr.next()
            k.op(k.dve, lambda: nc.vector.tensor_tensor(out=mn[:], in0=e[:], in1=caus[:], op=ALU.mult),
                 reads=[eb, cb_], writes=[mnb])
            md, mdb = mdr.next()
            k.op(k.dve, lambda: nc.vector.tensor_tensor(out=md[:], in0=e[:], in1=cdil[:], op=ALU.mult),
                 reads=[eb, db_], writes=[mdb])
            k.dma(k.pool, C.Mnear_d[h], mn[:], reads=[mnb])
            k.dma(k.pool, C.Mdil_d[h], md[:], reads=[mdb])


def moba_prepass(k, cfg, C):
    nc = k.nc
    S, T = cfg.S, cfg.T
    NB = S // 256
    with Phase(k, "G") as ph:
        pm = ph.sb([128, 16, 16], F32, "pm")
        o3 = ph.sb([128, 16, 16], F32, "o3")
        pmb, o3b = Buf(), Buf()
        k.dma(k.sp, pm[:], C.pastmask_in[:, :, :], writes=[pmb])
        k.dma(k.sp, o3[:], C.own30k_in[:, :, :], writes=[o3b])
        qr = Ring(ph, 2, [128, S], BF16, "q")
        kr = Ring(ph, 2, [128, S], BF16, "k")
        ksr = Ring(ph, 2, [128, 16], F32, "ks")
        kmr = Ring(ph, 2, [128, 16], BF16, "km")
        qmr = Ring(ph, 2, [128, S], BF16, "qm")
        tpr = Ring(ph, 2, [128, 512], F32, "tp", psum=True)
        gpr = [Ring(ph, 2, [128, 512], F32, "gpa", psum=True), Ring(ph, 2, [128, 512], F32, "gpb", psum=True)]
        gmr = Ring(ph, 3, [128, 2, 16], F32, "gm")
        t8r = Ring(ph, 3, [128, 2, 8], F32, "t8")
        thr = Ring(ph, 3, [128, 2], F32, "th")
        t1r = Ring(ph, 3, [128, 2, 16], F32, "t1")
        mvr = Ring(ph, 3, [128, 128], BF16, "mv")
        for i in range(3):
            k.op(k.dve, lambda i=i: nc.vector.memset(mvr.t[i][:], 0.0), writes=[mvr.b[i]])
        for c in range(4):
            qt, qb = qr.next()
            kt, kb = kr.next()
            k.dma(k.sp, qt[:], C.QT_d[c], writes=[qb])
            k.dma(k.sp, kt[:], C.KT_d[c], writes=[kb])
            ks, ksb = ksr.next()
            km, kmb = kmr.next()
            k.op(k.dve, lambda: nc.vector.memset(ks[:], 0.0), writes=[ksb])
            k.op(k.dve, lambda: nc.vector.tensor_reduce(out=ks[:, 0:NB], in_=kt[:].rearrange("p (n b) -> p n b", b=256),
                                                        axis=AX.X, op=ALU.add), reads=[kb], writes=[ksb])
            k.op(k.dve, lambda: nc.vector.tensor_scalar(out=km[:], in0=ks[:], scalar1=1.0 / 256, scalar2=None,
                                                        op0=ALU.mult), reads=[ksb], writes=[kmb])
            qm, qmb = qmr.next()
            LV = getattr(cfg, "lv", 9)
            for t in range(T):
                if LV < 1:
                    break
                own = t // 2
                gps = [gpr[0].next(), gpr[1].next()]
                for hh in range(2):
                    k.op(k.pe, lambda hh=hh: nc.tensor.matmul(
                        gps[hh][0][:, 0:16], lhsT=qt[hh * 64:(hh + 1) * 64, t * 128:(t + 1) * 128],
                        rhs=km[hh * 64:(hh + 1) * 64, :], start=True, stop=True),
                        reads=[qb, kmb], writes=[gps[hh][1]])
                if LV < 2:
                    continue
                gm, gmb = gmr.next()
                t8, t8b = t8r.next()
                th, thb = thr.next()
                t1, t1b = t1r.next()
                mv, mvb = mvr.next()
                for hh in range(2):
                    k.op(k.dve, lambda hh=hh: nc.vector.tensor_tensor(
                        out=gm[:, hh, :], in0=gps[hh][0][:, 0:16], in1=pm[:, own, :], op=ALU.add),
                        reads=[gps[hh][1], pmb], writes=[gmb])
                for hh in range(2):
                    k.op(k.dve, lambda hh=hh: nc.vector.max(out=t8[:, hh, :], in_=gm[:, hh, :]),
                         reads=[gmb], writes=[t8b])
                k.op(k.dve, lambda: nc.vector.tensor_scalar(out=th[:], in0=t8[:, :, 2], scalar1=-1e29, scalar2=None,
                                                            op0=ALU.max), reads=[t8b], writes=[thb])
                for hh in range(2):
                    k.op(k.dve, lambda hh=hh: nc.vector.tensor_scalar(
                        out=t1[:, hh, :], in0=gm[:, hh, :], scalar1=th[:, hh:hh + 1], scalar2=1.0,
                        op0=ALU.is_ge, op1=ALU.subtract), reads=[gmb, thb], writes=[t1b])
                for hh in range(2):
                    k.op(k.dve, lambda hh=hh: nc.vector.scalar_tensor_tensor(
                        out=mv[:, 64 * hh:64 * hh + 16], in0=t1[:, hh, :], scalar=30000.0, in1=o3[:, own, :],
                        op0=ALU.mult, op1=ALU.add), reads=[t1b, o3b], writes=[mvb])
                if LV < 3:
                    continue
                tp, tpb = tpr.next()
                k.op(k.pe, lambda: nc.tensor.matmul(tp[:, 0:128], lhsT=mv[:], rhs=C.ident[:],
                                                    start=True, stop=True), reads=[mvb], writes=[tpb])
                if LV == 3:
                    continue
                k.op(k.act, lambda: nc.scalar.copy(out=qm[:, t * 128:(t + 1) * 128], in_=tp[:, 0:128]),
                     reads=[tpb], writes=[qmb])
            if LV >= 4:
                k.dma(k.pool, C.QM_d[c], qm[:], reads=[qmb])


def dsa_prepass(k, cfg, C, layer):
    nc = k.nc
    S, T, TB = cfg.S, cfg.T, cfg.TB
    KEEP = min(256, S // 4)
    with Phase(k, "I") as ph:
        kiT = ph.sb([128, S], BF16, "kiT")
        kiTb = [Buf() for _ in range(T)]
        kw = ph.sb([128, T, 80], F32, "kw")
        kwb = Buf()
        k.dma(k.sp, kw[:], C.KW_d[:, :, :], writes=[kwb])
        gk = ph.sb([128, 64], F32, "gk")
        gkb = Buf()
        k.dma(k.sp, gk[:], C.gk_in[layer // 2], writes=[gkb])
        tri = ph.sb([128, 128], F32, "tri")
        trib = Buf()
        k.dma(k.sp, tri[:], C.tri_in[:, :], writes=[trib])
        pw2 = ph.sb([128, NIT], F32, "pw2")
        pw2b = Buf()
        k.dma(k.sp, pw2[:], C.pow2_in[:, :], writes=[pw2b])
        wabs = ph.sb([128, T, 16], F32, "wabs")
        wsgn = ph.sb([128, T, 16], F32, "wsgn")
        wab, wsb_ = Buf(), Buf()
        k.op(k.act, lambda: nc.scalar.activation(out=wabs[:], in_=kw[:, :, 64:80], func=AF.Abs, scale=1.0 / 32),
             reads=[kwb], writes=[wab])
        k.op(k.act, lambda: nc.scalar.activation(out=wsgn[:], in_=kw[:, :, 64:80], func=AF.Sign),
             reads=[kwb], writes=[wsb_])
        ssr = Ring(ph, 4, [128, 4], F32, "ss")
        jkr = Ring(ph, 1, [128, 64], F32, "jk")
        kkr = Ring(ph, 2, [128, 128], BF16, "kk")
        tpr = Ring(ph, 2, [128, 1024], BF16, "tp", psum=True)
        for t in range(T):
            ss, ssb = ssr.next()
            jk, jkb = jkr.next()
            rstd_chain(k, kw[:, t, 0:64], kwb, ss, ssb, jk[:], jkb, 64)
            kk, kkb = kkr.next()
            k.op(k.dve, lambda: nc.vector.scalar_tensor_tensor(out=kk[:, 0:64], in0=kw[:, t, 0:64], scalar=ss[:, 3:4],
                                                               in1=gk[:], op0=ALU.mult, op1=ALU.mult),
                 reads=[kwb, ssb, gkb], writes=[kkb])
            k.op(k.dve, lambda: nc.vector.tensor_copy(out=kk[:, 64:128], in_=kk[:, 0:64]), reads=[kkb], writes=[kkb])
            tp, tpb = tpr.next()
            k.op(k.pe, lambda: nc.tensor.transpose(out=tp[:, 0:128], in_=kk[:], identity=C.ident[:]),
                 reads=[kkb], writes=[tpb])
            k.op(k.act, lambda: nc.scalar.copy(out=kiT[:, t * 128:(t + 1) * 128], in_=tp[:, 0:128]),
                 reads=[tpb], writes=[kiTb[t]])
        qir = Ring(ph, 2, [128, 8, 128], BF16, "qi")
        dsr = Ring(ph, 2, [128, 16, 128], BF16, "ds")
        spr = Ring(ph, 4, [128, 512], F32, "sp", psum=True)
        apr = Ring(ph, 2, [128, 512], F32, "ap", psum=True)
        rr = Ring(ph, 4, [128, 512], BF16, "r")
        scr = Ring(ph, 2, [128, S], F32, "sc")
        cjr = Ring(ph, 1, [128, S], BF16, "cj")
        m01r = Ring(ph, 2, [128, S], BF16, "m01")
        mtsr = Ring(ph, 2, [128, T, 128], BF16, "mts")
        smr = Ring(ph, 2, [128, 8], F32, "sm")
        hwr = Ring(ph, 2, [128, NIT], F32, "hw")
        for i in range(mtsr.n):
            k.op(k.pool, lambda i=i: nc.gpsimd.memset(mtsr.t[i][:], 0.0), writes=[mtsr.b[i]])
        for t in range(T):
            L = (t + 1) * 128
            qi, qib = qir.next()
            k.dma(k.sp, qi[:], C.QI_d[:, :, t * 128:(t + 1) * 128].rearrange("c p n -> p c n"), writes=[qib])
            ds, dsb = dsr.next()
            for h in range(16):
                k.op(k.pool, lambda h=h: nc.gpsimd.tensor_scalar(out=ds[:, h, :], in0=C.ident[:],
                                                                 scalar1=wsgn[:, t, h:h + 1], scalar2=None,
                                                                 op0=ALU.mult), reads=[wsb_], writes=[dsb])
            sc, scb = scr.next()
            nkb = (L + 511) // 512
            for kb in range(nkb):
                kw_ = min(512, L - kb * 512)
                ap_, apb = apr.next()
                for h in range(16):
                    c, hh = h // 2, h % 2
                    sp_, spb = spr.next()
                    k.op(k.pe, lambda: nc.tensor.matmul(
                        sp_[:, 0:kw_], lhsT=qi[hh * 64:(hh + 1) * 64, c, :],
                        rhs=kiT[hh * 64:(hh + 1) * 64, kb * 512:kb * 512 + kw_], start=True, stop=True),
                        reads=[qib] + kiTb[kb * 4:kb * 4 + (kw_ // 128)], writes=[spb])
                    r, rb = rr.next()
                    k.op(k.act, lambda: nc.scalar.activation(out=r[:, 0:kw_], in_=sp_[:, 0:kw_], func=AF.Relu,
                                                             scale=wabs[:, t, h:h + 1]),
                         reads=[spb, wab], writes=[rb])
                    k.op(k.pe, lambda: nc.tensor.matmul(ap_[:, 0:kw_], lhsT=ds[:, h, :], rhs=r[:, 0:kw_],
                                                        start=(h == 0), stop=(h == 15)),
                         reads=[dsb, rb], writes=[apb])
                k.op(k.dve, lambda: nc.vector.tensor_copy(out=sc[:, kb * 512:kb * 512 + kw_], in_=ap_[:, 0:kw_]),
                     reads=[apb], writes=[scb])
            sm, smb = smr.next()
            hw, hwb = hwr.next()
            k.op(k.dve, lambda: nc.vector.tensor_reduce(out=sm[:, 0:1], in_=sc[:, 0:L], axis=AX.X, op=ALU.max,
                                                        apply_absolute_value=True), reads=[scb], writes=[smb])
            k.op(k.dve, lambda: nc.vector.tensor_tensor(out=sc[:, L - 128:L], in0=sc[:, L - 128:L], in1=tri[:],
                                                        op=ALU.add), reads=[scb, trib], writes=[scb])
            k.op(k.dve, lambda: nc.vector.tensor_scalar(out=sm[:, 1:2], in0=sm[:, 0:1], scalar1=-1.0, scalar2=None,
                                                        op0=ALU.mult), reads=[smb], writes=[smb])
            k.op(k.dve, lambda: nc.vector.tensor_scalar(out=hw[:], in0=pw2[:], scalar1=sm[:, 0:1], scalar2=None,
                                                        op0=ALU.mult), reads=[smb, pw2b], writes=[hwb])
            cj, cjb = cjr.next()
            for it in range(NIT):
                k.op(k.dve, lambda: nc.vector.tensor_tensor(out=sm[:, 2:3], in0=sm[:, 1:2], in1=hw[:, it:it + 1],
                                                            op=ALU.add), reads=[smb, hwb], writes=[smb])
                k.op(k.dve, lambda: nc.vector.tensor_scalar(out=cj[:, 0:L], in0=sc[:, 0:L], scalar1=sm[:, 2:3],
                                                            scalar2=0.0, op0=ALU.is_ge, op1=ALU.add,
                                                            accum_out=sm[:, 3:4]),
                     reads=[scb, smb], writes=[cjb, smb])
                k.op(k.dve, lambda: nc.vector.scalar_tensor_tensor(out=sm[:, 4:5], in0=sm[:, 3:4],
                                                                   scalar=KEEP - 0.5, in1=hw[:, it:it + 1],
                                                                   op0=ALU.is_ge, op1=ALU.mult),
                     reads=[smb, hwb], writes=[smb])
                k.op(k.dve, lambda: nc.vector.tensor_tensor(out=sm[:, 1:2], in0=sm[:, 1:2], in1=sm[:, 4:5],
                                                            op=ALU.add), reads=[smb], writes=[smb])
            m01, m01b = m01r.next()
            k.op(k.dve, lambda: nc.vector.tensor_scalar(out=m01[:, 0:L], in0=sc[:, 0:L], scalar1=sm[:, 1:2],
                                                        scalar2=None, op0=ALU.is_ge), reads=[scb, smb], writes=[m01b])
            mts, mtsb = mtsr.next()
            for j0 in range(0, t + 1, 8):
                nj = min(8, t + 1 - j0)
                tp, tpb = tpr.next()
                for jj in range(nj):
                    k.op(k.pe, lambda jj=jj: nc.tensor.transpose(
                        out=tp[:, jj * 128:(jj + 1) * 128], in_=m01[:, (j0 + jj) * 128:(j0 + jj + 1) * 128],
                        identity=C.ident[:]), reads=[m01b], writes=[tpb])
                k.op(k.act, lambda: nc.scalar.copy(out=mts[:, j0:j0 + nj, :],
                                                   in_=tp[:, 0:nj * 128].rearrange("p (j n) -> p j n", n=128)),
                     reads=[tpb], writes=[mtsb])
            i_q, sub = t // 4, t % 4
            nkt = 4 * i_q + 4
            k.dma(k.pool, C.MK_d[i_q, :, 0:nkt, sub * 128:(sub + 1) * 128], mts[:, 0:nkt, :], reads=[mtsb])


def attention(k, cfg, C, mode, pairs, tag):
    nc = k.nc
    S, T, TB = cfg.S, cfg.T, cfg.TB
    with Phase(k, "B" + tag) as ph:
        qr = Ring(ph, 2, [128, S], BF16, "q")
        kr = Ring(ph, 2, [128, S], BF16, "k")
        ver = Ring(ph, 2, [128, T, 128], BF16, "ve")
        vor = Ring(ph, 2, [128, T, 128], BF16, "vo")
        for i in range(2):
            k.op(k.pool, lambda i=i: nc.gpsimd.memset(ver.t[i][:], 0.0), writes=[ver.b[i]])
            k.op(k.pool, lambda i=i: nc.gpsimd.memset(vor.t[i][:], 0.0), writes=[vor.b[i]])
        mnr = Ring(ph, 2, [128, 2, 1024], BF16, "mn")
        ptr = Ring(ph, 6, [128, 512], BF16, "pt")
        mor = Ring(ph, 2, [128, S], BF16, "mo")
        recr = Ring(ph, 2, [128, 512], F32, "rec")
        psr = Ring(ph, 4, [128, 512], F32, "ps", psum=True)
        por = Ring(ph, 2, [128, 512], F32, "po", psum=True)
        pdr = Ring(ph, 2, [128, 512], F32, "pd", psum=True)
        Mx_d = C.Mdil_d if mode == "dil" else C.Mnear_d
        if mode == "moba":
            oh = ph.sb([128, S], BF16, "oh")
            ohb = Buf()
            k.dma(k.sp, oh[:], C.onehot_in[:, :], writes=[ohb])
            qmr = Ring(ph, 2, [128, S], BF16, "qm")
        if mode == "dsa":
            mkr = Ring(ph, 2, [128, 16, 512], BF16, "mk")
        if mode == "dil":
            cf = ph.sb([128, 2432], BF16, "cf")
            cfb = Buf()
            k.dma(k.sp, cf[:], C.cfar_in[:, :], writes=[cfb])
        for c in pairs:
            qt, qb = qr.next()
            kt, kb = kr.next()
            ve, veb = ver.next()
            vo, vob = vor.next()
            mn, mnb = mnr.next()
            k.dma(k.sp, qt[:], C.QT_d[c], writes=[qb])
            k.dma(k.sp, kt[:], C.KT_d[c], writes=[kb])
            k.dma(k.sp, ve[:, :, 0:64], C.V_d[c, :, :, 0:64], writes=[veb])
            k.dma(k.sp, vo[:, :, 64:128], C.V_d[c, :, :, 64:128], writes=[vob])
            k.dma(k.sp, mn[:], Mx_d[2 * c:2 * c + 2].rearrange("h p n -> p h n"), writes=[mnb])
            if mode == "moba":
                qm, qmb = qmr.next()
                k.dma(k.sp, qm[:], C.QM_d[c], writes=[qmb])
            mo, mob = mor.next()
            for i in range(TB):
                q0 = i * 512
                jhi = 4 * i + 3
                jlo = max(0, 4 * i - 16) if mode == "dil" else 0
                po, pob = por.next()
                pd, pdb = pdr.next()
                seq = [(j, hh) for j in range(jlo, jhi + 1) for hh in range(2)]
                mk = None
                for idx, (j, hh) in enumerate(seq):
                    if mode == "dsa" and hh == 0 and (j % 16 == 0):
                        mk, mkb = mkr.next()
                        g1 = min(jhi + 1, j + 16)
                        k.dma(k.sp, mk[:, 0:g1 - j, :], C.MK_d[i, :, j:g1, :], writes=[mkb])
                    rel = q0 - 128 * j
                    near = rel <= 128
                    hd = 2 * c + hh
                    ps, psb = psr.next()
                    k.op(k.pe, lambda: nc.tensor.matmul(
                        ps[:], lhsT=kt[hh * 64:(hh + 1) * 64, j * 128:(j + 1) * 128],
                        rhs=qt[hh * 64:(hh + 1) * 64, q0:q0 + 512], start=True, stop=(mode != "moba")),
                        reads=[kb, qb], writes=[psb])
                    if mode == "moba":
                        k.op(k.pe, lambda: nc.tensor.matmul(
                            ps[:], lhsT=oh[64 * hh:64 * hh + 16, j * 128:(j + 1) * 128],
                            rhs=qm[64 * hh:64 * hh + 16, q0:q0 + 512],
                            start=False, stop=True), reads=[ohb, qmb], writes=[psb])
                    pt, ptb = ptr.next()
                    k.op(k.act, lambda: nc.scalar.activation(out=pt[:], in_=ps[:], func=AF.Exp,
                                                             bias=C.c31[:, hd:hd + 1], scale=1.0),
                         reads=[psb, C.c31b], writes=[ptb])
                    if near:
                        o = rel + 384
                        k.op(k.pool, lambda: nc.gpsimd.tensor_tensor(out=pt[:], in0=pt[:], in1=mn[:, hh, o:o + 512],
                                                                     op=ALU.mult), reads=[ptb, mnb], writes=[ptb])
                    elif mode == "dil":
                        o = rel - 129
                        k.op(k.pool, lambda: nc.gpsimd.tensor_tensor(out=pt[:], in0=pt[:], in1=cf[:, o:o + 512],
                                                                     op=ALU.mult), reads=[ptb, cfb], writes=[ptb])
                    if mode == "dsa":
                        jj = j % 16
                        k.op(k.pool, lambda: nc.gpsimd.tensor_tensor(out=pt[:], in0=pt[:], in1=mk[:, jj, :],
                                                                     op=ALU.mult), reads=[ptb, mkb], writes=[ptb])
                    first, last = idx == 0, idx == len(seq) - 1
                    vv, vvb = (ve, veb) if hh == 0 else (vo, vob)
                    k.op(k.pe, lambda: nc.tensor.matmul(po[:], lhsT=vv[:, j, :], rhs=pt[:], start=first, stop=last),
                         reads=[vvb, ptb], writes=[pob])
                    on = C.onesE if hh == 0 else C.onesO
                    k.op(k.pe, lambda: nc.tensor.matmul(pd[:], lhsT=on[:], rhs=pt[:], start=first, stop=last),
                         reads=[ptb, C.onesb], writes=[pdb])
                rec, recb = recr.next()
                k.op(k.dve, lambda: nc.vector.reciprocal(out=rec[:], in_=pd[:]), reads=[pdb], writes=[recb])
                k.op(k.dve, lambda: nc.vector.tensor_tensor(out=mo[:, q0:q0 + 512], in0=po[:], in1=rec[:],
                                                            op=ALU.mult), reads=[pob, recb], writes=[mob])
            k.dma(k.pool, C.MT_d[c], mo[:], reads=[mob])


def phase_C(k, cfg, C, hT, hTb, layer):
    nc = k.nc
    S, T = cfg.S, cfg.T
    with Phase(k, "C") as ph:
        wo = ph.sb([128, KC, D], BF16, "wo")
        wob = Buf()
        wst = Ring(ph, 2, [128, KC, 512], F32, "wst")
        Wv = C.w_o[layer].rearrange("(c p) n -> p c n", p=128)
        for nb in range(2):
            ws, wsb = wst.next()
            k.dma(k.sp, ws[:], Wv[:, :, nb * 512:(nb + 1) * 512], writes=[wsb])
            k.op(k.pool, lambda: nc.gpsimd.tensor_copy(out=wo[:, :, nb * 512:(nb + 1) * 512], in_=ws[:]),
                 reads=[wsb], writes=[wob])
        g_bc = ph.sb([128, D], F32, "g")
        gb = Buf()
        k.dma(k.sp, g_bc[:], C.g_ffn[layer], writes=[gb])
        rings = norm_rings(ph)
        mtr = Ring(ph, 2, [128, KC, 128], BF16, "mt")
        xr = Ring(ph, 3, [128, D], F32, "x")
        xmr = Ring(ph, 2, [128, D], F32, "xm")
        pacc = Ring(ph, 4, [128, 512], F32, "pacc", psum=True)
        xsrc = C.x_in if layer == 0 else C.xs
        for t in range(T):
            mt, mtb = mtr.next()
            k.dma(k.sp, mt[:], C.MT_d[:, :, t * 128:(t + 1) * 128].rearrange("c p n -> p c n"), writes=[mtb])
            xt, xb = xr.next()
            k.dma(k.sp, xt[:], xsrc[t * 128:(t + 1) * 128, :], writes=[xb])
            xm, xmb = xmr.next()
            for nb in range(2):
                pa, pab = pacc.next()
                for c in range(KC):
                    k.op(k.pe, lambda c=c: nc.tensor.matmul(pa[:], lhsT=mt[:, c, :],
                                                            rhs=wo[:, c, nb * 512:(nb + 1) * 512],
                                                            start=(c == 0), stop=(c == KC - 1)),
                         reads=[mtb, wob], writes=[pab])
                k.op(k.dve, lambda: nc.vector.tensor_tensor(out=xm[:, nb * 512:(nb + 1) * 512], in0=pa[:],
                                                            in1=xt[:, nb * 512:(nb + 1) * 512], op=ALU.add),
                     reads=[pab, xb], writes=[xmb])
            k.dma(k.pool, C.xs[t * 128:(t + 1) * 128, :], xm[:], reads=[xmb])
            norm_tile(k, C, xm, xmb, g_bc, gb, hT, hTb[t], t, rings)


def phase_F1(k, cfg, C, hT, hTb, layer):
    nc = k.nc
    S, T, TB = cfg.S, cfg.T, cfg.TB
    with Phase(k, "F1") as ph:
        cp = ph.sb([128, 4, 44], F32, "cp")
        cpb = Buf()
        k.dma(k.sp, cp[:], C.convp[layer], writes=[cpb])
        Wv = C.w_up[layer].rearrange("(c p) n -> p c n", p=128)
        wst = Ring(ph, 2, [128, KC, 256], F32, "wst")
        wbf = Ring(ph, 2, [128, KC, 256], BF16, "wbf")
        pacc = Ring(ph, 4, [128, 512], F32, "pacc", psum=True)
        ubr = [Ring(ph, 2, [128, 514], F32, "ubv"), Ring(ph, 2, [128, 514], F32, "ubg")]
        car = Ring(ph, 3, [128, 512], F32, "ca")
        cbr = Ring(ph, 3, [128, 512], F32, "cb")
        sgr = Ring(ph, 2, [128, 512], F32, "sg")
        asr = Ring(ph, 2, [128, S], BF16, "as")
        for f in range(NF):
            ws, wsb = wst.next()
            k.dma(k.sp, ws[:, :, 0:128], Wv[:, :, f * 128:(f + 1) * 128], writes=[wsb])
            k.dma(k.sp, ws[:, :, 128:256], Wv[:, :, DFF + f * 128:DFF + (f + 1) * 128], writes=[wsb])
            wb, wbb = wbf.next()
            k.op(k.pool, lambda: nc.gpsimd.tensor_copy(out=wb[:], in_=ws[:]), reads=[wsb], writes=[wbb])
            a_s, asb = asr.next()
            prev = [None, None]
            for tb in range(TB):
                cv = [None, None]
                for half in range(2):
                    col = f if half == 0 else NF + f
                    pa, pab = pacc.next()
                    for kc in range(KC):
                        k.op(k.pe, lambda kc=kc: nc.tensor.matmul(
                            pa[:], lhsT=wb[:, kc, half * 128:(half + 1) * 128],
                            rhs=hT[:, kc, tb * 512:(tb + 1) * 512], start=(kc == 0), stop=(kc == KC - 1)),
                            reads=[wbb] + hTb[tb * 4:(tb + 1) * 4], writes=[pab])
                    ub, ubb = ubr[half].next()
                    if tb == 0:
                        k.op(k.pool, lambda: nc.gpsimd.memset(ub[:, 0:2], 0.0), writes=[ubb])
                    else:
                        pu, pub = prev[half]
                        k.op(k.pool, lambda: nc.gpsimd.tensor_copy(out=ub[:, 0:2], in_=pu[:, 512:514]),
                             reads=[pub], writes=[ubb])
                    k.op(k.act, lambda: nc.scalar.copy(out=ub[:, 2:514], in_=pa[:]), reads=[pab], writes=[ubb])
                    prev[half] = (ub, ubb)
                    r = car if half == 0 else cbr
                    c1, c1b = r.next()
                    k.op(k.dve, lambda: nc.vector.tensor_scalar(out=c1[:], in0=ub[:, 2:514],
                                                                scalar1=cp[:, 2, col:col + 1],
                                                                scalar2=cp[:, 3, col:col + 1],
                                                                op0=ALU.mult, op1=ALU.add),
                         reads=[ubb, cpb], writes=[c1b])
                    c2, c2b = r.next()
                    k.op(k.dve, lambda: nc.vector.scalar_tensor_tensor(out=c2[:], in0=ub[:, 1:513],
                                                                       scalar=cp[:, 1, col:col + 1], in1=c1[:],
                                                                       op0=ALU.mult, op1=ALU.add),
                         reads=[ubb, cpb, c1b], writes=[c2b])
                    c3, c3b = r.next()
                    k.op(k.dve, lambda: nc.vector.scalar_tensor_tensor(out=c3[:], in0=ub[:, 0:512],
                                                                       scalar=cp[:, 0, col:col + 1], in1=c2[:],
                                                                       op0=ALU.mult, op1=ALU.add),
                         reads=[ubb, cpb, c2b], writes=[c3b])
                    cv[half] = (c3, c3b)
                sg, sgb = sgr.next()
                k.op(k.act, lambda: nc.scalar.activation(out=sg[:], in_=cv[1][0][:], func=AF.Silu),
                     reads=[cv[1][1]], writes=[sgb])
                k.op(k.dve, lambda: nc.vector.tensor_tensor(out=a_s[:, tb * 512:(tb + 1) * 512], in0=sg[:],
                                                            in1=cv[0][0][:], op=ALU.mult),
                     reads=[sgb, cv[0][1]], writes=[asb])
            k.dma(k.pool, C.AT_d[f], a_s[:], reads=[asb])


def phase_F2(k, cfg, C, hT, hTb, layer, last):
    nc = k.nc
    S, T = cfg.S, cfg.T
    with Phase(k, "F2") as ph:
        wd = ph.sb([128, NF, D], BF16, "wd")
        wdb = Buf()
        wst = Ring(ph, 2, [128, NF, 128], F32, "wst")
        Wv = C.w_down[layer].rearrange("(f p) n -> p f n", p=128)
        for cb in range(8):
            ws, wsb = wst.next()
            k.dma(k.sp, ws[:], Wv[:, :, cb * 128:(cb + 1) * 128], writes=[wsb])
            k.op(k.pool, lambda: nc.gpsimd.tensor_copy(out=wd[:, :, cb * 128:(cb + 1) * 128], in_=ws[:]),
                 reads=[wsb], writes=[wdb])
        g_bc = ph.sb([128, D], F32, "g")
        gb = Buf()
        k.dma(k.sp, g_bc[:], C.g_fin[:, :] if last else C.g_attn[layer + 1], writes=[gb])
        rings = norm_rings(ph)
        atr = Ring(ph, 2, [128, NF, 128], BF16, "at")
        xr = Ring(ph, 3, [128, D], F32, "x")
        xnr = Ring(ph, 2, [128, D], F32, "xn")
        yor = Ring(ph, 2, [128, D], F32, "yo")
        pacc = Ring(ph, 4, [128, 512], F32, "pacc", psum=True)
        for t in range(T):
            at, atb = atr.next()
            k.dma(k.sp, at[:], C.AT_d[:, :, t * 128:(t + 1) * 128].rearrange("f p n -> p f n"), writes=[atb])
            xt, xb = xr.next()
            k.dma(k.sp, xt[:], C.xs[t * 128:(t + 1) * 128, :], writes=[xb])
            xn, xnb = xnr.next()
            for nb in range(2):
                pa, pab = pacc.next()
                for f in range(NF):
                    k.op(k.pe, lambda f=f: nc.tensor.matmul(pa[:], lhsT=at[:, f, :],
                                                            rhs=wd[:, f, nb * 512:(nb + 1) * 512],
                                                            start=(f == 0), stop=(f == NF - 1)),
                         reads=[atb, wdb], writes=[pab])
                k.op(k.dve, lambda: nc.vector.tensor_tensor(out=xn[:, nb * 512:(nb + 1) * 512], in0=pa[:],
                                                            in1=xt[:, nb * 512:(nb + 1) * 512], op=ALU.add),
                     reads=[pab, xb], writes=[xnb])
            if last:
                junk, junkb = rings["junk"].next()
                ss, ssb = rings["ss"].next()
                rstd_chain(k, xn[:], xnb, ss, ssb, junk[:], junkb, D)
                yo, yob = yor.next()
                k.op(k.dve, lambda: nc.vector.scalar_tensor_tensor(out=yo[:], in0=xn[:], scalar=ss[:, 3:4],
                                                                   in1=g_bc[:], op0=ALU.mult, op1=ALU.mult),
                     reads=[xnb, ssb, gb], writes=[yob])
                k.dma(k.pool, C.out[t * 128:(t + 1) * 128, :], yo[:], reads=[yob])
            else:
                k.dma(k.pool, C.xs[t * 128:(t + 1) * 128, :], xn[:], reads=[xnb])
                norm_tile(k, C, xn, xnb, g_bc, gb, hT, hTb[t], t, rings)


def build(cfg):
    S, T, TB = cfg.S, cfg.T, cfg.TB
    nc = bass.Bass("TRN2", target_bir_lowering=False)
    k = K(nc)
    C = NS()

    def din(name, shape, dt=F32):
        return nc.dram_tensor(name, list(shape), dt, kind="ExternalInput").ap()

    def dscr(name, shape, dt):
        kind = "ExternalOutput" if (cfg.debug and name in cfg.debug) else "Internal"
        return nc.dram_tensor(name, list(shape), dt, kind=kind).ap()

    C.x_in = din("x", [S, D])
    C.w_in_even = din("w_in_even", [2, D, 4176])
    C.w_in_odd = din("w_in_odd", [2, D, 3072])
    C.w_o = din("w_o", [4, D, D])
    C.w_up = din("w_up", [4, D, 2 * DFF])
    C.w_down = din("w_down", [4, DFF, D])
    C.g_attn = din("g_attn", [4, 128, D])
    C.g_ffn = din("g_ffn", [4, 128, D])
    C.g_fin = din("g_fin", [128, D])
    C.gk_in = din("gk", [2, 128, 64])
    C.convp = din("convp", [4, 128, 4, 44])
    C.G_in = din("G", [16, 128, 1024])
    c31_in = din("c31", [128, 16])
    ident_in = din("ident", [128, 128], BF16)
    ones_in = din("ones2", [128, 2, 128], BF16)
    C.caus_in = din("caus", [128, 1024])
    C.cdil_in = din("cdil", [128, 1024])
    C.cfar_in = din("cfar", [128, 2432], BF16)
    C.onehot_in = din("onehot", [128, S], BF16)
    C.pastmask_in = din("pastmask", [128, 16, 16])
    C.own30k_in = din("own30k", [128, 16, 16])
    C.tri_in = din("tri", [128, 128])
    C.pow2_in = din("pow2", [128, NIT])

    C.QT_d = dscr("QT_d", [8, 128, S], BF16)
    C.KT_d = dscr("KT_d", [8, 128, S], BF16)
    C.V_d = dscr("V_d", [8, 128, T, 128], BF16)
    C.QI_d = dscr("QI_d", [8, 128, S], BF16)
    C.KW_d = dscr("KW_d", [128, T, 80], F32)
    C.QM_d = dscr("QM_d", [4, 128, S], BF16)
    C.MK_d = dscr("MK_d", [TB, 128, T, 512], BF16)
    C.MT_d = dscr("MT_d", [8, 128, S], BF16)
    C.AT_d = dscr("AT_d", [NF, 128, S], BF16)
    C.xs = dscr("xs", [S, D], F32)
    C.Mnear_d = dscr("Mnear_d", [16, 128, 1024], BF16)
    C.Mdil_d = dscr("Mdil_d", [16, 128, 1024], BF16)
    C.out = nc.dram_tensor("out", [S, D], F32, kind="ExternalOutput").ap()

    stop_after = cfg.stop_after

    with ExitStack() as top:
        C.ident = top.enter_context(nc.sbuf_tensor("ident_sb", [128, 128], BF16))
        C.c31 = top.enter_context(nc.sbuf_tensor("c31_sb", [128, 16], F32))
        ones2 = top.enter_context(nc.sbuf_tensor("ones_sb", [128, 2, 128], BF16))
        C.onesE = ones2[:, 0, :]
        C.onesO = ones2[:, 1, :]
        C.c31b, C.onesb, identb = Buf(), Buf(), Buf()
        k.dma(k.sp, C.ident[:], ident_in[:, :], writes=[identb])
        k.dma(k.sp, C.c31[:], c31_in[:, :], writes=[C.c31b])
        k.dma(k.sp, ones2[:], ones_in[:, :, :], writes=[C.onesb])
        k.barrier()
        phase_tables(k, cfg, C)

        def done(tag):
            return stop_after == tag

        fin = False
        for layer in range(cfg.depth):
            if fin:
                break
            if layer == 0:
                with Phase(k, "H0") as hp:
                    hT = hp.sb([128, KC, S], BF16, "hT")
                    hTb = [Buf() for _ in range(T)]
                    phase_A1(k, cfg, C, hT, hTb, 0)
                    phase_A2(k, cfg, C, hT, hTb, 0)
            if done("A%d" % layer):
                break
            if layer % 2 == 0:
                moba_prepass(k, cfg, C)
                if done("G%d" % layer):
                    break
                attention(k, cfg, C, "moba", [0, 1, 2, 3], "m")
                if done("M%d" % layer):
                    break
                dsa_prepass(k, cfg, C, layer)
                attention(k, cfg, C, "dsa", [4, 5, 6, 7], "d")
            else:
                attention(k, cfg, C, "dil", list(range(8)), "l")
            if done("B%d" % layer):
                break
            with Phase(k, "HC") as hp:
                hT = hp.sb([128, KC, S], BF16, "hT")
                hTb = [Buf() for _ in range(T)]
                phase_C(k, cfg, C, hT, hTb, layer)
                if done("C%d" % layer):
                    fin = True
                else:
                    phase_F1(k, cfg, C, hT, hTb, layer)
            if fin or done("F1%d" % layer):
                break
            last = layer == cfg.depth - 1
            with Phase(k, "HF") as hp:
                hT = hp.sb([128, KC, S], BF16, "hT")
                hTb = [Buf() for _ in range(T)]
                phase_F2(k, cfg, C, hT, hTb, layer, last)
                if not last and not done("F2%d" % layer):
                    phase_A2(k, cfg, C, hT, hTb, layer + 1)
            if done("F2%d" % layer):
                break
        k.final_wait()
    k.stack.close()
    return nc


def host_inputs(cfg, b, x, w_in_even, idx_k_norm, w_in_odd, w_o, rel_bias, attn_norm, ffn_norm,
                w_up, conv_w, conv_b, w_down, final_norm):
    S = cfg.S
    bf = ml_dtypes.bfloat16
    m = {}
    m["x"] = np.ascontiguousarray(x[b], dtype=np.float32)
    m["w_in_even"] = w_in_even
    m["w_in_odd"] = w_in_odd
    m["w_o"] = w_o
    m["w_up"] = w_up
    m["w_down"] = w_down
    m["g_attn"] = np.ascontiguousarray(np.broadcast_to(attn_norm[:, None, :], (4, 128, D)))
    m["g_ffn"] = np.ascontiguousarray(np.broadcast_to(ffn_norm[:, None, :], (4, 128, D)))
    m["g_fin"] = np.ascontiguousarray(np.broadcast_to(final_norm[None, :], (128, D)))
    m["gk"] = np.ascontiguousarray(np.broadcast_to(idx_k_norm[:, None, :], (2, 128, 64)))
    cp = np.zeros((4, 128, 4, 44), np.float32)
    for l in range(4):
        for j in range(3):
            cp[l, :, j, :] = conv_w[l, j].reshape(44, 128).T
        cp[l, :, 3, :] = conv_b[l].reshape(44, 128).T
    m["convp"] = cp
    p = np.arange(128)[:, None]
    j = np.arange(1024)[None, :]
    d = j - 384 - p
    bk = t5_bucket_np(d)
    m["G"] = np.ascontiguousarray(np.transpose(rel_bias[bk, :], (2, 0, 1))).astype(np.float32)
    m["c31"] = np.ascontiguousarray(np.broadcast_to(rel_bias[31][None, :], (128, 16))).astype(np.float32)
    m["ident"] = np.eye(128, dtype=np.float32).astype(bf)
    o2 = np.zeros((128, 2, 128), np.float32)
    o2[:, 0, 0:64] = 1.0
    o2[:, 1, 64:128] = 1.0
    m["ones2"] = o2.astype(bf)
    m["caus"] = (d >= 0).astype(np.float32)

    def cmul(dd):
        return (((dd >= 0) & (dd <= 128)).astype(np.float32)
                + ((dd >= 0) & (dd <= 512) & (dd % 4 == 0)).astype(np.float32)
                + ((dd >= 0) & (dd <= 2048) & (dd % 16 == 0)).astype(np.float32))
    m["cdil"] = cmul(d)
    jf = np.arange(2432)[None, :]
    m["cfar"] = cmul(jf - p + 129).astype(bf)
    kk = np.arange(S)[None, :]
    n128 = np.arange(128)[:, None]
    m["onehot"] = ((kk // 256 == (n128 % 64)) & ((n128 % 64) < 16)).astype(np.float32).astype(bf)
    own = np.arange(16)[:, None]
    nn = np.arange(16)[None, :]
    pmk = np.where(nn < own, 0.0, -1e30).astype(np.float32)
    m["pastmask"] = np.ascontiguousarray(np.broadcast_to(pmk[None], (128, 16, 16)))
    o3 = np.where(nn == own, 30000.0, 0.0).astype(np.float32)
    m["own30k"] = np.ascontiguousarray(np.broadcast_to(o3[None], (128, 16, 16)))
    q = np.arange(128)[:, None]
    kx = np.arange(128)[None, :]
    m["tri"] = np.where(kx <= q, 0.0, -1e30).astype(np.float32)
    m["pow2"] = np.ascontiguousarray(np.broadcast_to((2.0 ** -np.arange(NIT))[None, :], (128, NIT))).astype(np.float32)
    return m


_CACHE = {}


def kernel(**inputs):
    inputs = {k_: np.asarray(v) for k_, v in inputs.items()}
    x = inputs["x"]
    B, S, _ = x.shape
    cfg = Cfg(S=S, depth=4)
    if "nc" not in _CACHE:
        _CACHE["nc"] = build(cfg)
    nc = _CACHE["nc"]
    in_maps = []
    for core in range(8):
        in_maps.append(host_inputs(cfg, core // 2, **inputs))
    res = run_bass_kernel_spmd(nc, in_maps, core_ids=list(range(8)))
    outs = [res.results[2 * b]["out"] for b in range(B)]
    return np.stack(outs, axis=0).astype(np.float32)
```

```python
import math
from contextlib import ExitStack

import numpy as np
import ml_dtypes
import concourse.bass as bass
import concourse.mybir as mybir
from concourse.bass_utils import run_bass_kernel_spmd

F32 = mybir.dt.float32
BF16 = mybir.dt.bfloat16
AF = mybir.ActivationFunctionType
ALU = mybir.AluOpType
AX = mybir.AxisListType

D = 1024
KC = 8
DFF = 2816
NF = 22
NEG = -30000.0
SAME_ENGINE_SYNC = True


class Buf:
    __slots__ = ("name", "w", "r")

    def __init__(self, name=""):
        self.name = name
        self.w = {}
        self.r = {}


class Eng:
    def __init__(self, K, name, e, is_pe=False):
        self.K = K
        self.name = name
        self.e = e
        self.is_pe = is_pe
        self.sem = K.new_sem("pg_" + name)
        self.n = 0
        self.waited = {}

    def wait(self, sem, val):
        key = id(sem)
        if self.waited.get(key, 0) >= val:
            return
        self.waited[key] = val
        self.e.wait_ge(sem, val)


class K:
    def __init__(self, nc):
        self.nc = nc
        self.stack = ExitStack()
        self.sems = []
        self.pe = Eng(self, "pe", nc.tensor, is_pe=True)
        self.act = Eng(self, "act", nc.scalar)
        self.dve = Eng(self, "dve", nc.vector)
        self.pool = Eng(self, "pool", nc.gpsimd)
        self.sp = Eng(self, "sp", nc.sync)
        self.engs = [self.pe, self.act, self.dve, self.pool, self.sp]
        self.dq = {}
        for q in (self.sp, self.pool, self.act):
            self.dq[q.name] = dict(sems=[self.new_sem("dq_%s%d" % (q.name, i)) for i in range(8)],
                                   tot=[0] * 8, i=0)

    def new_sem(self, name):
        s = self.stack.enter_context(self.nc.semaphore(name))
        self.sems.append(s)
        return s

    def _deps(self, X, reads, writes, acc=False):
        for b in reads:
            for sem, val in b.w.values():
                if (sem is X.sem) and (X.is_pe or not SAME_ENGINE_SYNC):
                    continue
                X.wait(sem, val)
        for b in writes:
            for sem, val in list(b.w.values()) + list(b.r.values()):
                if (sem is X.sem) and (X.is_pe or not SAME_ENGINE_SYNC):
                    continue
                X.wait(sem, val)

    def _mark(self, ev, reads, writes):
        sem, val = ev
        for b in reads:
            b.r[id(sem)] = ev
        for b in writes:
            b.w = {id(sem): ev}
            b.r = {}

    def op(self, X, ins_fn, reads=(), writes=()):
        self._deps(X, reads, writes)
        ins = ins_fn()
        ins.then_inc(X.sem, 1)
        X.n += 1
        self._mark((X.sem, X.n), reads, writes)
        return ins

    def dma(self, Q, out, in_, reads=(), writes=(), **kw):
        self._deps(Q, reads, writes)
        dq = self.dq[Q.name]
        i = dq["i"]
        dq["i"] = (i + 1) % len(dq["sems"])
        sem = dq["sems"][i]
        if dq["tot"][i] > 0:
            Q.wait(sem, dq["tot"][i])
        Q.e.dma_start(out=out, in_=in_, **kw).then_inc(sem, 16)
        dq["tot"][i] += 16
        self._mark((sem, dq["tot"][i]), reads, writes)

    def barrier(self):
        for X in self.engs:
            for Y in self.engs:
                if Y is not X and Y.n > 0:
                    X.wait(Y.sem, Y.n)
            for dq in self.dq.values():
                for sem, tot in zip(dq["sems"], dq["tot"]):
                    if tot > 0:
                        X.wait(sem, tot)

    def final_wait(self):
        X = self.sp
        for Y in self.engs:
            if Y is not X and Y.n > 0:
                X.wait(Y.sem, Y.n)
        for dq in self.dq.values():
            for sem, tot in zip(dq["sems"], dq["tot"]):
                if tot > 0:
                    X.wait(sem, tot)


class Phase:
    _uid = [0]

    def __init__(self, k, name):
        self.k = k
        Phase._uid[0] += 1
        self.name = "%s%d" % (name, Phase._uid[0])
        self.stack = ExitStack()
        self.cnt = 0

    def __enter__(self):
        return self

    def __exit__(self, *a):
        self.k.barrier()
        self.stack.close()
        return False

    def sb(self, shape, dt, name=None):
        self.cnt += 1
        t = self.stack.enter_context(
            self.k.nc.sbuf_tensor("%s_%s%d" % (self.name, name or "t", self.cnt), list(shape), dt))
        return t

    def ps(self, shape, dt, name=None):
        self.cnt += 1
        t = self.stack.enter_context(
            self.k.nc.psum_tensor("%s_%s%d" % (self.name, name or "p", self.cnt), list(shape), dt))
        return t


class Ring:
    def __init__(self, ph, n, shape, dt, name, psum=False):
        self.t = [(ph.ps if psum else ph.sb)(shape, dt, name) for _ in range(n)]
        self.b = [Buf("%s%d" % (name, i)) for i in range(n)]
        self.i = -1
        self.n = n

    def next(self):
        self.i = (self.i + 1) % self.n
        return self.t[self.i], self.b[self.i]


def t5_bucket_np(dist):
    n = np.maximum(dist, 0)
    max_exact = 16
    nf = np.maximum(n, max_exact).astype(np.float32)
    large = max_exact + (np.log(nf / max_exact) / math.log(128 / max_exact) * (32 - max_exact)).astype(np.int32)
    large = np.minimum(large, 31)
    return np.where(n < max_exact, n, large)


class Cfg:
    def __init__(self, S=4096, depth=4, debug=None, stop_after=None):
        self.S = S
        self.T = S // 128
        self.TB = S // 512
        self.depth = depth
        self.debug = debug
        self.stop_after = stop_after


NIT = 18


class NS:
    pass


class Lag:
    def __init__(self, n):
        self.q = []
        self.n = n

    def push(self, fn):
        self.q.append(fn)
        while len(self.q) > self.n:
            self.q.pop(0)()

    def flush(self):
        while self.q:
            self.q.pop(0)()


def norm_rings(ph):
    return dict(
        junk=Ring(ph, 1, [128, D], BF16, "junk"),
        ss=Ring(ph, 4, [128, 4], F32, "ss"),
        hb=Ring(ph, 2, [128, D], BF16, "hb"),
        pT=Ring(ph, 2, [128, D], BF16, "pT", psum=True),
    )


def rstd_chain(k, xt, xb, ss, ssb, junk, junkb, n):
    nc = k.nc
    k.op(k.act, lambda: nc.scalar.activation(out=junk, in_=xt, func=AF.Square, accum_out=ss[:, 0:1]),
         reads=[xb], writes=[junkb, ssb])
    k.op(k.dve, lambda: nc.vector.tensor_scalar(out=ss[:, 1:2], in0=ss[:, 0:1], scalar1=1.0 / n, scalar2=1e-6,
                                                op0=ALU.mult, op1=ALU.add), reads=[ssb], writes=[ssb])
    k.op(k.act, lambda: nc.scalar.activation(out=ss[:, 2:3], in_=ss[:, 1:2], func=AF.Sqrt),
         reads=[ssb], writes=[ssb])
    k.op(k.dve, lambda: nc.vector.reciprocal(out=ss[:, 3:4], in_=ss[:, 2:3]), reads=[ssb], writes=[ssb])


def norm_tile(k, C, xt, xb, g_bc, gb, hT, hTb_tile, t, rings):
    nc = k.nc
    junk, junkb = rings["junk"].next()
    ss, ssb = rings["ss"].next()
    rstd_chain(k, xt[:], xb, ss, ssb, junk[:], junkb, D)
    hb, hbb = rings["hb"].next()
    k.op(k.dve, lambda: nc.vector.scalar_tensor_tensor(out=hb[:], in0=xt[:], scalar=ss[:, 3:4], in1=g_bc[:],
                                                       op0=ALU.mult, op1=ALU.mult),
         reads=[xb, ssb, gb], writes=[hbb])
    pT, pTb = rings["pT"].next()
    for kc in range(KC):
        k.op(k.pe, lambda kc=kc: nc.tensor.transpose(out=pT[:, kc * 128:(kc + 1) * 128],
                                                     in_=hb[:, kc * 128:(kc + 1) * 128], identity=C.ident[:]),
             reads=[hbb], writes=[pTb])
    k.op(k.act, lambda: nc.scalar.copy(out=hT[:, :, t * 128:(t + 1) * 128],
                                       in_=pT[:].rearrange("p (c n) -> p c n", c=KC)),
         reads=[pTb], writes=[hTb_tile])


def phase_A1(k, cfg, C, hT, hTb, layer):
    nc = k.nc
    with Phase(k, "A1") as ph:
        g_bc = ph.sb([128, D], F32, "g")
        gb = Buf("g")
        k.dma(k.sp, g_bc[:], C.g_attn[layer], writes=[gb])
        rings = norm_rings(ph)
        xr = Ring(ph, 3, [128, D], F32, "x")
        for t in range(cfg.T):
            xt, xb = xr.next()
            k.dma(k.sp, xt[:], C.x_in[t * 128:(t + 1) * 128, :], writes=[xb])
            norm_tile(k, C, xt, xb, g_bc, gb, hT, hTb[t], t, rings)


def phase_A2(k, cfg, C, hT, hTb, layer):
    nc = k.nc
    S, T, TB = cfg.S, cfg.T, cfg.TB
    with Phase(k, "A2") as ph:
        W = C.w_in_even[layer // 2] if layer % 2 == 0 else C.w_in_odd[layer // 2]
        Wv = W.rearrange("(c p) n -> p c n", p=128)
        if layer % 2 == 0:
            blocks = [(0, 512, "fm", ("Q", 0, 0.125)), (512, 512, "fm", ("K", 0, 1.0)),
                      (1024, 512, "tm", ("V", 0)),
                      (1536, 512, "fm", ("Q", 4, 0.125)), (2048, 512, "fm", ("K", 4, 1.0)),
                      (2560, 512, "tm", ("V", 4)),
                      (3072, 512, "fm", ("QI", 0, 1.0)), (3584, 512, "fm", ("QI", 4, 1.0)),
                      (4096, 80, "tm", ("KW", 0))]
        else:
            blocks = [(0, 512, "fm", ("Q", 0, 0.125)), (512, 512, "fm", ("Q", 4, 0.125)),
                      (1024, 512, "fm", ("K", 0, 1.0)), (1536, 512, "fm", ("K", 4, 1.0)),
                      (2048, 512, "tm", ("V", 0)), (2560, 512, "tm", ("V", 4))]
        wst = Ring(ph, 2, [128, KC, 512], F32, "wst")
        wbf = Ring(ph, 2, [128, KC, 512], BF16, "wbf")
        pacc = Ring(ph, 4, [128, 512], F32, "pacc", psum=True)
        ostg = Ring(ph, 2, [128, S], BF16, "ostg")
        vstg = Ring(ph, 3, [128, 512], BF16, "vstg")
        kwstg = Ring(ph, 3, [128, 80], F32, "kwstg")
        flip = 0
        for (c0, ncol, kind, dest) in blocks:
            ws, wsb = wst.next()
            k.dma(k.sp, ws[:, :, 0:ncol], Wv[:, :, c0:c0 + ncol], writes=[wsb])
            wb, wbb = wbf.next()
            k.op(k.pool, lambda: nc.gpsimd.tensor_copy(out=wb[:, :, 0:ncol], in_=ws[:, :, 0:ncol]),
                 reads=[wsb], writes=[wbb])
            if kind == "fm":
                name, cbase, scale = dest
                dst = {"Q": C.QT_d, "K": C.KT_d, "QI": C.QI_d}[name]
                for ci in range(ncol // 128):
                    og, ogb = ostg.next()
                    for tb in range(TB):
                        pa, pab = pacc.next()
                        for kc in range(KC):
                            k.op(k.pe, lambda kc=kc: nc.tensor.matmul(
                                pa[:], lhsT=wb[:, kc, ci * 128:(ci + 1) * 128],
                                rhs=hT[:, kc, tb * 512:(tb + 1) * 512],
                                start=(kc == 0), stop=(kc == KC - 1)),
                                reads=[wbb] + hTb[tb * 4:(tb + 1) * 4], writes=[pab])
                        flip ^= 1
                        if flip:
                            k.op(k.act, lambda: nc.scalar.mul(out=og[:, tb * 512:(tb + 1) * 512], in_=pa[:],
                                                              mul=scale), reads=[pab], writes=[ogb])
                        else:
                            k.op(k.dve, lambda: nc.vector.tensor_scalar(
                                out=og[:, tb * 512:(tb + 1) * 512], in0=pa[:], scalar1=scale, scalar2=None,
                                op0=ALU.mult), reads=[pab], writes=[ogb])
                    k.dma(k.pool, dst[cbase + ci], og[:], reads=[ogb])
            else:
                name, cbase = dest
                for t in range(T):
                    pa, pab = pacc.next()
                    for kc in range(KC):
                        k.op(k.pe, lambda kc=kc: nc.tensor.matmul(
                            pa[:, 0:ncol], lhsT=hT[:, kc, t * 128:(t + 1) * 128], rhs=wb[:, kc, 0:ncol],
                            start=(kc == 0), stop=(kc == KC - 1)),
                            reads=[wbb, hTb[t]], writes=[pab])
                    flip ^= 1
                    if name == "V":
                        vs, vsb = vstg.next()
                        if flip:
                            k.op(k.act, lambda: nc.scalar.copy(out=vs[:], in_=pa[:]), reads=[pab], writes=[vsb])
                        else:
                            k.op(k.dve, lambda: nc.vector.tensor_copy(out=vs[:], in_=pa[:]),
                                 reads=[pab], writes=[vsb])
                        k.dma(k.pool, C.V_d[cbase:cbase + 4, :, t, :].rearrange("c p n -> p c n"),
                              vs[:].rearrange("p (c n) -> p c n", c=4), reads=[vsb])
                    else:
                        vs, vsb = kwstg.next()
                        k.op(k.dve, lambda: nc.vector.tensor_copy(out=vs[:], in_=pa[:, 0:80]),
                             reads=[pab], writes=[vsb])
                        k.dma(k.pool, C.KW_d[:, t, :], vs[:], reads=[vsb])


def phase_tables(k, cfg, C):
    nc = k.nc
    with Phase(k, "T") as ph:
        negc = ph.sb([128, 16], F32, "negc")
        negb = Buf()
        k.op(k.dve, lambda: nc.vector.tensor_scalar(out=negc[:], in0=C.c31[:], scalar1=-1.0, scalar2=None,
                                                    op0=ALU.mult), reads=[C.c31b], writes=[negb])
        caus = ph.sb([128, 1024], F32, "caus")
        cdil = ph.sb([128, 1024], F32, "cdil")
        cb_, db_ = Buf(), Buf()
        k.dma(k.sp, caus[:], C.caus_in[:, :], writes=[cb_])
        k.dma(k.sp, cdil[:], C.cdil_in[:, :], writes=[db_])
        gr = Ring(ph, 2, [128, 1024], F32, "g")
        er = Ring(ph, 2, [128, 1024], F32, "e")
        mnr = Ring(ph, 2, [128, 1024], BF16, "mn")
        mdr = Ring(ph, 2, [128, 1024], BF16, "md")
        for h in range(16):
            g, gb = gr.next()
            k.dma(k.sp, g[:], C.G_in[h], writes=[gb])
            e, eb = er.next()
            k.op(k.act, lambda: nc.scalar.activation(out=e[:], in_=g[:], func=AF.Exp, bias=negc[:, h:h + 1],
                                                     scale=1.0), reads=[gb, negb], writes=[eb])
            mn, mnb = mnr.next()
            k.op(k.dve, lambda: nc.vector.tensor_tensor(out=mn[:], in0=e[:], in1=caus[:], op=ALU.mult),
                 reads=[eb, cb_], writes=[mnb])
            md, mdb = mdr.next()
            k.op(k.dve, lambda: nc.vector.tensor_tensor(out=md[:], in0=e[:], in1=cdil[:], op=ALU.mult),
                 reads=[eb, db_], writes=[mdb])
            k.dma(k.pool, C.Mnear_d[h], mn[:], reads=[mnb])
            k.dma(k.pool, C.Mdil_d[h], md[:], reads=[mdb])


def moba_prepass(k, cfg, C):
    nc = k.nc
    S, T = cfg.S, cfg.T
    NB = S // 256
    with Phase(k, "G") as ph:
        pm = ph.sb([128, 16, 16], F32, "pm")
        o3 = ph.sb([128, 16, 16], F32, "o3")
        pmb, o3b = Buf(), Buf()
        k.dma(k.sp, pm[:], C.pastmask_in[:, :, :], writes=[pmb])
        k.dma(k.sp, o3[:], C.own30k_in[:, :, :], writes=[o3b])
        qr = Ring(ph, 2, [128, S], BF16, "q")
        kr = Ring(ph, 2, [128, S], BF16, "k")
        ksr = Ring(ph, 2, [128, 16], F32, "ks")
        kmr = Ring(ph, 2, [128, 16], BF16, "km")
        qmr = Ring(ph, 2, [128, S], BF16, "qm")
        tpr = Ring(ph, 2, [128, 512], F32, "tp", psum=True)
        gpr = [Ring(ph, 2, [128, 512], F32, "gpa", psum=True), Ring(ph, 2, [128, 512], F32, "gpb", psum=True)]
        gmr = Ring(ph, 3, [128, 2, 16], F32, "gm")
        t8r = Ring(ph, 3, [128, 2, 8], F32, "t8")
        thr = Ring(ph, 3, [128, 2], F32, "th")
        t1r = Ring(ph, 3, [128, 2, 16], F32, "t1")
        mvr = Ring(ph, 3, [128, 128], BF16, "mv")
        for i in range(3):
            k.op(k.dve, lambda i=i: nc.vector.memset(mvr.t[i][:], 0.0), writes=[mvr.b[i]])
        for c in range(4):
            qt, qb = qr.next()
            kt, kb = kr.next()
            k.dma(k.sp, qt[:], C.QT_d[c], writes=[qb])
            k.dma(k.sp, kt[:], C.KT_d[c], writes=[kb])
            ks, ksb = ksr.next()
            km, kmb = kmr.next()
            k.op(k.dve, lambda: nc.vector.memset(ks[:], 0.0), writes=[ksb])
            k.op(k.dve, lambda: nc.vector.tensor_reduce(out=ks[:, 0:NB], in_=kt[:].rearrange("p (n b) -> p n b", b=256),
                                                        axis=AX.X, op=ALU.add), reads=[kb], writes=[ksb])
            k.op(k.dve, lambda: nc.vector.tensor_scalar(out=km[:], in0=ks[:], scalar1=1.0 / 256, scalar2=None,
                                                        op0=ALU.mult), reads=[ksb], writes=[kmb])
            qm, qmb = qmr.next()
            LV = getattr(cfg, "lv", 9)
            for t in range(T):
                if LV < 1:
                    break
                own = t // 2
                gps = [gpr[0].next(), gpr[1].next()]
                for hh in range(2):
                    k.op(k.pe, lambda hh=hh: nc.tensor.matmul(
                        gps[hh][0][:, 0:16], lhsT=qt[hh * 64:(hh + 1) * 64, t * 128:(t + 1) * 128],
                        rhs=km[hh * 64:(hh + 1) * 64, :], start=True, stop=True),
                        reads=[qb, kmb], writes=[gps[hh][1]])
                if LV < 2:
                    continue
                gm, gmb = gmr.next()
                t8, t8b = t8r.next()
                th, thb = thr.next()
                t1, t1b = t1r.next()
                mv, mvb = mvr.next()
                for hh in range(2):
                    k.op(k.dve, lambda hh=hh: nc.vector.tensor_tensor(
                        out=gm[:, hh, :], in0=gps[hh][0][:, 0:16], in1=pm[:, own, :], op=ALU.add),
                        reads=[gps[hh][1], pmb], writes=[gmb])
                for hh in range(2):
                    k.op(k.dve, lambda hh=hh: nc.vector.max(out=t8[:, hh, :], in_=gm[:, hh, :]),
                         reads=[gmb], writes=[t8b])
                k.op(k.dve, lambda: nc.vector.tensor_scalar(out=th[:], in0=t8[:, :, 2], scalar1=-1e29, scalar2=None,
                                                            op0=ALU.max), reads=[t8b], writes=[thb])
                for hh in range(2):
                    k.op(k.dve, lambda hh=hh: nc.vector.tensor_scalar(
                        out=t1[:, hh, :], in0=gm[:, hh, :], scalar1=th[:, hh:hh + 1], scalar2=1.0,
                        op0=ALU.is_ge, op1=ALU.subtract), reads=[gmb, thb], writes=[t1b])
                for hh in range(2):
                    k.op(k.dve, lambda hh=hh: nc.vector.scalar_tensor_tensor(
                        out=mv[:, 64 * hh:64 * hh + 16], in0=t1[:, hh, :], scalar=30000.0, in1=o3[:, own, :],
                        op0=ALU.mult, op1=ALU.add), reads=[t1b, o3b], writes=[mvb])
                if LV < 3:
                    continue
                tp, tpb = tpr.next()
                k.op(k.pe, lambda: nc.tensor.matmul(tp[:, 0:128], lhsT=mv[:], rhs=C.ident[:],
                                                    start=True, stop=True), reads=[mvb], writes=[tpb])
                if LV == 3:
                    continue
                k.op(k.act, lambda: nc.scalar.copy(out=qm[:, t * 128:(t + 1) * 128], in_=tp[:, 0:128]),
                     reads=[tpb], writes=[qmb])
            if LV >= 4:
                k.dma(k.pool, C.QM_d[c], qm[:], reads=[qmb])


def dsa_prepass(k, cfg, C, layer):
    nc = k.nc
    S, T, TB = cfg.S, cfg.T, cfg.TB
    KEEP = min(256, S // 4)
    with Phase(k, "I") as ph:
        kiT = ph.sb([128, S], BF16, "kiT")
        kiTb = [Buf() for _ in range(T)]
        kw = ph.sb([128, T, 80], F32, "kw")
        kwb = Buf()
        k.dma(k.sp, kw[:], C.KW_d[:, :, :], writes=[kwb])
        gk = ph.sb([128, 64], F32, "gk")
        gkb = Buf()
        k.dma(k.sp, gk[:], C.gk_in[layer // 2], writes=[gkb])
        tri = ph.sb([128, 128], F32, "tri")
        trib = Buf()
        k.dma(k.sp, tri[:], C.tri_in[:, :], writes=[trib])
        pw2 = ph.sb([128, NIT], F32, "pw2")
        pw2b = Buf()
        k.dma(k.sp, pw2[:], C.pow2_in[:, :], writes=[pw2b])
        wabs = ph.sb([128, T, 16], F32, "wabs")
        wsgn = ph.sb([128, T, 16], F32, "wsgn")
        wab, wsb_ = Buf(), Buf()
        k.op(k.act, lambda: nc.scalar.activation(out=wabs[:], in_=kw[:, :, 64:80], func=AF.Abs, scale=1.0 / 32),
             reads=[kwb], writes=[wab])
        k.op(k.act, lambda: nc.scalar.activation(out=wsgn[:], in_=kw[:, :, 64:80], func=AF.Sign),
             reads=[kwb], writes=[wsb_])
        ssr = Ring(ph, 4, [128, 4], F32, "ss")
        jkr = Ring(ph, 1, [128, 64], F32, "jk")
        kkr = Ring(ph, 2, [128, 128], BF16, "kk")
        tpr = Ring(ph, 2, [128, 1024], BF16, "tp", psum=True)
        for t in range(T):
            ss, ssb = ssr.next()
            jk, jkb = jkr.next()
            rstd_chain(k, kw[:, t, 0:64], kwb, ss, ssb, jk[:], jkb, 64)
            kk, kkb = kkr.next()
            k.op(k.dve, lambda: nc.vector.scalar_tensor_tensor(out=kk[:, 0:64], in0=kw[:, t, 0:64], scalar=ss[:, 3:4],
                                                               in1=gk[:], op0=ALU.mult, op1=ALU.mult),
                 reads=[kwb, ssb, gkb], writes=[kkb])
            k.op(k.dve, lambda: nc.vector.tensor_copy(out=kk[:, 64:128], in_=kk[:, 0:64]), reads=[kkb], writes=[kkb])
            tp, tpb = tpr.next()
            k.op(k.pe, lambda: nc.tensor.transpose(out=tp[:, 0:128], in_=kk[:], identity=C.ident[:]),
                 reads=[kkb], writes=[tpb])
            k.op(k.act, lambda: nc.scalar.copy(out=kiT[:, t * 128:(t + 1) * 128], in_=tp[:, 0:128]),
                 reads=[tpb], writes=[kiTb[t]])
        qir = Ring(ph, 2, [128, 8, 128], BF16, "qi")
        dsr = Ring(ph, 2, [128, 16, 128], BF16, "ds")
        spr = Ring(ph, 4, [128, 512], F32, "sp", psum=True)
        apr = Ring(ph, 2, [128, 512], F32, "ap", psum=True)
        rr = Ring(ph, 4, [128, 512], BF16, "r")
        scr = Ring(ph, 2, [128, S], F32, "sc")
        cjr = Ring(ph, 1, [128, S], BF16, "cj")
        m01r = Ring(ph, 2, [128, S], BF16, "m01")
        mtsr = Ring(ph, 2, [128, T, 128], BF16, "mts")
        smr = Ring(ph, 2, [128, 8], F32, "sm")
        hwr = Ring(ph, 2, [128, NIT], F32, "hw")
        for i in range(mtsr.n):
            k.op(k.pool, lambda i=i: nc.gpsimd.memset(mtsr.t[i][:], 0.0), writes=[mtsr.b[i]])
        ilag = Lag(3)
        for t in range(T):
            L = (t + 1) * 128
            qi, qib = qir.next()
            k.dma(k.sp, qi[:], C.QI_d[:, :, t * 128:(t + 1) * 128].rearrange("c p n -> p c n"), writes=[qib])
            ds, dsb = dsr.next()
            for h in range(16):
                k.op(k.pool, lambda h=h: nc.gpsimd.tensor_scalar(out=ds[:, h, :], in0=C.ident[:],
                                                                 scalar1=wsgn[:, t, h:h + 1], scalar2=None,
                                                                 op0=ALU.mult), reads=[wsb_], writes=[dsb])
            sc, scb = scr.next()
            nkb = (L + 511) // 512
            for kb in range(nkb):
                kw_ = min(512, L - kb * 512)
                ap_, apb = apr.next()
                for h in range(16):
                    c, hh = h // 2, h % 2
                    sp_, spb = spr.next()
                    k.op(k.pe, lambda: nc.tensor.matmul(
                        sp_[:, 0:kw_], lhsT=qi[hh * 64:(hh + 1) * 64, c, :],
                        rhs=kiT[hh * 64:(hh + 1) * 64, kb * 512:kb * 512 + kw_], start=True, stop=True),
                        reads=[qib] + kiTb[kb * 4:kb * 4 + (kw_ // 128)], writes=[spb])
                    r, rb = rr.next()
                    k.op(k.act, lambda: nc.scalar.activation(out=r[:, 0:kw_], in_=sp_[:, 0:kw_], func=AF.Relu,
                                                             scale=wabs[:, t, h:h + 1]),
                         reads=[spb, wab], writes=[rb])

                    def sgn(ap_=ap_, apb=apb, ds=ds, dsb=dsb, r=r, rb=rb, h=h, kw_=kw_):
                        k.op(k.pe, lambda: nc.tensor.matmul(ap_[:, 0:kw_], lhsT=ds[:, h, :], rhs=r[:, 0:kw_],
                                                            start=(h == 0), stop=(h == 15)),
                             reads=[dsb, rb], writes=[apb])
                    ilag.push(sgn)

                def cp(sc=sc, scb=scb, ap_=ap_, apb=apb, kb=kb, kw_=kw_):
                    k.op(k.dve, lambda: nc.vector.tensor_copy(out=sc[:, kb * 512:kb * 512 + kw_], in_=ap_[:, 0:kw_]),
                         reads=[apb], writes=[scb])
                ilag.push(cp)
            ilag.flush()
            sm, smb = smr.next()
            hw, hwb = hwr.next()
            k.op(k.dve, lambda: nc.vector.tensor_reduce(out=sm[:, 0:1], in_=sc[:, 0:L], axis=AX.X, op=ALU.max,
                                                        apply_absolute_value=True), reads=[scb], writes=[smb])
            k.op(k.dve, lambda: nc.vector.tensor_tensor(out=sc[:, L - 128:L], in0=sc[:, L - 128:L], in1=tri[:],
                                                        op=ALU.add), reads=[scb, trib], writes=[scb])
            k.op(k.dve, lambda: nc.vector.tensor_scalar(out=sm[:, 1:2], in0=sm[:, 0:1], scalar1=-1.0, scalar2=None,
                                                        op0=ALU.mult), reads=[smb], writes=[smb])
            k.op(k.dve, lambda: nc.vector.tensor_scalar(out=hw[:], in0=pw2[:], scalar1=sm[:, 0:1], scalar2=None,
                                                        op0=ALU.mult), reads=[smb, pw2b], writes=[hwb])
            cj, cjb = cjr.next()
            for it in range(NIT):
                k.op(k.dve, lambda: nc.vector.tensor_tensor(out=sm[:, 2:3], in0=sm[:, 1:2], in1=hw[:, it:it + 1],
                                                            op=ALU.add), reads=[smb, hwb], writes=[smb])
                k.op(k.dve, lambda: nc.vector.tensor_scalar(out=cj[:, 0:L], in0=sc[:, 0:L], scalar1=sm[:, 2:3],
                                                            scalar2=0.0, op0=ALU.is_ge, op1=ALU.add,
                                                            accum_out=sm[:, 3:4]),
                     reads=[scb, smb], writes=[cjb, smb])
                k.op(k.dve, lambda: nc.vector.scalar_tensor_tensor(out=sm[:, 4:5], in0=sm[:, 3:4],
                                                                   scalar=KEEP - 0.5, in1=hw[:, it:it + 1],
                                                                   op0=ALU.is_ge, op1=ALU.mult),
                     reads=[smb, hwb], writes=[smb])
                k.op(k.dve, lambda: nc.vector.tensor_tensor(out=sm[:, 1:2], in0=sm[:, 1:2], in1=sm[:, 4:5],
                                                            op=ALU.add), reads=[smb], writes=[smb])
            m01, m01b = m01r.next()
            k.op(k.dve, lambda: nc.vector.tensor_scalar(out=m01[:, 0:L], in0=sc[:, 0:L], scalar1=sm[:, 1:2],
                                                        scalar2=None, op0=ALU.is_ge), reads=[scb, smb], writes=[m01b])
            mts, mtsb = mtsr.next()
            for j0 in range(0, t + 1, 8):
                nj = min(8, t + 1 - j0)
                tp, tpb = tpr.next()
                for jj in range(nj):
                    k.op(k.pe, lambda jj=jj: nc.tensor.transpose(
                        out=tp[:, jj * 128:(jj + 1) * 128], in_=m01[:, (j0 + jj) * 128:(j0 + jj + 1) * 128],
                        identity=C.ident[:]), reads=[m01b], writes=[tpb])
                k.op(k.act, lambda: nc.scalar.copy(out=mts[:, j0:j0 + nj, :],
                                                   in_=tp[:, 0:nj * 128].rearrange("p (j n) -> p j n", n=128)),
                     reads=[tpb], writes=[mtsb])
            i_q, sub = t // 4, t % 4
            nkt = 4 * i_q + 4
            k.dma(k.pool, C.MK_d[i_q, :, 0:nkt, sub * 128:(sub + 1) * 128], mts[:, 0:nkt, :], reads=[mtsb])


def attention(k, cfg, C, mode, pairs, tag):
    nc = k.nc
    S, T, TB = cfg.S, cfg.T, cfg.TB
    with Phase(k, "B" + tag) as ph:
        qr = Ring(ph, 2, [128, S], BF16, "q")
        kr = Ring(ph, 2, [128, S], BF16, "k")
        ver = Ring(ph, 2, [128, T, 128], BF16, "ve")
        vor = Ring(ph, 2, [128, T, 128], BF16, "vo")
        for i in range(2):
            k.op(k.pool, lambda i=i: nc.gpsimd.memset(ver.t[i][:], 0.0), writes=[ver.b[i]])
            k.op(k.pool, lambda i=i: nc.gpsimd.memset(vor.t[i][:], 0.0), writes=[vor.b[i]])
        mnr = Ring(ph, 2, [128, 2, 1024], BF16, "mn")
        ptr = Ring(ph, 6, [128, 512], BF16, "pt")
        mor = Ring(ph, 2, [128, S], BF16, "mo")
        recr = Ring(ph, 2, [128, 512], F32, "rec")
        psr = Ring(ph, 4, [128, 512], F32, "ps", psum=True)
        por = Ring(ph, 2, [128, 512], F32, "po", psum=True)
        pdr = Ring(ph, 2, [128, 512], F32, "pd", psum=True)
        Mx_d = C.Mdil_d if mode == "dil" else C.Mnear_d
        if mode == "moba":
            oh = ph.sb([128, S], BF16, "oh")
            ohb = Buf()
            k.dma(k.sp, oh[:], C.onehot_in[:, :], writes=[ohb])
            qmr = Ring(ph, 2, [128, S], BF16, "qm")
        if mode == "dsa":
            mkr = Ring(ph, 2, [128, 16, 512], BF16, "mk")
        if mode == "dil":
            cf = ph.sb([128, 2432], BF16, "cf")
            cfb = Buf()
            k.dma(k.sp, cf[:], C.cfar_in[:, :], writes=[cfb])
        lag = Lag(3)
        tile_no = [0]
        for c in pairs:
            qt, qb = qr.next()
            kt, kb = kr.next()
            ve, veb = ver.next()
            vo, vob = vor.next()
            mn, mnb = mnr.next()
            k.dma(k.sp, qt[:], C.QT_d[c], writes=[qb])
            k.dma(k.sp, kt[:], C.KT_d[c], writes=[kb])
            k.dma(k.sp, ve[:, :, 0:64], C.V_d[c, :, :, 0:64], writes=[veb])
            k.dma(k.sp, vo[:, :, 64:128], C.V_d[c, :, :, 64:128], writes=[vob])
            k.dma(k.sp, mn[:], Mx_d[2 * c:2 * c + 2].rearrange("h p n -> p h n"), writes=[mnb])
            qm = qmb = None
            if mode == "moba":
                qm, qmb = qmr.next()
                k.dma(k.sp, qm[:], C.QM_d[c], writes=[qmb])
            mo, mob = mor.next()
            for i in range(TB):
                q0 = i * 512
                jhi = 4 * i + 3
                jlo = max(0, 4 * i - 16) if mode == "dil" else 0
                po, pob = por.next()
                pd, pdb = pdr.next()
                seq = [(j, hh) for j in range(jlo, jhi + 1) for hh in range(2)]
                mk = mkb = None
                for idx, (j, hh) in enumerate(seq):
                    if mode == "dsa" and hh == 0 and (j % 16 == 0):
                        mk, mkb = mkr.next()
                        g1 = min(jhi + 1, j + 16)
                        k.dma(k.sp, mk[:, 0:g1 - j, :], C.MK_d[i, :, j:g1, :], writes=[mkb])
                    rel = q0 - 128 * j
                    near = rel <= 128
                    hd = 2 * c + hh
                    ps, psb = psr.next()
                    k.op(k.pe, lambda: nc.tensor.matmul(
                        ps[:], lhsT=kt[hh * 64:(hh + 1) * 64, j * 128:(j + 1) * 128],
                        rhs=qt[hh * 64:(hh + 1) * 64, q0:q0 + 512], start=True, stop=(mode != "moba")),
                        reads=[kb, qb], writes=[psb])
                    if mode == "moba":
                        k.op(k.pe, lambda: nc.tensor.matmul(
                            ps[:], lhsT=oh[64 * hh:64 * hh + 16, j * 128:(j + 1) * 128],
                            rhs=qm[64 * hh:64 * hh + 16, q0:q0 + 512],
                            start=False, stop=True), reads=[ohb, qmb], writes=[psb])
                    pt, ptb = ptr.next()
                    k.op(k.act, lambda: nc.scalar.activation(out=pt[:], in_=ps[:], func=AF.Exp,
                                                             bias=C.c31[:, hd:hd + 1], scale=1.0),
                         reads=[psb, C.c31b], writes=[ptb])
                    tile_no[0] += 1
                    if tile_no[0] % 2 == 0:
                        ME, mfn = k.pool, nc.gpsimd.tensor_tensor
                    else:
                        ME, mfn = k.dve, nc.vector.tensor_tensor
                    if near:
                        o = rel + 384
                        k.op(ME, lambda: mfn(out=pt[:], in0=pt[:], in1=mn[:, hh, o:o + 512], op=ALU.mult),
                             reads=[ptb, mnb], writes=[ptb])
                    elif mode == "dil":
                        o = rel - 129
                        k.op(ME, lambda: mfn(out=pt[:], in0=pt[:], in1=cf[:, o:o + 512], op=ALU.mult),
                             reads=[ptb, cfb], writes=[ptb])
                    if mode == "dsa":
                        jj = j % 16
                        k.op(ME, lambda: mfn(out=pt[:], in0=pt[:], in1=mk[:, jj, :], op=ALU.mult),
                             reads=[ptb, mkb], writes=[ptb])
                    first, last = idx == 0, idx == len(seq) - 1

                    def pv(pt=pt, ptb=ptb, j=j, hh=hh, first=first, last=last, po=po, pob=pob, pd=pd, pdb=pdb,
                           ve=ve, veb=veb, vo=vo, vob=vob):
                        vv, vvb = (ve, veb) if hh == 0 else (vo, vob)
                        k.op(k.pe, lambda: nc.tensor.matmul(po[:], lhsT=vv[:, j, :], rhs=pt[:], start=first,
                                                            stop=last), reads=[vvb, ptb], writes=[pob])
                        on = C.onesE if hh == 0 else C.onesO
                        k.op(k.pe, lambda: nc.tensor.matmul(pd[:], lhsT=on, rhs=pt[:], start=first, stop=last),
                             reads=[ptb, C.onesb], writes=[pdb])
                    lag.push(pv)

                def fin(po=po, pob=pob, pd=pd, pdb=pdb, mo=mo, mob=mob, q0=q0):
                    rec, recb = recr.next()
                    k.op(k.dve, lambda: nc.vector.reciprocal(out=rec[:], in_=pd[:]), reads=[pdb], writes=[recb])
                    k.op(k.dve, lambda: nc.vector.tensor_tensor(out=mo[:, q0:q0 + 512], in0=po[:], in1=rec[:],
                                                                op=ALU.mult), reads=[pob, recb], writes=[mob])
                lag.push(fin)

            def st(mo=mo, mob=mob, c=c):
                k.dma(k.pool, C.MT_d[c], mo[:], reads=[mob])
            lag.push(st)
        lag.flush()


def phase_C(k, cfg, C, hT, hTb, layer):
    nc = k.nc
    S, T = cfg.S, cfg.T
    with Phase(k, "C") as ph:
        wo = ph.sb([128, KC, D], BF16, "wo")
        wob = Buf()
        wst = Ring(ph, 2, [128, KC, 512], F32, "wst")
        Wv = C.w_o[layer].rearrange("(c p) n -> p c n", p=128)
        for nb in range(2):
            ws, wsb = wst.next()
            k.dma(k.sp, ws[:], Wv[:, :, nb * 512:(nb + 1) * 512], writes=[wsb])
            k.op(k.pool, lambda: nc.gpsimd.tensor_copy(out=wo[:, :, nb * 512:(nb + 1) * 512], in_=ws[:]),
                 reads=[wsb], writes=[wob])
        g_bc = ph.sb([128, D], F32, "g")
        gb = Buf()
        k.dma(k.sp, g_bc[:], C.g_ffn[layer], writes=[gb])
        rings = norm_rings(ph)
        mtr = Ring(ph, 2, [128, KC, 128], BF16, "mt")
        xr = Ring(ph, 3, [128, D], F32, "x")
        xmr = Ring(ph, 2, [128, D], F32, "xm")
        pacc = Ring(ph, 4, [128, 512], F32, "pacc", psum=True)
        xsrc = C.x_in if layer == 0 else C.xs
        for t in range(T):
            mt, mtb = mtr.next()
            k.dma(k.sp, mt[:], C.MT_d[:, :, t * 128:(t + 1) * 128].rearrange("c p n -> p c n"), writes=[mtb])
            xt, xb = xr.next()
            k.dma(k.sp, xt[:], xsrc[t * 128:(t + 1) * 128, :], writes=[xb])
            xm, xmb = xmr.next()
            for nb in range(2):
                pa, pab = pacc.next()
                for c in range(KC):
                    k.op(k.pe, lambda c=c: nc.tensor.matmul(pa[:], lhsT=mt[:, c, :],
                                                            rhs=wo[:, c, nb * 512:(nb + 1) * 512],
                                                            start=(c == 0), stop=(c == KC - 1)),
                         reads=[mtb, wob], writes=[pab])
                k.op(k.dve, lambda: nc.vector.tensor_tensor(out=xm[:, nb * 512:(nb + 1) * 512], in0=pa[:],
                                                            in1=xt[:, nb * 512:(nb + 1) * 512], op=ALU.add),
                     reads=[pab, xb], writes=[xmb])
            k.dma(k.pool, C.xs[t * 128:(t + 1) * 128, :], xm[:], reads=[xmb])
            norm_tile(k, C, xm, xmb, g_bc, gb, hT, hTb[t], t, rings)


def phase_F1(k, cfg, C, hT, hTb, layer):
    nc = k.nc
    S, T, TB = cfg.S, cfg.T, cfg.TB
    with Phase(k, "F1") as ph:
        cp = ph.sb([128, 4, 44], F32, "cp")
        cpb = Buf()
        k.dma(k.sp, cp[:], C.convp[layer], writes=[cpb])
        Wv = C.w_up[layer].rearrange("(c p) n -> p c n", p=128)
        wst = Ring(ph, 2, [128, KC, 256], F32, "wst")
        wbf = Ring(ph, 2, [128, KC, 256], BF16, "wbf")
        pacc = Ring(ph, 4, [128, 512], F32, "pacc", psum=True)
        ubr = [Ring(ph, 2, [128, 514], F32, "ubv"), Ring(ph, 2, [128, 514], F32, "ubg")]
        car = Ring(ph, 3, [128, 512], F32, "ca")
        cbr = Ring(ph, 3, [128, 512], F32, "cb")
        sgr = Ring(ph, 2, [128, 512], F32, "sg")
        asr = Ring(ph, 2, [128, S], BF16, "as")
        for f in range(NF):
            ws, wsb = wst.next()
            k.dma(k.sp, ws[:, :, 0:128], Wv[:, :, f * 128:(f + 1) * 128], writes=[wsb])
            k.dma(k.sp, ws[:, :, 128:256], Wv[:, :, DFF + f * 128:DFF + (f + 1) * 128], writes=[wsb])
            wb, wbb = wbf.next()
            k.op(k.pool, lambda: nc.gpsimd.tensor_copy(out=wb[:], in_=ws[:]), reads=[wsb], writes=[wbb])
            a_s, asb = asr.next()
            prev = [None, None]
            for tb in range(TB):
                cv = [None, None]
                for half in range(2):
                    col = f if half == 0 else NF + f
                    pa, pab = pacc.next()
                    for kc in range(KC):
                        k.op(k.pe, lambda kc=kc: nc.tensor.matmul(
                            pa[:], lhsT=wb[:, kc, half * 128:(half + 1) * 128],
                            rhs=hT[:, kc, tb * 512:(tb + 1) * 512], start=(kc == 0), stop=(kc == KC - 1)),
                            reads=[wbb] + hTb[tb * 4:(tb + 1) * 4], writes=[pab])
                    ub, ubb = ubr[half].next()
                    if tb == 0:
                        k.op(k.pool, lambda: nc.gpsimd.memset(ub[:, 0:2], 0.0), writes=[ubb])
                    else:
                        pu, pub = prev[half]
                        k.op(k.pool, lambda: nc.gpsimd.tensor_copy(out=ub[:, 0:2], in_=pu[:, 512:514]),
                             reads=[pub], writes=[ubb])
                    k.op(k.act, lambda: nc.scalar.copy(out=ub[:, 2:514], in_=pa[:]), reads=[pab], writes=[ubb])
                    prev[half] = (ub, ubb)
                    r = car if half == 0 else cbr
                    c1, c1b = r.next()
                    k.op(k.dve, lambda: nc.vector.tensor_scalar(out=c1[:], in0=ub[:, 2:514],
                                                                scalar1=cp[:, 2, col:col + 1],
                                                                scalar2=cp[:, 3, col:col + 1],
                                                                op0=ALU.mult, op1=ALU.add),
                         reads=[ubb, cpb], writes=[c1b])
                    c2, c2b = r.next()
                    k.op(k.dve, lambda: nc.vector.scalar_tensor_tensor(out=c2[:], in0=ub[:, 1:513],
                                                                       scalar=cp[:, 1, col:col + 1], in1=c1[:],
                                                                       op0=ALU.mult, op1=ALU.add),
                         reads=[ubb, cpb, c1b], writes=[c2b])
                    c3, c3b = r.next()
                    k.op(k.dve, lambda: nc.vector.scalar_tensor_tensor(out=c3[:], in0=ub[:, 0:512],
                                                                       scalar=cp[:, 0, col:col + 1], in1=c2[:],
                                                                       op0=ALU.mult, op1=ALU.add),
                         reads=[ubb, cpb, c2b], writes=[c3b])
                    cv[half] = (c3, c3b)
                sg, sgb = sgr.next()
                k.op(k.act, lambda: nc.scalar.activation(out=sg[:], in_=cv[1][0][:], func=AF.Silu),
                     reads=[cv[1][1]], writes=[sgb])
                k.op(k.dve, lambda: nc.vector.tensor_tensor(out=a_s[:, tb * 512:(tb + 1) * 512], in0=sg[:],
                                                            in1=cv[0][0][:], op=ALU.mult),
                     reads=[sgb, cv[0][1]], writes=[asb])
            k.dma(k.pool, C.AT_d[f], a_s[:], reads=[asb])


def phase_F2(k, cfg, C, hT, hTb, layer, last):
    nc = k.nc
    S, T = cfg.S, cfg.T
    with Phase(k, "F2") as ph:
        wd = ph.sb([128, NF, D], BF16, "wd")
        wdb = Buf()
        wst = Ring(ph, 2, [128, NF, 128], F32, "wst")
        Wv = C.w_down[layer].rearrange("(f p) n -> p f n", p=128)
        for cb in range(8):
            ws, wsb = wst.next()
            k.dma(k.sp, ws[:], Wv[:, :, cb * 128:(cb + 1) * 128], writes=[wsb])
            k.op(k.pool, lambda: nc.gpsimd.tensor_copy(out=wd[:, :, cb * 128:(cb + 1) * 128], in_=ws[:]),
                 reads=[wsb], writes=[wdb])
        g_bc = ph.sb([128, D], F32, "g")
        gb = Buf()
        k.dma(k.sp, g_bc[:], C.g_fin[:, :] if last else C.g_attn[layer + 1], writes=[gb])
        rings = norm_rings(ph)
        atr = Ring(ph, 2, [128, NF, 128], BF16, "at")
        xr = Ring(ph, 3, [128, D], F32, "x")
        xnr = Ring(ph, 2, [128, D], F32, "xn")
        yor = Ring(ph, 2, [128, D], F32, "yo")
        pacc = Ring(ph, 4, [128, 512], F32, "pacc", psum=True)
        for t in range(T):
            at, atb = atr.next()
            k.dma(k.sp, at[:], C.AT_d[:, :, t * 128:(t + 1) * 128].rearrange("f p n -> p f n"), writes=[atb])
            xt, xb = xr.next()
            k.dma(k.sp, xt[:], C.xs[t * 128:(t + 1) * 128, :], writes=[xb])
            xn, xnb = xnr.next()
            for nb in range(2):
                pa, pab = pacc.next()
                for f in range(NF):
                    k.op(k.pe, lambda f=f: nc.tensor.matmul(pa[:], lhsT=at[:, f, :],
                                                            rhs=wd[:, f, nb * 512:(nb + 1) * 512],
                                                            start=(f == 0), stop=(f == NF - 1)),
                         reads=[atb, wdb], writes=[pab])
                k.op(k.dve, lambda: nc.vector.tensor_tensor(out=xn[:, nb * 512:(nb + 1) * 512], in0=pa[:],
                                                            in1=xt[:, nb * 512:(nb + 1) * 512], op=ALU.add),
                     reads=[pab, xb], writes=[xnb])
            if last:
                junk, junkb = rings["junk"].next()
                ss, ssb = rings["ss"].next()
                rstd_chain(k, xn[:], xnb, ss, ssb, junk[:], junkb, D)
                yo, yob = yor.next()
                k.op(k.dve, lambda: nc.vector.scalar_tensor_tensor(out=yo[:], in0=xn[:], scalar=ss[:, 3:4],
                                                                   in1=g_bc[:], op0=ALU.mult, op1=ALU.mult),
                     reads=[xnb, ssb, gb], writes=[yob])
                k.dma(k.pool, C.out[t * 128:(t + 1) * 128, :], yo[:], reads=[yob])
            else:
                k.dma(k.pool, C.xs[t * 128:(t + 1) * 128, :], xn[:], reads=[xnb])
                norm_tile(k, C, xn, xnb, g_bc, gb, hT, hTb[t], t, rings)


def build(cfg):
    S, T, TB = cfg.S, cfg.T, cfg.TB
    nc = bass.Bass("TRN2", target_bir_lowering=False)
    k = K(nc)
    C = NS()

    def din(name, shape, dt=F32):
        return nc.dram_tensor(name, list(shape), dt, kind="ExternalInput").ap()

    def dscr(name, shape, dt):
        kind = "ExternalOutput" if (cfg.debug and name in cfg.debug) else "Internal"
        return nc.dram_tensor(name, list(shape), dt, kind=kind).ap()

    C.x_in = din("x", [S, D])
    C.w_in_even = din("w_in_even", [2, D, 4176])
    C.w_in_odd = din("w_in_odd", [2, D, 3072])
    C.w_o = din("w_o", [4, D, D])
    C.w_up = din("w_up", [4, D, 2 * DFF])
    C.w_down = din("w_down", [4, DFF, D])
    C.g_attn = din("g_attn", [4, 128, D])
    C.g_ffn = din("g_ffn", [4, 128, D])
    C.g_fin = din("g_fin", [128, D])
    C.gk_in = din("gk", [2, 128, 64])
    C.convp = din("convp", [4, 128, 4, 44])
    C.G_in = din("G", [16, 128, 1024])
    c31_in = din("c31", [128, 16])
    ident_in = din("ident", [128, 128], BF16)
    ones_in = din("ones2", [128, 2, 128], BF16)
    C.caus_in = din("caus", [128, 1024])
    C.cdil_in = din("cdil", [128, 1024])
    C.cfar_in = din("cfar", [128, 2432], BF16)
    C.onehot_in = din("onehot", [128, S], BF16)
    C.pastmask_in = din("pastmask", [128, 16, 16])
    C.own30k_in = din("own30k", [128, 16, 16])
    C.tri_in = din("tri", [128, 128])
    C.pow2_in = din("pow2", [128, NIT])

    C.QT_d = dscr("QT_d", [8, 128, S], BF16)
    C.KT_d = dscr("KT_d", [8, 128, S], BF16)
    C.V_d = dscr("V_d", [8, 128, T, 128], BF16)
    C.QI_d = dscr("QI_d", [8, 128, S], BF16)
    C.KW_d = dscr("KW_d", [128, T, 80], F32)
    C.QM_d = dscr("QM_d", [4, 128, S], BF16)
    C.MK_d = dscr("MK_d", [TB, 128, T, 512], BF16)
    C.MT_d = dscr("MT_d", [8, 128, S], BF16)
    C.AT_d = dscr("AT_d", [NF, 128, S], BF16)
    C.xs = dscr("xs", [S, D], F32)
    C.Mnear_d = dscr("Mnear_d", [16, 128, 1024], BF16)
    C.Mdil_d = dscr("Mdil_d", [16, 128, 1024], BF16)
    C.out = nc.dram_tensor("out", [S, D], F32, kind="ExternalOutput").ap()

    stop_after = cfg.stop_after

    with ExitStack() as top:
        C.ident = top.enter_context(nc.sbuf_tensor("ident_sb", [128, 128], BF16))
        C.c31 = top.enter_context(nc.sbuf_tensor("c31_sb", [128, 16], F32))
        ones2 = top.enter_context(nc.sbuf_tensor("ones_sb", [128, 2, 128], BF16))
        C.onesE = ones2[:, 0, :]
        C.onesO = ones2[:, 1, :]
        C.c31b, C.onesb, identb = Buf(), Buf(), Buf()
        k.dma(k.sp, C.ident[:], ident_in[:, :], writes=[identb])
        k.dma(k.sp, C.c31[:], c31_in[:, :], writes=[C.c31b])
        k.dma(k.sp, ones2[:], ones_in[:, :, :], writes=[C.onesb])
        k.barrier()
        phase_tables(k, cfg, C)

        def done(tag):
            return stop_after == tag

        fin = False
        for layer in range(cfg.depth):
            if fin:
                break
            if layer == 0:
                with Phase(k, "H0") as hp:
                    hT = hp.sb([128, KC, S], BF16, "hT")
                    hTb = [Buf() for _ in range(T)]
                    phase_A1(k, cfg, C, hT, hTb, 0)
                    phase_A2(k, cfg, C, hT, hTb, 0)
            if done("A%d" % layer):
                break
            if layer % 2 == 0:
                moba_prepass(k, cfg, C)
                if done("G%d" % layer):
                    break
                attention(k, cfg, C, "moba", [0, 1, 2, 3], "m")
                if done("M%d" % layer):
                    break
                dsa_prepass(k, cfg, C, layer)
                attention(k, cfg, C, "dsa", [4, 5, 6, 7], "d")
            else:
                attention(k, cfg, C, "dil", list(range(8)), "l")
            if done("B%d" % layer):
                break
            with Phase(k, "HC") as hp:
                hT = hp.sb([128, KC, S], BF16, "hT")
                hTb = [Buf() for _ in range(T)]
                phase_C(k, cfg, C, hT, hTb, layer)
                if done("C%d" % layer):
                    fin = True
                else:
                    phase_F1(k, cfg, C, hT, hTb, layer)
            if fin or done("F1%d" % layer):
                break
            last = layer == cfg.depth - 1
            with Phase(k, "HF") as hp:
                hT = hp.sb([128, KC, S], BF16, "hT")
                hTb = [Buf() for _ in range(T)]
                phase_F2(k, cfg, C, hT, hTb, layer, last)
                if not last and not done("F2%d" % layer):
                    phase_A2(k, cfg, C, hT, hTb, layer + 1)
            if done("F2%d" % layer):
                break
        k.final_wait()
    k.stack.close()
    return nc


def host_inputs(cfg, b, x, w_in_even, idx_k_norm, w_in_odd, w_o, rel_bias, attn_norm, ffn_norm,
                w_up, conv_w, conv_b, w_down, final_norm):
    S = cfg.S
    bf = ml_dtypes.bfloat16
    m = {}
    m["x"] = np.ascontiguousarray(x[b], dtype=np.float32)
    m["w_in_even"] = w_in_even
    m["w_in_odd"] = w_in_odd
    m["w_o"] = w_o
    m["w_up"] = w_up
    m["w_down"] = w_down
    m["g_attn"] = np.ascontiguousarray(np.broadcast_to(attn_norm[:, None, :], (4, 128, D)))
    m["g_ffn"] = np.ascontiguousarray(np.broadcast_to(ffn_norm[:, None, :], (4, 128, D)))
    m["g_fin"] = np.ascontiguousarray(np.broadcast_to(final_norm[None, :], (128, D)))
    m["gk"] = np.ascontiguousarray(np.broadcast_to(idx_k_norm[:, None, :], (2, 128, 64)))
    cp = np.zeros((4, 128, 4, 44), np.float32)
    for l in range(4):
        for j in range(3):
            cp[l, :, j, :] = conv_w[l, j].reshape(44, 128).T
        cp[l, :, 3, :] = conv_b[l].reshape(44, 128).T
    m["convp"] = cp
    p = np.arange(128)[:, None]
    j = np.arange(1024)[None, :]
    d = j - 384 - p
    bk = t5_bucket_np(d)
    m["G"] = np.ascontiguousarray(np.transpose(rel_bias[bk, :], (2, 0, 1))).astype(np.float32)
    m["c31"] = np.ascontiguousarray(np.broadcast_to(rel_bias[31][None, :], (128, 16))).astype(np.float32)
    m["ident"] = np.eye(128, dtype=np.float32).astype(bf)
    o2 = np.zeros((128, 2, 128), np.float32)
    o2[:, 0, 0:64] = 1.0
    o2[:, 1, 64:128] = 1.0
    m["ones2"] = o2.astype(bf)
    m["caus"] = (d >= 0).astype(np.float32)

    def cmul(dd):
        return (((dd >= 0) & (dd <= 128)).astype(np.float32)
                + ((dd >= 0) & (dd <= 512) & (dd % 4 == 0)).astype(np.float32)
                + ((dd >= 0) & (dd <= 2048) & (dd % 16 == 0)).astype(np.float32))
    m["cdil"] = cmul(d)
    jf = np.arange(2432)[None, :]
    m["cfar"] = cmul(jf - p + 129).astype(bf)
    kk = np.arange(S)[None, :]
    n128 = np.arange(128)[:, None]
    m["onehot"] = ((kk // 256 == (n128 % 64)) & ((n128 % 64) < 16)).astype(np.float32).astype(bf)
    own = np.arange(16)[:, None]
    nn = np.arange(16)[None, :]
    pmk = np.where(nn < own, 0.0, -1e30).astype(np.float32)
    m["pastmask"] = np.ascontiguousarray(np.broadcast_to(pmk[None], (128, 16, 16)))
    o3 = np.where(nn == own, 30000.0, 0.0).astype(np.float32)
    m["own30k"] = np.ascontiguousarray(np.broadcast_to(o3[None], (128, 16, 16)))
    q = np.arange(128)[:, None]
    kx = np.arange(128)[None, :]
    m["tri"] = np.where(kx <= q, 0.0, -1e30).astype(np.float32)
    m["pow2"] = np.ascontiguousarray(np.broadcast_to((2.0 ** -np.arange(NIT))[None, :], (128, NIT))).astype(np.float32)
    return m


_CACHE = {}


def kernel(**inputs):
    inputs = {k_: np.asarray(v) for k_, v in inputs.items()}
    x = inputs["x"]
    B, S, _ = x.shape
    cfg = Cfg(S=S, depth=4)
    if "nc" not in _CACHE:
        _CACHE["nc"] = build(cfg)
    nc = _CACHE["nc"]
    in_maps = []
    for core in range(8):
        in_maps.append(host_inputs(cfg, core // 2, **inputs))
    res = run_bass_kernel_spmd(nc, in_maps, core_ids=list(range(8)))
    outs = [res.results[2 * b]["out"] for b in range(B)]
    return np.stack(outs, axis=0).astype(np.float32)
```

```python
import math
from contextlib import ExitStack

import numpy as np
import ml_dtypes
import concourse.bass as bass
import concourse.mybir as mybir
from concourse.bass_utils import run_bass_kernel_spmd

F32 = mybir.dt.float32
BF16 = mybir.dt.bfloat16
AF = mybir.ActivationFunctionType
ALU = mybir.AluOpType
AX = mybir.AxisListType

D = 1024
KC = 8
DFF = 2816
NF = 22
NEG = -30000.0
SAME_ENGINE_SYNC = True


class Buf:
    __slots__ = ("name", "w", "r")

    def __init__(self, name=""):
        self.name = name
        self.w = {}
        self.r = {}


class Eng:
    def __init__(self, K, name, e, is_pe=False):
        self.K = K
        self.name = name
        self.e = e
        self.is_pe = is_pe
        self.sem = K.new_sem("pg_" + name)
        self.n = 0
        self.waited = {}

    def wait(self, sem, val):
        key = id(sem)
        if self.waited.get(key, 0) >= val:
            return
        self.waited[key] = val
        self.e.wait_ge(sem, val)


class K:
    def __init__(self, nc):
        self.nc = nc
        self.stack = ExitStack()
        self.sems = []
        self.pe = Eng(self, "pe", nc.tensor, is_pe=True)
        self.act = Eng(self, "act", nc.scalar)
        self.dve = Eng(self, "dve", nc.vector)
        self.pool = Eng(self, "pool", nc.gpsimd)
        self.sp = Eng(self, "sp", nc.sync)
        self.engs = [self.pe, self.act, self.dve, self.pool, self.sp]
        self.dq = {}
        for q in (self.sp, self.pool, self.act):
            self.dq[q.name] = dict(sems=[self.new_sem("dq_%s%d" % (q.name, i)) for i in range(8)],
                                   tot=[0] * 8, i=0)

    def new_sem(self, name):
        s = self.stack.enter_context(self.nc.semaphore(name))
        self.sems.append(s)
        return s

    def _deps(self, X, reads, writes, acc=False):
        for b in reads:
            for sem, val in b.w.values():
                if (sem is X.sem) and (X.is_pe or not SAME_ENGINE_SYNC):
                    continue
                X.wait(sem, val)
        for b in writes:
            for sem, val in list(b.w.values()) + list(b.r.values()):
                if (sem is X.sem) and (X.is_pe or not SAME_ENGINE_SYNC):
                    continue
                X.wait(sem, val)

    def _mark(self, ev, reads, writes):
        sem, val = ev
        for b in reads:
            b.r[id(sem)] = ev
        for b in writes:
            b.w = {id(sem): ev}
            b.r = {}

    def op(self, X, ins_fn, reads=(), writes=()):
        self._deps(X, reads, writes)
        ins = ins_fn()
        ins.then_inc(X.sem, 1)
        X.n += 1
        self._mark((X.sem, X.n), reads, writes)
        return ins

    def dma(self, Q, out, in_, reads=(), writes=(), **kw):
        self._deps(Q, reads, writes)
        dq = self.dq[Q.name]
        i = dq["i"]
        dq["i"] = (i + 1) % len(dq["sems"])
        sem = dq["sems"][i]
        if dq["tot"][i] > 0:
            Q.wait(sem, dq["tot"][i])
        Q.e.dma_start(out=out, in_=in_, **kw).then_inc(sem, 16)
        dq["tot"][i] += 16
        self._mark((sem, dq["tot"][i]), reads, writes)

    def barrier(self):
        for X in self.engs:
            for Y in self.engs:
                if Y is not X and Y.n > 0:
                    X.wait(Y.sem, Y.n)
            for dq in self.dq.values():
                for sem, tot in zip(dq["sems"], dq["tot"]):
                    if tot > 0:
                        X.wait(sem, tot)

    def final_wait(self):
        X = self.sp
        for Y in self.engs:
            if Y is not X and Y.n > 0:
                X.wait(Y.sem, Y.n)
        for dq in self.dq.values():
            for sem, tot in zip(dq["sems"], dq["tot"]):
                if tot > 0:
                    X.wait(sem, tot)


class Phase:
    _uid = [0]

    def __init__(self, k, name):
        self.k = k
        Phase._uid[0] += 1
        self.name = "%s%d" % (name, Phase._uid[0])
        self.stack = ExitStack()
        self.cnt = 0

    def __enter__(self):
        return self

    def __exit__(self, *a):
        self.k.barrier()
        self.stack.close()
        return False

    def sb(self, shape, dt, name=None):
        self.cnt += 1
        t = self.stack.enter_context(
            self.k.nc.sbuf_tensor("%s_%s%d" % (self.name, name or "t", self.cnt), list(shape), dt))
        return t

    def ps(self, shape, dt, name=None):
        self.cnt += 1
        t = self.stack.enter_context(
            self.k.nc.psum_tensor("%s_%s%d" % (self.name, name or "p", self.cnt), list(shape), dt))
        return t


class Ring:
    def __init__(self, ph, n, shape, dt, name, psum=False):
        self.t = [(ph.ps if psum else ph.sb)(shape, dt, name) for _ in range(n)]
        self.b = [Buf("%s%d" % (name, i)) for i in range(n)]
        self.i = -1
        self.n = n

    def next(self):
        self.i = (self.i + 1) % self.n
        return self.t[self.i], self.b[self.i]


def t5_bucket_np(dist):
    n = np.maximum(dist, 0)
    max_exact = 16
    nf = np.maximum(n, max_exact).astype(np.float32)
    large = max_exact + (np.log(nf / max_exact) / math.log(128 / max_exact) * (32 - max_exact)).astype(np.int32)
    large = np.minimum(large, 31)
    return np.where(n < max_exact, n, large)


class Cfg:
    def __init__(self, S=4096, depth=4, debug=None, stop_after=None):
        self.S = S
        self.T = S // 128
        self.TB = S // 512
        self.depth = depth
        self.debug = debug
        self.stop_after = stop_after


NIT = 18


class NS:
    pass


class Lag:
    def __init__(self, n):
        self.q = []
        self.n = n

    def push(self, fn):
        self.q.append(fn)
        while len(self.q) > self.n:
            self.q.pop(0)()

    def flush(self):
        while self.q:
            self.q.pop(0)()


def norm_rings(ph):
    return dict(
        junk=Ring(ph, 1, [128, D], BF16, "junk"),
        ss=Ring(ph, 4, [128, 4], F32, "ss"),
        hb=Ring(ph, 2, [128, D], BF16, "hb"),
        pT=Ring(ph, 2, [128, D], BF16, "pT", psum=True),
    )


def rstd_chain(k, xt, xb, ss, ssb, junk, junkb, n):
    nc = k.nc
    k.op(k.act, lambda: nc.scalar.activation(out=junk, in_=xt, func=AF.Square, accum_out=ss[:, 0:1]),
         reads=[xb], writes=[junkb, ssb])
    k.op(k.dve, lambda: nc.vector.tensor_scalar(out=ss[:, 1:2], in0=ss[:, 0:1], scalar1=1.0 / n, scalar2=1e-6,
                                                op0=ALU.mult, op1=ALU.add), reads=[ssb], writes=[ssb])
    k.op(k.act, lambda: nc.scalar.activation(out=ss[:, 2:3], in_=ss[:, 1:2], func=AF.Sqrt),
         reads=[ssb], writes=[ssb])
    k.op(k.dve, lambda: nc.vector.reciprocal(out=ss[:, 3:4], in_=ss[:, 2:3]), reads=[ssb], writes=[ssb])


def norm_tile(k, C, xt, xb, g_bc, gb, hT, hTb_tile, t, rings):
    nc = k.nc
    junk, junkb = rings["junk"].next()
    ss, ssb = rings["ss"].next()
    rstd_chain(k, xt[:], xb, ss, ssb, junk[:], junkb, D)
    hb, hbb = rings["hb"].next()
    k.op(k.dve, lambda: nc.vector.scalar_tensor_tensor(out=hb[:], in0=xt[:], scalar=ss[:, 3:4], in1=g_bc[:],
                                                       op0=ALU.mult, op1=ALU.mult),
         reads=[xb, ssb, gb], writes=[hbb])
    pT, pTb = rings["pT"].next()
    for kc in range(KC):
        k.op(k.pe, lambda kc=kc: nc.tensor.transpose(out=pT[:, kc * 128:(kc + 1) * 128],
                                                     in_=hb[:, kc * 128:(kc + 1) * 128], identity=C.ident[:]),
             reads=[hbb], writes=[pTb])
    k.op(k.act, lambda: nc.scalar.copy(out=hT[:, :, t * 128:(t + 1) * 128],
                                       in_=pT[:].rearrange("p (c n) -> p c n", c=KC)),
         reads=[pTb], writes=[hTb_tile])


def phase_A1(k, cfg, C, hT, hTb, layer):
    nc = k.nc
    with Phase(k, "A1") as ph:
        g_bc = ph.sb([128, D], F32, "g")
        gb = Buf("g")
        k.dma(k.sp, g_bc[:], C.g_attn[layer], writes=[gb])
        rings = norm_rings(ph)
        xr = Ring(ph, 3, [128, D], F32, "x")
        for t in range(cfg.T):
            xt, xb = xr.next()
            k.dma(k.sp, xt[:], C.x_in[t * 128:(t + 1) * 128, :], writes=[xb])
            norm_tile(k, C, xt, xb, g_bc, gb, hT, hTb[t], t, rings)


def phase_A2(k, cfg, C, hT, hTb, layer):
    nc = k.nc
    S, T, TB = cfg.S, cfg.T, cfg.TB
    with Phase(k, "A2") as ph:
        W = C.w_in_even[layer // 2] if layer % 2 == 0 else C.w_in_odd[layer // 2]
        Wv = W.rearrange("(c p) n -> p c n", p=128)
        if layer % 2 == 0:
            blocks = [(0, 512, "fm", ("Q", 0, 0.125)), (512, 512, "fm", ("K", 0, 1.0)),
                      (1024, 512, "tm", ("V", 0)),
                      (1536, 512, "fm", ("Q", 4, 0.125)), (2048, 512, "fm", ("K", 4, 1.0)),
                      (2560, 512, "tm", ("V", 4)),
                      (3072, 512, "fm", ("QI", 0, 1.0)), (3584, 512, "fm", ("QI", 4, 1.0)),
                      (4096, 80, "tm", ("KW", 0))]
        else:
            blocks = [(0, 512, "fm", ("Q", 0, 0.125)), (512, 512, "fm", ("Q", 4, 0.125)),
                      (1024, 512, "fm", ("K", 0, 1.0)), (1536, 512, "fm", ("K", 4, 1.0)),
                      (2048, 512, "tm", ("V", 0)), (2560, 512, "tm", ("V", 4))]
        wst = Ring(ph, 2, [128, KC, 512], F32, "wst")
        wbf = Ring(ph, 2, [128, KC, 512], BF16, "wbf")
        pacc = Ring(ph, 4, [128, 512], F32, "pacc", psum=True)
        ostg = Ring(ph, 2, [128, S], BF16, "ostg")
        vstg = Ring(ph, 3, [128, 512], BF16, "vstg")
        kwstg = Ring(ph, 3, [128, 80], F32, "kwstg")
        flip = 0
        for (c0, ncol, kind, dest) in blocks:
            ws, wsb = wst.next()
            k.dma(k.sp, ws[:, :, 0:ncol], Wv[:, :, c0:c0 + ncol], writes=[wsb])
            wb, wbb = wbf.next()
            k.op(k.pool, lambda: nc.gpsimd.tensor_copy(out=wb[:, :, 0:ncol], in_=ws[:, :, 0:ncol]),
                 reads=[wsb], writes=[wbb])
            if kind == "fm":
                name, cbase, scale = dest
                dst = {"Q": C.QT_d, "K": C.KT_d, "QI": C.QI_d}[name]
                for ci in range(ncol // 128):
                    og, ogb = ostg.next()
                    for tb in range(TB):
                        pa, pab = pacc.next()
                        for kc in range(KC):
                            k.op(k.pe, lambda kc=kc: nc.tensor.matmul(
                                pa[:], lhsT=wb[:, kc, ci * 128:(ci + 1) * 128],
                                rhs=hT[:, kc, tb * 512:(tb + 1) * 512],
                                start=(kc == 0), stop=(kc == KC - 1)),
                                reads=[wbb] + hTb[tb * 4:(tb + 1) * 4], writes=[pab])
                        flip ^= 1
                        if flip:
                            k.op(k.act, lambda: nc.scalar.mul(out=og[:, tb * 512:(tb + 1) * 512], in_=pa[:],
                                                              mul=scale), reads=[pab], writes=[ogb])
                        else:
                            k.op(k.dve, lambda: nc.vector.tensor_scalar(
                                out=og[:, tb * 512:(tb + 1) * 512], in0=pa[:], scalar1=scale, scalar2=None,
                                op0=ALU.mult), reads=[pab], writes=[ogb])
                    k.dma(k.pool, dst[cbase + ci], og[:], reads=[ogb])
            else:
                name, cbase = dest
                for t in range(T):
                    pa, pab = pacc.next()
                    for kc in range(KC):
                        k.op(k.pe, lambda kc=kc: nc.tensor.matmul(
                            pa[:, 0:ncol], lhsT=hT[:, kc, t * 128:(t + 1) * 128], rhs=wb[:, kc, 0:ncol],
                            start=(kc == 0), stop=(kc == KC - 1)),
                            reads=[wbb, hTb[t]], writes=[pab])
                    flip ^= 1
                    if name == "V":
                        vs, vsb = vstg.next()
                        if flip:
                            k.op(k.act, lambda: nc.scalar.copy(out=vs[:], in_=pa[:]), reads=[pab], writes=[vsb])
                        else:
                            k.op(k.dve, lambda: nc.vector.tensor_copy(out=vs[:], in_=pa[:]),
                                 reads=[pab], writes=[vsb])
                        k.dma(k.pool, C.V_d[cbase:cbase + 4, :, t, :].rearrange("c p n -> p c n"),
                              vs[:].rearrange("p (c n) -> p c n", c=4), reads=[vsb])
                    else:
                        vs, vsb = kwstg.next()
                        k.op(k.dve, lambda: nc.vector.tensor_copy(out=vs[:], in_=pa[:, 0:80]),
                             reads=[pab], writes=[vsb])
                        k.dma(k.pool, C.KW_d[:, t, :], vs[:], reads=[vsb])


def phase_tables(k, cfg, C):
    nc = k.nc
    with Phase(k, "T") as ph:
        negc = ph.sb([128, 16], F32, "negc")
        negb = Buf()
        k.op(k.dve, lambda: nc.vector.tensor_scalar(out=negc[:], in0=C.c31[:], scalar1=-1.0, scalar2=None,
                                                    op0=ALU.mult), reads=[C.c31b], writes=[negb])
        caus = ph.sb([128, 1024], F32, "caus")
        cdil = ph.sb([128, 1024], F32, "cdil")
        cb_, db_ = Buf(), Buf()
        k.dma(k.sp, caus[:], C.caus_in[:, :], writes=[cb_])
        k.dma(k.sp, cdil[:], C.cdil_in[:, :], writes=[db_])
        gr = Ring(ph, 2, [128, 1024], F32, "g")
        er = Ring(ph, 2, [128, 1024], F32, "e")
        mnr = Ring(ph, 2, [128, 1024], BF16, "mn")
        mdr = Ring(ph, 2, [128, 1024], BF16, "md")
        for h in range(16):
            g, gb = gr.next()
            k.dma(k.sp, g[:], C.G_in[h], writes=[gb])
            e, eb = er.next()
            k.op(k.act, lambda: nc.scalar.activation(out=e[:], in_=g[:], func=AF.Exp, bias=negc[:, h:h + 1],
                                                     scale=1.0), reads=[gb, negb], writes=[eb])
            mn, mnb = mnr.next()
            k.op(k.dve, lambda: nc.vector.tensor_tensor(out=mn[:], in0=e[:], in1=caus[:], op=ALU.mult),
                 reads=[eb, cb_], writes=[mnb])
            md, mdb = mdr.next()
            k.op(k.dve, lambda: nc.vector.tensor_tensor(out=md[:], in0=e[:], in1=cdil[:], op=ALU.mult),
                 reads=[eb, db_], writes=[mdb])
            k.dma(k.pool, C.Mnear_d[h], mn[:], reads=[mnb])
            k.dma(k.pool, C.Mdil_d[h], md[:], reads=[mdb])


def moba_prepass(k, cfg, C):
    nc = k.nc
    S, T = cfg.S, cfg.T
    NB = S // 256
    with Phase(k, "G") as ph:
        pm = ph.sb([128, 16, 16], F32, "pm")
        o3 = ph.sb([128, 16, 16], F32, "o3")
        pmb, o3b = Buf(), Buf()
        k.dma(k.sp, pm[:], C.pastmask_in[:, :, :], writes=[pmb])
        k.dma(k.sp, o3[:], C.own30k_in[:, :, :], writes=[o3b])
        qr = Ring(ph, 2, [128, S], BF16, "q")
        kr = Ring(ph, 2, [128, S], BF16, "k")
        ksr = Ring(ph, 2, [128, 16], F32, "ks")
        kmr = Ring(ph, 2, [128, 16], BF16, "km")
        qmr = Ring(ph, 2, [128, S], BF16, "qm")
        tpr = Ring(ph, 2, [128, 512], F32, "tp", psum=True)
        gpr = [Ring(ph, 2, [128, 512], F32, "gpa", psum=True), Ring(ph, 2, [128, 512], F32, "gpb", psum=True)]
        gmr = Ring(ph, 3, [128, 2, 16], F32, "gm")
        t8r = Ring(ph, 3, [128, 2, 8], F32, "t8")
        thr = Ring(ph, 3, [128, 2], F32, "th")
        t1r = Ring(ph, 3, [128, 2, 16], F32, "t1")
        mvr = Ring(ph, 3, [128, 128], BF16, "mv")
        for i in range(3):
            k.op(k.dve, lambda i=i: nc.vector.memset(mvr.t[i][:], 0.0), writes=[mvr.b[i]])
        for c in range(4):
            qt, qb = qr.next()
            kt, kb = kr.next()
            k.dma(k.sp, qt[:], C.QT_d[c], writes=[qb])
            k.dma(k.sp, kt[:], C.KT_d[c], writes=[kb])
            ks, ksb = ksr.next()
            km, kmb = kmr.next()
            k.op(k.dve, lambda: nc.vector.memset(ks[:], 0.0), writes=[ksb])
            k.op(k.dve, lambda: nc.vector.tensor_reduce(out=ks[:, 0:NB], in_=kt[:].rearrange("p (n b) -> p n b", b=256),
                                                        axis=AX.X, op=ALU.add), reads=[kb], writes=[ksb])
            k.op(k.dve, lambda: nc.vector.tensor_scalar(out=km[:], in0=ks[:], scalar1=1.0 / 256, scalar2=None,
                                                        op0=ALU.mult), reads=[ksb], writes=[kmb])
            qm, qmb = qmr.next()
            LV = getattr(cfg, "lv", 9)
            for t in range(T):
                if LV < 1:
                    break
                own = t // 2
                gps = [gpr[0].next(), gpr[1].next()]
                for hh in range(2):
                    k.op(k.pe, lambda hh=hh: nc.tensor.matmul(
                        gps[hh][0][:, 0:16], lhsT=qt[hh * 64:(hh + 1) * 64, t * 128:(t + 1) * 128],
                        rhs=km[hh * 64:(hh + 1) * 64, :], start=True, stop=True),
                        reads=[qb, kmb], writes=[gps[hh][1]])
                if LV < 2:
                    continue
                gm, gmb = gmr.next()
                t8, t8b = t8r.next()
                th, thb = thr.next()
                t1, t1b = t1r.next()
                mv, mvb = mvr.next()
                for hh in range(2):
                    k.op(k.dve, lambda hh=hh: nc.vector.tensor_tensor(
                        out=gm[:, hh, :], in0=gps[hh][0][:, 0:16], in1=pm[:, own, :], op=ALU.add),
                        reads=[gps[hh][1], pmb], writes=[gmb])
                for hh in range(2):
                    k.op(k.dve, lambda hh=hh: nc.vector.max(out=t8[:, hh, :], in_=gm[:, hh, :]),
                         reads=[gmb], writes=[t8b])
                k.op(k.dve, lambda: nc.vector.tensor_scalar(out=th[:], in0=t8[:, :, 2], scalar1=-1e29, scalar2=None,
                                                            op0=ALU.max), reads=[t8b], writes=[thb])
                for hh in range(2):
                    k.op(k.dve, lambda hh=hh: nc.vector.tensor_scalar(
                        out=t1[:, hh, :], in0=gm[:, hh, :], scalar1=th[:, hh:hh + 1], scalar2=1.0,
                        op0=ALU.is_ge, op1=ALU.subtract), reads=[gmb, thb], writes=[t1b])
                for hh in range(2):
                    k.op(k.dve, lambda hh=hh: nc.vector.scalar_tensor_tensor(
                        out=mv[:, 64 * hh:64 * hh + 16], in0=t1[:, hh, :], scalar=30000.0, in1=o3[:, own, :],
                        op0=ALU.mult, op1=ALU.add), reads=[t1b, o3b], writes=[mvb])
                if LV < 3:
                    continue
                tp, tpb = tpr.next()
                k.op(k.pe, lambda: nc.tensor.matmul(tp[:, 0:128], lhsT=mv[:], rhs=C.ident[:],
                                                    start=True, stop=True), reads=[mvb], writes=[tpb])
                if LV == 3:
                    continue
                k.op(k.act, lambda: nc.scalar.copy(out=qm[:, t * 128:(t + 1) * 128], in_=tp[:, 0:128]),
                     reads=[tpb], writes=[qmb])
            if LV >= 4:
                k.dma(k.pool, C.QM_d[c], qm[:], reads=[qmb])


def dsa_prepass(k, cfg, C, layer):
    nc = k.nc
    S, T, TB = cfg.S, cfg.T, cfg.TB
    KEEP = min(256, S // 4)
    with Phase(k, "I") as ph:
        kiT = ph.sb([128, S], BF16, "kiT")
        kiTb = [Buf() for _ in range(T)]
        kw = ph.sb([128, T, 80], F32, "kw")
        kwb = Buf()
        k.dma(k.sp, kw[:], C.KW_d[:, :, :], writes=[kwb])
        gk = ph.sb([128, 64], F32, "gk")
        gkb = Buf()
        k.dma(k.sp, gk[:], C.gk_in[layer // 2], writes=[gkb])
        tri = ph.sb([128, 128], F32, "tri")
        trib = Buf()
        k.dma(k.sp, tri[:], C.tri_in[:, :], writes=[trib])
        pw2 = ph.sb([128, NIT], F32, "pw2")
        pw2b = Buf()
        k.dma(k.sp, pw2[:], C.pow2_in[:, :], writes=[pw2b])
        wabs = ph.sb([128, T, 16], F32, "wabs")
        wsgn = ph.sb([128, T, 16], F32, "wsgn")
        wab, wsb_ = Buf(), Buf()
        k.op(k.act, lambda: nc.scalar.activation(out=wabs[:], in_=kw[:, :, 64:80], func=AF.Abs, scale=1.0 / 32),
             reads=[kwb], writes=[wab])
        k.op(k.act, lambda: nc.scalar.activation(out=wsgn[:], in_=kw[:, :, 64:80], func=AF.Sign),
             reads=[kwb], writes=[wsb_])
        ssr = Ring(ph, 4, [128, 4], F32, "ss")
        jkr = Ring(ph, 1, [128, 64], F32, "jk")
        kkr = Ring(ph, 2, [128, 128], BF16, "kk")
        tpr = Ring(ph, 2, [128, 1024], BF16, "tp", psum=True)
        for t in range(T):
            ss, ssb = ssr.next()
            jk, jkb = jkr.next()
            rstd_chain(k, kw[:, t, 0:64], kwb, ss, ssb, jk[:], jkb, 64)
            kk, kkb = kkr.next()
            k.op(k.dve, lambda: nc.vector.scalar_tensor_tensor(out=kk[:, 0:64], in0=kw[:, t, 0:64], scalar=ss[:, 3:4],
                                                               in1=gk[:], op0=ALU.mult, op1=ALU.mult),
                 reads=[kwb, ssb, gkb], writes=[kkb])
            k.op(k.dve, lambda: nc.vector.tensor_copy(out=kk[:, 64:128], in_=kk[:, 0:64]), reads=[kkb], writes=[kkb])
            tp, tpb = tpr.next()
            k.op(k.pe, lambda: nc.tensor.transpose(out=tp[:, 0:128], in_=kk[:], identity=C.ident[:]),
                 reads=[kkb], writes=[tpb])
            k.op(k.act, lambda: nc.scalar.copy(out=kiT[:, t * 128:(t + 1) * 128], in_=tp[:, 0:128]),
                 reads=[tpb], writes=[kiTb[t]])
        qir = Ring(ph, 2, [128, 8, 128], BF16, "qi")
        dsr = Ring(ph, 2, [128, 16, 128], BF16, "ds")
        spr = Ring(ph, 4, [128, 512], F32, "sp", psum=True)
        apr = Ring(ph, 2, [128, 512], F32, "ap", psum=True)
        rr = Ring(ph, 4, [128, 512], BF16, "r")
        scr = Ring(ph, 2, [128, S], F32, "sc")
        cjr = Ring(ph, 1, [128, S], BF16, "cj")
        m01r = Ring(ph, 2, [128, S], BF16, "m01")
        mtsr = Ring(ph, 2, [128, T, 128], BF16, "mts")
        smr = Ring(ph, 2, [128, 8], F32, "sm")
        hwr = Ring(ph, 2, [128, NIT], F32, "hw")
        for i in range(mtsr.n):
            k.op(k.pool, lambda i=i: nc.gpsimd.memset(mtsr.t[i][:], 0.0), writes=[mtsr.b[i]])
        ilag = Lag(3)
        prev_post = None
        for t in range(T):
            L = (t + 1) * 128
            qi, qib = qir.next()
            k.dma(k.sp, qi[:], C.QI_d[:, :, t * 128:(t + 1) * 128].rearrange("c p n -> p c n"), writes=[qib])
            ds, dsb = dsr.next()
            for h in range(16):
                k.op(k.pool, lambda h=h: nc.gpsimd.tensor_scalar(out=ds[:, h, :], in0=C.ident[:],
                                                                 scalar1=wsgn[:, t, h:h + 1], scalar2=None,
                                                                 op0=ALU.mult), reads=[wsb_], writes=[dsb])
            sc, scb = scr.next()
            nkb = (L + 511) // 512
            for kb in range(nkb):
                kw_ = min(512, L - kb * 512)
                ap_, apb = apr.next()
                for h in range(16):
                    c, hh = h // 2, h % 2
                    sp_, spb = spr.next()
                    k.op(k.pe, lambda: nc.tensor.matmul(
                        sp_[:, 0:kw_], lhsT=qi[hh * 64:(hh + 1) * 64, c, :],
                        rhs=kiT[hh * 64:(hh + 1) * 64, kb * 512:kb * 512 + kw_], start=True, stop=True),
                        reads=[qib] + kiTb[kb * 4:kb * 4 + (kw_ // 128)], writes=[spb])
                    r, rb = rr.next()
                    k.op(k.act, lambda: nc.scalar.activation(out=r[:, 0:kw_], in_=sp_[:, 0:kw_], func=AF.Relu,
                                                             scale=wabs[:, t, h:h + 1]),
                         reads=[spb, wab], writes=[rb])

                    def sgn(ap_=ap_, apb=apb, ds=ds, dsb=dsb, r=r, rb=rb, h=h, kw_=kw_):
                        k.op(k.pe, lambda: nc.tensor.matmul(ap_[:, 0:kw_], lhsT=ds[:, h, :], rhs=r[:, 0:kw_],
                                                            start=(h == 0), stop=(h == 15)),
                             reads=[dsb, rb], writes=[apb])
                    ilag.push(sgn)

                def cp(sc=sc, scb=scb, ap_=ap_, apb=apb, kb=kb, kw_=kw_):
                    k.op(k.act, lambda: nc.scalar.copy(out=sc[:, kb * 512:kb * 512 + kw_], in_=ap_[:, 0:kw_]),
                         reads=[apb], writes=[scb])
                ilag.push(cp)
            ilag.flush()
            sm, smb = smr.next()
            hw, hwb = hwr.next()
            k.op(k.dve, lambda: nc.vector.tensor_reduce(out=sm[:, 0:1], in_=sc[:, 0:L], axis=AX.X, op=ALU.max,
                                                        apply_absolute_value=True), reads=[scb], writes=[smb])
            k.op(k.dve, lambda: nc.vector.tensor_tensor(out=sc[:, L - 128:L], in0=sc[:, L - 128:L], in1=tri[:],
                                                        op=ALU.add), reads=[scb, trib], writes=[scb])
            k.op(k.dve, lambda: nc.vector.tensor_scalar(out=sm[:, 1:2], in0=sm[:, 0:1], scalar1=-1.0, scalar2=None,
                                                        op0=ALU.mult), reads=[smb], writes=[smb])
            k.op(k.dve, lambda: nc.vector.tensor_scalar(out=hw[:], in0=pw2[:], scalar1=sm[:, 0:1], scalar2=None,
                                                        op0=ALU.mult), reads=[smb, pw2b], writes=[hwb])
            cj, cjb = cjr.next()
            for it in range(NIT):
                k.op(k.dve, lambda: nc.vector.tensor_tensor(out=sm[:, 2:3], in0=sm[:, 1:2], in1=hw[:, it:it + 1],
                                                            op=ALU.add), reads=[smb, hwb], writes=[smb])
                k.op(k.dve, lambda: nc.vector.tensor_scalar(out=cj[:, 0:L], in0=sc[:, 0:L], scalar1=sm[:, 2:3],
                                                            scalar2=0.0, op0=ALU.is_ge, op1=ALU.add,
                                                            accum_out=sm[:, 3:4]),
                     reads=[scb, smb], writes=[cjb, smb])
                k.op(k.dve, lambda: nc.vector.scalar_tensor_tensor(out=sm[:, 4:5], in0=sm[:, 3:4],
                                                                   scalar=KEEP - 0.5, in1=hw[:, it:it + 1],
                                                                   op0=ALU.is_ge, op1=ALU.mult),
                     reads=[smb, hwb], writes=[smb])
                k.op(k.dve, lambda: nc.vector.tensor_tensor(out=sm[:, 1:2], in0=sm[:, 1:2], in1=sm[:, 4:5],
                                                            op=ALU.add), reads=[smb], writes=[smb])
            m01, m01b = m01r.next()
            k.op(k.dve, lambda: nc.vector.tensor_scalar(out=m01[:, 0:L], in0=sc[:, 0:L], scalar1=sm[:, 1:2],
                                                        scalar2=None, op0=ALU.is_ge), reads=[scb, smb], writes=[m01b])
            def post(t=t, m01=m01, m01b=m01b):
                mts, mtsb = mtsr.next()
                for j0 in range(0, t + 1, 8):
                    nj = min(8, t + 1 - j0)
                    tp, tpb = tpr.next()
                    for jj in range(nj):
                        k.op(k.pe, lambda jj=jj: nc.tensor.transpose(
                            out=tp[:, jj * 128:(jj + 1) * 128], in_=m01[:, (j0 + jj) * 128:(j0 + jj + 1) * 128],
                            identity=C.ident[:]), reads=[m01b], writes=[tpb])
                    k.op(k.act, lambda: nc.scalar.copy(out=mts[:, j0:j0 + nj, :],
                                                       in_=tp[:, 0:nj * 128].rearrange("p (j n) -> p j n", n=128)),
                         reads=[tpb], writes=[mtsb])
                i_q, sub = t // 4, t % 4
                nkt = 4 * i_q + 4
                k.dma(k.pool, C.MK_d[i_q, :, 0:nkt, sub * 128:(sub + 1) * 128], mts[:, 0:nkt, :], reads=[mtsb])
            if prev_post is not None:
                prev_post()
            prev_post = post
        prev_post()


def attention(k, cfg, C, mode, pairs, tag):
    nc = k.nc
    S, T, TB = cfg.S, cfg.T, cfg.TB
    with Phase(k, "B" + tag) as ph:
        qr = Ring(ph, 2, [128, S], BF16, "q")
        kr = Ring(ph, 2, [128, S], BF16, "k")
        ver = Ring(ph, 2, [128, T, 128], BF16, "ve")
        vor = Ring(ph, 2, [128, T, 128], BF16, "vo")
        for i in range(2):
            k.op(k.pool, lambda i=i: nc.gpsimd.memset(ver.t[i][:], 0.0), writes=[ver.b[i]])
            k.op(k.pool, lambda i=i: nc.gpsimd.memset(vor.t[i][:], 0.0), writes=[vor.b[i]])
        mnr = Ring(ph, 2, [128, 2, 1024], BF16, "mn")
        ptr = Ring(ph, 6, [128, 512], BF16, "pt")
        mor = Ring(ph, 2, [128, S], BF16, "mo")
        recr = Ring(ph, 2, [128, 512], F32, "rec")
        psr = Ring(ph, 4, [128, 512], F32, "ps", psum=True)
        por = Ring(ph, 2, [128, 512], F32, "po", psum=True)
        pdr = Ring(ph, 2, [128, 512], F32, "pd", psum=True)
        Mx_d = C.Mdil_d if mode == "dil" else C.Mnear_d
        if mode == "moba":
            oh = ph.sb([128, S], BF16, "oh")
            ohb = Buf()
            k.dma(k.sp, oh[:], C.onehot_in[:, :], writes=[ohb])
            qmr = Ring(ph, 2, [128, S], BF16, "qm")
        if mode == "dsa":
            mkr = Ring(ph, 2, [128, 16, 512], BF16, "mk")
        if mode == "dil":
            cf = ph.sb([128, 2432], BF16, "cf")
            cfb = Buf()
            k.dma(k.sp, cf[:], C.cfar_in[:, :], writes=[cfb])
        lag = Lag(3)
        tile_no = [0]
        for c in pairs:
            qt, qb = qr.next()
            kt, kb = kr.next()
            ve, veb = ver.next()
            vo, vob = vor.next()
            mn, mnb = mnr.next()
            k.dma(k.sp, qt[:], C.QT_d[c], writes=[qb])
            k.dma(k.sp, kt[:], C.KT_d[c], writes=[kb])
            k.dma(k.sp, ve[:, :, 0:64], C.V_d[c, :, :, 0:64], writes=[veb])
            k.dma(k.sp, vo[:, :, 64:128], C.V_d[c, :, :, 64:128], writes=[vob])
            k.dma(k.sp, mn[:], Mx_d[2 * c:2 * c + 2].rearrange("h p n -> p h n"), writes=[mnb])
            qm = qmb = None
            if mode == "moba":
                qm, qmb = qmr.next()
                k.dma(k.sp, qm[:], C.QM_d[c], writes=[qmb])
            mo, mob = mor.next()
            for i in range(TB):
                q0 = i * 512
                jhi = 4 * i + 3
                jlo = max(0, 4 * i - 16) if mode == "dil" else 0
                po, pob = por.next()
                pd, pdb = pdr.next()
                seq = [(j, hh) for j in range(jlo, jhi + 1) for hh in range(2)]
                mk = mkb = None
                for idx, (j, hh) in enumerate(seq):
                    if mode == "dsa" and hh == 0 and (j % 16 == 0):
                        mk, mkb = mkr.next()
                        g1 = min(jhi + 1, j + 16)
                        k.dma(k.sp, mk[:, 0:g1 - j, :], C.MK_d[i, :, j:g1, :], writes=[mkb])
                    rel = q0 - 128 * j
                    near = rel <= 128
                    hd = 2 * c + hh
                    ps, psb = psr.next()
                    k.op(k.pe, lambda: nc.tensor.matmul(
                        ps[:], lhsT=kt[hh * 64:(hh + 1) * 64, j * 128:(j + 1) * 128],
                        rhs=qt[hh * 64:(hh + 1) * 64, q0:q0 + 512], start=True, stop=(mode != "moba")),
                        reads=[kb, qb], writes=[psb])
                    if mode == "moba":
                        k.op(k.pe, lambda: nc.tensor.matmul(
                            ps[:], lhsT=oh[64 * hh:64 * hh + 16, j * 128:(j + 1) * 128],
                            rhs=qm[64 * hh:64 * hh + 16, q0:q0 + 512],
                            start=False, stop=True), reads=[ohb, qmb], writes=[psb])
                    pt, ptb = ptr.next()
                    k.op(k.act, lambda: nc.scalar.activation(out=pt[:], in_=ps[:], func=AF.Exp,
                                                             bias=C.c31[:, hd:hd + 1], scale=1.0),
                         reads=[psb, C.c31b], writes=[ptb])
                    tile_no[0] += 1
                    if tile_no[0] % 2 == 0:
                        ME, mfn = k.pool, nc.gpsimd.tensor_tensor
                    else:
                        ME, mfn = k.dve, nc.vector.tensor_tensor
                    if near:
                        o = rel + 384
                        k.op(ME, lambda: mfn(out=pt[:], in0=pt[:], in1=mn[:, hh, o:o + 512], op=ALU.mult),
                             reads=[ptb, mnb], writes=[ptb])
                    elif mode == "dil":
                        o = rel - 129
                        k.op(ME, lambda: mfn(out=pt[:], in0=pt[:], in1=cf[:, o:o + 512], op=ALU.mult),
                             reads=[ptb, cfb], writes=[ptb])
                    if mode == "dsa":
                        jj = j % 16
                        k.op(ME, lambda: mfn(out=pt[:], in0=pt[:], in1=mk[:, jj, :], op=ALU.mult),
                             reads=[ptb, mkb], writes=[ptb])
                    first, last = idx == 0, idx == len(seq) - 1

                    def pv(pt=pt, ptb=ptb, j=j, hh=hh, first=first, last=last, po=po, pob=pob, pd=pd, pdb=pdb,
                           ve=ve, veb=veb, vo=vo, vob=vob):
                        vv, vvb = (ve, veb) if hh == 0 else (vo, vob)
                        k.op(k.pe, lambda: nc.tensor.matmul(po[:], lhsT=vv[:, j, :], rhs=pt[:], start=first,
                                                            stop=last), reads=[vvb, ptb], writes=[pob])
                        on = C.onesE if hh == 0 else C.onesO
                        k.op(k.pe, lambda: nc.tensor.matmul(pd[:], lhsT=on, rhs=pt[:], start=first, stop=last),
                             reads=[ptb, C.onesb], writes=[pdb])
                    lag.push(pv)

                def fin(po=po, pob=pob, pd=pd, pdb=pdb, mo=mo, mob=mob, q0=q0):
                    rec, recb = recr.next()
                    k.op(k.dve, lambda: nc.vector.reciprocal(out=rec[:], in_=pd[:]), reads=[pdb], writes=[recb])
                    k.op(k.dve, lambda: nc.vector.tensor_tensor(out=mo[:, q0:q0 + 512], in0=po[:], in1=rec[:],
                                                                op=ALU.mult), reads=[pob, recb], writes=[mob])
                lag.push(fin)

            def st(mo=mo, mob=mob, c=c):
                k.dma(k.pool, C.MT_d[c], mo[:], reads=[mob])
            lag.push(st)
        lag.flush()


def phase_C(k, cfg, C, hT, hTb, layer):
    nc = k.nc
    S, T = cfg.S, cfg.T
    with Phase(k, "C") as ph:
        wo = ph.sb([128, KC, D], BF16, "wo")
        wob = Buf()
        wst = Ring(ph, 2, [128, KC, 512], F32, "wst")
        Wv = C.w_o[layer].rearrange("(c p) n -> p c n", p=128)
        for nb in range(2):
            ws, wsb = wst.next()
            k.dma(k.sp, ws[:], Wv[:, :, nb * 512:(nb + 1) * 512], writes=[wsb])
            k.op(k.pool, lambda: nc.gpsimd.tensor_copy(out=wo[:, :, nb * 512:(nb + 1) * 512], in_=ws[:]),
                 reads=[wsb], writes=[wob])
        g_bc = ph.sb([128, D], F32, "g")
        gb = Buf()
        k.dma(k.sp, g_bc[:], C.g_ffn[layer], writes=[gb])
        rings = norm_rings(ph)
        mtr = Ring(ph, 2, [128, KC, 128], BF16, "mt")
        xr = Ring(ph, 3, [128, D], F32, "x")
        xmr = Ring(ph, 2, [128, D], F32, "xm")
        pacc = Ring(ph, 4, [128, 512], F32, "pacc", psum=True)
        xsrc = C.x_in if layer == 0 else C.xs
        for t in range(T):
            mt, mtb = mtr.next()
            k.dma(k.sp, mt[:], C.MT_d[:, :, t * 128:(t + 1) * 128].rearrange("c p n -> p c n"), writes=[mtb])
            xt, xb = xr.next()
            k.dma(k.sp, xt[:], xsrc[t * 128:(t + 1) * 128, :], writes=[xb])
            xm, xmb = xmr.next()
            for nb in range(2):
                pa, pab = pacc.next()
                for c in range(KC):
                    k.op(k.pe, lambda c=c: nc.tensor.matmul(pa[:], lhsT=mt[:, c, :],
                                                            rhs=wo[:, c, nb * 512:(nb + 1) * 512],
                                                            start=(c == 0), stop=(c == KC - 1)),
                         reads=[mtb, wob], writes=[pab])
                k.op(k.dve, lambda: nc.vector.tensor_tensor(out=xm[:, nb * 512:(nb + 1) * 512], in0=pa[:],
                                                            in1=xt[:, nb * 512:(nb + 1) * 512], op=ALU.add),
                     reads=[pab, xb], writes=[xmb])
            k.dma(k.pool, C.xs[t * 128:(t + 1) * 128, :], xm[:], reads=[xmb])
            norm_tile(k, C, xm, xmb, g_bc, gb, hT, hTb[t], t, rings)


def phase_F1(k, cfg, C, hT, hTb, layer):
    nc = k.nc
    S, T, TB = cfg.S, cfg.T, cfg.TB
    with Phase(k, "F1") as ph:
        cp = ph.sb([128, 4, 44], F32, "cp")
        cpb = Buf()
        k.dma(k.sp, cp[:], C.convp[layer], writes=[cpb])
        Wv = C.w_up[layer].rearrange("(c p) n -> p c n", p=128)
        wst = Ring(ph, 2, [128, KC, 256], F32, "wst")
        wbf = Ring(ph, 2, [128, KC, 256], BF16, "wbf")
        pacc = Ring(ph, 4, [128, 512], F32, "pacc", psum=True)
        ubr = [Ring(ph, 2, [128, 514], F32, "ubv"), Ring(ph, 2, [128, 514], F32, "ubg")]
        car = Ring(ph, 3, [128, 512], F32, "ca")
        cbr = Ring(ph, 3, [128, 512], F32, "cb")
        sgr = Ring(ph, 2, [128, 512], F32, "sg")
        asr = Ring(ph, 2, [128, S], BF16, "as")
        for f in range(NF):
            ws, wsb = wst.next()
            k.dma(k.sp, ws[:, :, 0:128], Wv[:, :, f * 128:(f + 1) * 128], writes=[wsb])
            k.dma(k.sp, ws[:, :, 128:256], Wv[:, :, DFF + f * 128:DFF + (f + 1) * 128], writes=[wsb])
            wb, wbb = wbf.next()
            k.op(k.pool, lambda: nc.gpsimd.tensor_copy(out=wb[:], in_=ws[:]), reads=[wsb], writes=[wbb])
            a_s, asb = asr.next()
            prev = [None, None]
            for tb in range(TB):
                cv = [None, None]
                for half in range(2):
                    col = f if half == 0 else NF + f
                    pa, pab = pacc.next()
                    for kc in range(KC):
                        k.op(k.pe, lambda kc=kc: nc.tensor.matmul(
                            pa[:], lhsT=wb[:, kc, half * 128:(half + 1) * 128],
                            rhs=hT[:, kc, tb * 512:(tb + 1) * 512], start=(kc == 0), stop=(kc == KC - 1)),
                            reads=[wbb] + hTb[tb * 4:(tb + 1) * 4], writes=[pab])
                    ub, ubb = ubr[half].next()
                    if tb == 0:
                        k.op(k.pool, lambda: nc.gpsimd.memset(ub[:, 0:2], 0.0), writes=[ubb])
                    else:
                        pu, pub = prev[half]
                        k.op(k.pool, lambda: nc.gpsimd.tensor_copy(out=ub[:, 0:2], in_=pu[:, 512:514]),
                             reads=[pub], writes=[ubb])
                    k.op(k.act, lambda: nc.scalar.copy(out=ub[:, 2:514], in_=pa[:]), reads=[pab], writes=[ubb])
                    prev[half] = (ub, ubb)
                    r = car if half == 0 else cbr
                    c1, c1b = r.next()
                    k.op(k.dve, lambda: nc.vector.tensor_scalar(out=c1[:], in0=ub[:, 2:514],
                                                                scalar1=cp[:, 2, col:col + 1],
                                                                scalar2=cp[:, 3, col:col + 1],
                                                                op0=ALU.mult, op1=ALU.add),
                         reads=[ubb, cpb], writes=[c1b])
                    c2, c2b = r.next()
                    k.op(k.dve, lambda: nc.vector.scalar_tensor_tensor(out=c2[:], in0=ub[:, 1:513],
                                                                       scalar=cp[:, 1, col:col + 1], in1=c1[:],
                                                                       op0=ALU.mult, op1=ALU.add),
                         reads=[ubb, cpb, c1b], writes=[c2b])
                    c3, c3b = r.next()
                    k.op(k.dve, lambda: nc.vector.scalar_tensor_tensor(out=c3[:], in0=ub[:, 0:512],
                                                                       scalar=cp[:, 0, col:col + 1], in1=c2[:],
                                                                       op0=ALU.mult, op1=ALU.add),
                         reads=[ubb, cpb, c2b], writes=[c3b])
                    cv[half] = (c3, c3b)
                sg, sgb = sgr.next()
                k.op(k.act, lambda: nc.scalar.activation(out=sg[:], in_=cv[1][0][:], func=AF.Silu),
                     reads=[cv[1][1]], writes=[sgb])
                k.op(k.dve, lambda: nc.vector.tensor_tensor(out=a_s[:, tb * 512:(tb + 1) * 512], in0=sg[:],
                                                            in1=cv[0][0][:], op=ALU.mult),
                     reads=[sgb, cv[0][1]], writes=[asb])
            k.dma(k.pool, C.AT_d[f], a_s[:], reads=[asb])


def phase_F2(k, cfg, C, hT, hTb, layer, last):
    nc = k.nc
    S, T = cfg.S, cfg.T
    with Phase(k, "F2") as ph:
        wd = ph.sb([128, NF, D], BF16, "wd")
        wdb = Buf()
        wst = Ring(ph, 2, [128, NF, 128], F32, "wst")
        Wv = C.w_down[layer].rearrange("(f p) n -> p f n", p=128)
        for cb in range(8):
            ws, wsb = wst.next()
            k.dma(k.sp, ws[:], Wv[:, :, cb * 128:(cb + 1) * 128], writes=[wsb])
            k.op(k.pool, lambda: nc.gpsimd.tensor_copy(out=wd[:, :, cb * 128:(cb + 1) * 128], in_=ws[:]),
                 reads=[wsb], writes=[wdb])
        g_bc = ph.sb([128, D], F32, "g")
        gb = Buf()
        k.dma(k.sp, g_bc[:], C.g_fin[:, :] if last else C.g_attn[layer + 1], writes=[gb])
        rings = norm_rings(ph)
        atr = Ring(ph, 2, [128, NF, 128], BF16, "at")
        xr = Ring(ph, 3, [128, D], F32, "x")
        xnr = Ring(ph, 2, [128, D], F32, "xn")
        yor = Ring(ph, 2, [128, D], F32, "yo")
        pacc = Ring(ph, 4, [128, 512], F32, "pacc", psum=True)
        for t in range(T):
            at, atb = atr.next()
            k.dma(k.sp, at[:], C.AT_d[:, :, t * 128:(t + 1) * 128].rearrange("f p n -> p f n"), writes=[atb])
            xt, xb = xr.next()
            k.dma(k.sp, xt[:], C.xs[t * 128:(t + 1) * 128, :], writes=[xb])
            xn, xnb = xnr.next()
            for nb in range(2):
                pa, pab = pacc.next()
                for f in range(NF):
                    k.op(k.pe, lambda f=f: nc.tensor.matmul(pa[:], lhsT=at[:, f, :],
                                                            rhs=wd[:, f, nb * 512:(nb + 1) * 512],
                                                            start=(f == 0), stop=(f == NF - 1)),
                         reads=[atb, wdb], writes=[pab])
                k.op(k.dve, lambda: nc.vector.tensor_tensor(out=xn[:, nb * 512:(nb + 1) * 512], in0=pa[:],
                                                            in1=xt[:, nb * 512:(nb + 1) * 512], op=ALU.add),
                     reads=[pab, xb], writes=[xnb])
            if last:
                junk, junkb = rings["junk"].next()
                ss, ssb = rings["ss"].next()
                rstd_chain(k, xn[:], xnb, ss, ssb, junk[:], junkb, D)
                yo, yob = yor.next()
                k.op(k.dve, lambda: nc.vector.scalar_tensor_tensor(out=yo[:], in0=xn[:], scalar=ss[:, 3:4],
                                                                   in1=g_bc[:], op0=ALU.mult, op1=ALU.mult),
                     reads=[xnb, ssb, gb], writes=[yob])
                k.dma(k.pool, C.out[t * 128:(t + 1) * 128, :], yo[:], reads=[yob])
            else:
                k.dma(k.pool, C.xs[t * 128:(t + 1) * 128, :], xn[:], reads=[xnb])
                norm_tile(k, C, xn, xnb, g_bc, gb, hT, hTb[t], t, rings)


def build(cfg):
    S, T, TB = cfg.S, cfg.T, cfg.TB
    nc = bass.Bass("TRN2", target_bir_lowering=False)
    k = K(nc)
    C = NS()

    def din(name, shape, dt=F32):
        return nc.dram_tensor(name, list(shape), dt, kind="ExternalInput").ap()

    def dscr(name, shape, dt):
        kind = "ExternalOutput" if (cfg.debug and name in cfg.debug) else "Internal"
        return nc.dram_tensor(name, list(shape), dt, kind=kind).ap()

    C.x_in = din("x", [S, D])
    C.w_in_even = din("w_in_even", [2, D, 4176])
    C.w_in_odd = din("w_in_odd", [2, D, 3072])
    C.w_o = din("w_o", [4, D, D])
    C.w_up = din("w_up", [4, D, 2 * DFF])
    C.w_down = din("w_down", [4, DFF, D])
    C.g_attn = din("g_attn", [4, 128, D])
    C.g_ffn = din("g_ffn", [4, 128, D])
    C.g_fin = din("g_fin", [128, D])
    C.gk_in = din("gk", [2, 128, 64])
    C.convp = din("convp", [4, 128, 4, 44])
    C.G_in = din("G", [16, 128, 1024])
    c31_in = din("c31", [128, 16])
    ident_in = din("ident", [128, 128], BF16)
    ones_in = din("ones2", [128, 2, 128], BF16)
    C.caus_in = din("caus", [128, 1024])
    C.cdil_in = din("cdil", [128, 1024])
    C.cfar_in = din("cfar", [128, 2432], BF16)
    C.onehot_in = din("onehot", [128, S], BF16)
    C.pastmask_in = din("pastmask", [128, 16, 16])
    C.own30k_in = din("own30k", [128, 16, 16])
    C.tri_in = din("tri", [128, 128])
    C.pow2_in = din("pow2", [128, NIT])

    C.QT_d = dscr("QT_d", [8, 128, S], BF16)
    C.KT_d = dscr("KT_d", [8, 128, S], BF16)
    C.V_d = dscr("V_d", [8, 128, T, 128], BF16)
    C.QI_d = dscr("QI_d", [8, 128, S], BF16)
    C.KW_d = dscr("KW_d", [128, T, 80], F32)
    C.QM_d = dscr("QM_d", [4, 128, S], BF16)
    C.MK_d = dscr("MK_d", [TB, 128, T, 512], BF16)
    C.MT_d = dscr("MT_d", [8, 128, S], BF16)
    C.AT_d = dscr("AT_d", [NF, 128, S], BF16)
    C.xs = dscr("xs", [S, D], F32)
    C.Mnear_d = dscr("Mnear_d", [16, 128, 1024], BF16)
    C.Mdil_d = dscr("Mdil_d", [16, 128, 1024], BF16)
    C.out = nc.dram_tensor("out", [S, D], F32, kind="ExternalOutput").ap()

    stop_after = cfg.stop_after

    with ExitStack() as top:
        C.ident = top.enter_context(nc.sbuf_tensor("ident_sb", [128, 128], BF16))
        C.c31 = top.enter_context(nc.sbuf_tensor("c31_sb", [128, 16], F32))
        ones2 = top.enter_context(nc.sbuf_tensor("ones_sb", [128, 2, 128], BF16))
        C.onesE = ones2[:, 0, :]
        C.onesO = ones2[:, 1, :]
        C.c31b, C.onesb, identb = Buf(), Buf(), Buf()
        k.dma(k.sp, C.ident[:], ident_in[:, :], writes=[identb])
        k.dma(k.sp, C.c31[:], c31_in[:, :], writes=[C.c31b])
        k.dma(k.sp, ones2[:], ones_in[:, :, :], writes=[C.onesb])
        k.barrier()
        phase_tables(k, cfg, C)

        def done(tag):
            return stop_after == tag

        fin = False
        for layer in range(cfg.depth):
            if fin:
                break
            if layer == 0:
                with Phase(k, "H0") as hp:
                    hT = hp.sb([128, KC, S], BF16, "hT")
                    hTb = [Buf() for _ in range(T)]
                    phase_A1(k, cfg, C, hT, hTb, 0)
                    phase_A2(k, cfg, C, hT, hTb, 0)
            if done("A%d" % layer):
                break
            if layer % 2 == 0:
                moba_prepass(k, cfg, C)
                if done("G%d" % layer):
                    break
                attention(k, cfg, C, "moba", [0, 1, 2, 3], "m")
                if done("M%d" % layer):
                    break
                dsa_prepass(k, cfg, C, layer)
                attention(k, cfg, C, "dsa", [4, 5, 6, 7], "d")
            else:
                attention(k, cfg, C, "dil", list(range(8)), "l")
            if done("B%d" % layer):
                break
            with Phase(k, "HC") as hp:
                hT = hp.sb([128, KC, S], BF16, "hT")
                hTb = [Buf() for _ in range(T)]
                phase_C(k, cfg, C, hT, hTb, layer)
                if done("C%d" % layer):
                    fin = True
                else:
                    phase_F1(k, cfg, C, hT, hTb, layer)
            if fin or done("F1%d" % layer):
                break
            last = layer == cfg.depth - 1
            with Phase(k, "HF") as hp:
                hT = hp.sb([128, KC, S], BF16, "hT")
                hTb = [Buf() for _ in range(T)]
                phase_F2(k, cfg, C, hT, hTb, layer, last)
                if not last and not done("F2%d" % layer):
                    phase_A2(k, cfg, C, hT, hTb, layer + 1)
            if done("F2%d" % layer):
                break
        k.final_wait()
    k.stack.close()
    return nc


def host_inputs(cfg, b, x, w_in_even, idx_k_norm, w_in_odd, w_o, rel_bias, attn_norm, ffn_norm,
                w_up, conv_w, conv_b, w_down, final_norm):
    S = cfg.S
    bf = ml_dtypes.bfloat16
    m = {}
    m["x"] = np.ascontiguousarray(x[b], dtype=np.float32)
    m["w_in_even"] = w_in_even
    m["w_in_odd"] = w_in_odd
    m["w_o"] = w_o
    m["w_up"] = w_up
    m["w_down"] = w_down
    m["g_attn"] = np.ascontiguousarray(np.broadcast_to(attn_norm[:, None, :], (4, 128, D)))
    m["g_ffn"] = np.ascontiguousarray(np.broadcast_to(ffn_norm[:, None, :], (4, 128, D)))
    m["g_fin"] = np.ascontiguousarray(np.broadcast_to(final_norm[None, :], (128, D)))
    m["gk"] = np.ascontiguousarray(np.broadcast_to(idx_k_norm[:, None, :], (2, 128, 64)))
    cp = np.zeros((4, 128, 4, 44), np.float32)
    for l in range(4):
        for j in range(3):
            cp[l, :, j, :] = conv_w[l, j].reshape(44, 128).T
        cp[l, :, 3, :] = conv_b[l].reshape(44, 128).T
    m["convp"] = cp
    p = np.arange(128)[:, None]
    j = np.arange(1024)[None, :]
    d = j - 384 - p
    bk = t5_bucket_np(d)
    m["G"] = np.ascontiguousarray(np.transpose(rel_bias[bk, :], (2, 0, 1))).astype(np.float32)
    m["c31"] = np.ascontiguousarray(np.broadcast_to(rel_bias[31][None, :], (128, 16))).astype(np.float32)
    m["ident"] = np.eye(128, dtype=np.float32).astype(bf)
    o2 = np.zeros((128, 2, 128), np.float32)
    o2[:, 0, 0:64] = 1.0
    o2[:, 1, 64:128] = 1.0
    m["ones2"] = o2.astype(bf)
    m["caus"] = (d >= 0).astype(np.float32)

    def cmul(dd):
        return (((dd >= 0) & (dd <= 128)).astype(np.float32)
                + ((dd >= 0) & (dd <= 512) & (dd % 4 == 0)).astype(np.float32)
                + ((dd >= 0) & (dd <= 2048) & (dd % 16 == 0)).astype(np.float32))
    m["cdil"] = cmul(d)
    jf = np.arange(2432)[None, :]
    m["cfar"] = cmul(jf - p + 129).astype(bf)
    kk = np.arange(S)[None, :]
    n128 = np.arange(128)[:, None]
    m["onehot"] = ((kk // 256 == (n128 % 64)) & ((n128 % 64) < 16)).astype(np.float32).astype(bf)
    own = np.arange(16)[:, None]
    nn = np.arange(16)[None, :]
    pmk = np.where(nn < own, 0.0, -1e30).astype(np.float32)
    m["pastmask"] = np.ascontiguousarray(np.broadcast_to(pmk[None], (128, 16, 16)))
    o3 = np.where(nn == own, 30000.0, 0.0).astype(np.float32)
    m["own30k"] = np.ascontiguousarray(np.broadcast_to(o3[None], (128, 16, 16)))
    q = np.arange(128)[:, None]
    kx = np.arange(128)[None, :]
    m["tri"] = np.where(kx <= q, 0.0, -1e30).astype(np.float32)
    m["pow2"] = np.ascontiguousarray(np.broadcast_to((2.0 ** -np.arange(NIT))[None, :], (128, NIT))).astype(np.float32)
    return m


_CACHE = {}


def kernel(**inputs):
    inputs = {k_: np.asarray(v) for k_, v in inputs.items()}
    x = inputs["x"]
    B, S, _ = x.shape
    cfg = Cfg(S=S, depth=4)
    if "nc" not in _CACHE:
        _CACHE["nc"] = build(cfg)
    nc = _CACHE["nc"]
    in_maps = []
    for core in range(8):
        in_maps.append(host_inputs(cfg, core // 2, **inputs))
    res = run_bass_kernel_spmd(nc, in_maps, core_ids=list(range(8)))
    outs = [res.results[2 * b]["out"] for b in range(B)]
    return np.stack(outs, axis=0).astype(np.float32)
```

```python
import math
from contextlib import ExitStack

import numpy as np
import ml_dtypes
import concourse.bass as bass
import concourse.mybir as mybir
from concourse.bass_utils import run_bass_kernel_spmd

F32 = mybir.dt.float32
BF16 = mybir.dt.bfloat16
AF = mybir.ActivationFunctionType
ALU = mybir.AluOpType
AX = mybir.AxisListType

D = 1024
KC = 8
DFF = 2816
NF = 22
NEG = -30000.0
SAME_ENGINE_SYNC = True


class Buf:
    __slots__ = ("name", "w", "r")

    def __init__(self, name=""):
        self.name = name
        self.w = {}
        self.r = {}


class Eng:
    def __init__(self, K, name, e, is_pe=False):
        self.K = K
        self.name = name
        self.e = e
        self.is_pe = is_pe
        self.sem = K.new_sem("pg_" + name)
        self.n = 0
        self.waited = {}

    def wait(self, sem, val):
        key = id(sem)
        if self.waited.get(key, 0) >= val:
            return
        self.waited[key] = val
        self.e.wait_ge(sem, val)


class K:
    def __init__(self, nc):
        self.nc = nc
        self.stack = ExitStack()
        self.sems = []
        self.pe = Eng(self, "pe", nc.tensor, is_pe=True)
        self.act = Eng(self, "act", nc.scalar)
        self.dve = Eng(self, "dve", nc.vector)
        self.pool = Eng(self, "pool", nc.gpsimd)
        self.sp = Eng(self, "sp", nc.sync)
        self.engs = [self.pe, self.act, self.dve, self.pool, self.sp]
        self.dq = {}
        for q in (self.sp, self.pool, self.act):
            self.dq[q.name] = dict(sems=[self.new_sem("dq_%s%d" % (q.name, i)) for i in range(8)],
                                   tot=[0] * 8, i=0)

    def new_sem(self, name):
        s = self.stack.enter_context(self.nc.semaphore(name))
        self.sems.append(s)
        return s

    def _deps(self, X, reads, writes, acc=False):
        for b in reads:
            for sem, val in b.w.values():
                if (sem is X.sem) and (X.is_pe or not SAME_ENGINE_SYNC):
                    continue
                X.wait(sem, val)
        for b in writes:
            for sem, val in list(b.w.values()) + list(b.r.values()):
                if (sem is X.sem) and (X.is_pe or not SAME_ENGINE_SYNC):
                    continue
                X.wait(sem, val)

    def _mark(self, ev, reads, writes):
        sem, val = ev
        for b in reads:
            b.r[id(sem)] = ev
        for b in writes:
            b.w = {id(sem): ev}
            b.r = {}

    def op(self, X, ins_fn, reads=(), writes=()):
        self._deps(X, reads, writes)
        ins = ins_fn()
        ins.then_inc(X.sem, 1)
        X.n += 1
        self._mark((X.sem, X.n), reads, writes)
        return ins

    def dma(self, Q, out, in_, reads=(), writes=(), **kw):
        self._deps(Q, reads, writes)
        dq = self.dq[Q.name]
        i = dq["i"]
        dq["i"] = (i + 1) % len(dq["sems"])
        sem = dq["sems"][i]
        if dq["tot"][i] > 0:
            Q.wait(sem, dq["tot"][i])
        Q.e.dma_start(out=out, in_=in_, **kw).then_inc(sem, 16)
        dq["tot"][i] += 16
        self._mark((sem, dq["tot"][i]), reads, writes)

    def barrier(self):
        for X in self.engs:
            for Y in self.engs:
                if Y is not X and Y.n > 0:
                    X.wait(Y.sem, Y.n)
            for dq in self.dq.values():
                for sem, tot in zip(dq["sems"], dq["tot"]):
                    if tot > 0:
                        X.wait(sem, tot)

    def final_wait(self):
        X = self.sp
        for Y in self.engs:
            if Y is not X and Y.n > 0:
                X.wait(Y.sem, Y.n)
        for dq in self.dq.values():
            for sem, tot in zip(dq["sems"], dq["tot"]):
                if tot > 0:
                    X.wait(sem, tot)


class Phase:
    _uid = [0]

    def __init__(self, k, name):
        self.k = k
        Phase._uid[0] += 1
        self.name = "%s%d" % (name, Phase._uid[0])
        self.stack = ExitStack()
        self.cnt = 0

    def __enter__(self):
        return self

    def __exit__(self, *a):
        self.k.barrier()
        self.stack.close()
        return False

    def sb(self, shape, dt, name=None):
        self.cnt += 1
        t = self.stack.enter_context(
            self.k.nc.sbuf_tensor("%s_%s%d" % (self.name, name or "t", self.cnt), list(shape), dt))
        return t

    def ps(self, shape, dt, name=None):
        self.cnt += 1
        t = self.stack.enter_context(
            self.k.nc.psum_tensor("%s_%s%d" % (self.name, name or "p", self.cnt), list(shape), dt))
        return t


class Ring:
    def __init__(self, ph, n, shape, dt, name, psum=False):
        self.t = [(ph.ps if psum else ph.sb)(shape, dt, name) for _ in range(n)]
        self.b = [Buf("%s%d" % (name, i)) for i in range(n)]
        self.i = -1
        self.n = n

    def next(self):
        self.i = (self.i + 1) % self.n
        return self.t[self.i], self.b[self.i]


def t5_bucket_np(dist):
    n = np.maximum(dist, 0)
    max_exact = 16
    nf = np.maximum(n, max_exact).astype(np.float32)
    large = max_exact + (np.log(nf / max_exact) / math.log(128 / max_exact) * (32 - max_exact)).astype(np.int32)
    large = np.minimum(large, 31)
    return np.where(n < max_exact, n, large)


class Cfg:
    def __init__(self, S=4096, depth=4, debug=None, stop_after=None):
        self.S = S
        self.T = S // 128
        self.TB = S // 512
        self.depth = depth
        self.debug = debug
        self.stop_after = stop_after


NIT = 18


class NS:
    pass


class Lag:
    def __init__(self, n):
        self.q = []
        self.n = n

    def push(self, fn):
        self.q.append(fn)
        while len(self.q) > self.n:
            self.q.pop(0)()

    def flush(self):
        while self.q:
            self.q.pop(0)()


def norm_rings(ph):
    return dict(
        junk=Ring(ph, 1, [128, D], BF16, "junk"),
        ss=Ring(ph, 4, [128, 4], F32, "ss"),
        hb=Ring(ph, 2, [128, D], BF16, "hb"),
        pT=Ring(ph, 2, [128, D], BF16, "pT", psum=True),
    )


def rstd_chain(k, xt, xb, ss, ssb, junk, junkb, n):
    nc = k.nc
    k.op(k.act, lambda: nc.scalar.activation(out=junk, in_=xt, func=AF.Square, accum_out=ss[:, 0:1]),
         reads=[xb], writes=[junkb, ssb])
    k.op(k.dve, lambda: nc.vector.tensor_scalar(out=ss[:, 1:2], in0=ss[:, 0:1], scalar1=1.0 / n, scalar2=1e-6,
                                                op0=ALU.mult, op1=ALU.add), reads=[ssb], writes=[ssb])
    k.op(k.act, lambda: nc.scalar.activation(out=ss[:, 2:3], in_=ss[:, 1:2], func=AF.Sqrt),
         reads=[ssb], writes=[ssb])
    k.op(k.dve, lambda: nc.vector.reciprocal(out=ss[:, 3:4], in_=ss[:, 2:3]), reads=[ssb], writes=[ssb])


def norm_tile_a(k, C, xt, xb, g_bc, gb, rings):
    nc = k.nc
    junk, junkb = rings["junk"].next()
    ss, ssb = rings["ss"].next()
    rstd_chain(k, xt[:], xb, ss, ssb, junk[:], junkb, D)
    hb, hbb = rings["hb"].next()
    k.op(k.dve, lambda: nc.vector.scalar_tensor_tensor(out=hb[:], in0=xt[:], scalar=ss[:, 3:4], in1=g_bc[:],
                                                       op0=ALU.mult, op1=ALU.mult),
         reads=[xb, ssb, gb], writes=[hbb])
    return hb, hbb


def norm_tile_b(k, C, hb, hbb, hT, hTb_tile, t, rings):
    nc = k.nc
    pT, pTb = rings["pT"].next()
    for kc in range(KC):
        k.op(k.pe, lambda kc=kc: nc.tensor.transpose(out=pT[:, kc * 128:(kc + 1) * 128],
                                                     in_=hb[:, kc * 128:(kc + 1) * 128], identity=C.ident[:]),
             reads=[hbb], writes=[pTb])
    k.op(k.act, lambda: nc.scalar.copy(out=hT[:, :, t * 128:(t + 1) * 128],
                                       in_=pT[:].rearrange("p (c n) -> p c n", c=KC)),
         reads=[pTb], writes=[hTb_tile])


def norm_tile(k, C, xt, xb, g_bc, gb, hT, hTb_tile, t, rings):
    hb, hbb = norm_tile_a(k, C, xt, xb, g_bc, gb, rings)
    norm_tile_b(k, C, hb, hbb, hT, hTb_tile, t, rings)


def phase_A1(k, cfg, C, hT, hTb, layer):
    nc = k.nc
    with Phase(k, "A1") as ph:
        g_bc = ph.sb([128, D], F32, "g")
        gb = Buf("g")
        k.dma(k.sp, g_bc[:], C.g_attn[layer], writes=[gb])
        rings = norm_rings(ph)
        xr = Ring(ph, 3, [128, D], F32, "x")
        for t in range(cfg.T):
            xt, xb = xr.next()
            k.dma(k.sp, xt[:], C.x_in[t * 128:(t + 1) * 128, :], writes=[xb])
            norm_tile(k, C, xt, xb, g_bc, gb, hT, hTb[t], t, rings)


def phase_A2(k, cfg, C, hT, hTb, layer):
    nc = k.nc
    S, T, TB = cfg.S, cfg.T, cfg.TB
    with Phase(k, "A2") as ph:
        W = C.w_in_even[layer // 2] if layer % 2 == 0 else C.w_in_odd[layer // 2]
        Wv = W.rearrange("(c p) n -> p c n", p=128)
        if layer % 2 == 0:
            blocks = [(0, 512, "fm", ("Q", 0, 0.125)), (512, 512, "fm", ("K", 0, 1.0)),
                      (1024, 512, "tm", ("V", 0)),
                      (1536, 512, "fm", ("Q", 4, 0.125)), (2048, 512, "fm", ("K", 4, 1.0)),
                      (2560, 512, "tm", ("V", 4)),
                      (3072, 512, "fm", ("QI", 0, 1.0)), (3584, 512, "fm", ("QI", 4, 1.0)),
                      (4096, 80, "tm", ("KW", 0))]
        else:
            blocks = [(0, 512, "fm", ("Q", 0, 0.125)), (512, 512, "fm", ("Q", 4, 0.125)),
                      (1024, 512, "fm", ("K", 0, 1.0)), (1536, 512, "fm", ("K", 4, 1.0)),
                      (2048, 512, "tm", ("V", 0)), (2560, 512, "tm", ("V", 4))]
        wst = Ring(ph, 2, [128, KC, 512], F32, "wst")
        wbf = Ring(ph, 2, [128, KC, 512], BF16, "wbf")
        pacc = Ring(ph, 4, [128, 512], F32, "pacc", psum=True)
        ostg = Ring(ph, 2, [128, S], BF16, "ostg")
        vstg = Ring(ph, 3, [128, 512], BF16, "vstg")
        kwstg = Ring(ph, 3, [128, 80], F32, "kwstg")
        flip = 0
        for (c0, ncol, kind, dest) in blocks:
            ws, wsb = wst.next()
            k.dma(k.sp, ws[:, :, 0:ncol], Wv[:, :, c0:c0 + ncol], writes=[wsb])
            wb, wbb = wbf.next()
            k.op(k.pool, lambda: nc.gpsimd.tensor_copy(out=wb[:, :, 0:ncol], in_=ws[:, :, 0:ncol]),
                 reads=[wsb], writes=[wbb])
            if kind == "fm":
                name, cbase, scale = dest
                dst = {"Q": C.QT_d, "K": C.KT_d, "QI": C.QI_d}[name]
                for ci in range(ncol // 128):
                    og, ogb = ostg.next()
                    for tb in range(TB):
                        pa, pab = pacc.next()
                        for kc in range(KC):
                            k.op(k.pe, lambda kc=kc: nc.tensor.matmul(
                                pa[:], lhsT=wb[:, kc, ci * 128:(ci + 1) * 128],
                                rhs=hT[:, kc, tb * 512:(tb + 1) * 512],
                                start=(kc == 0), stop=(kc == KC - 1)),
                                reads=[wbb] + hTb[tb * 4:(tb + 1) * 4], writes=[pab])
                        flip ^= 1
                        if flip:
                            k.op(k.act, lambda: nc.scalar.mul(out=og[:, tb * 512:(tb + 1) * 512], in_=pa[:],
                                                              mul=scale), reads=[pab], writes=[ogb])
                        else:
                            k.op(k.dve, lambda: nc.vector.tensor_scalar(
                                out=og[:, tb * 512:(tb + 1) * 512], in0=pa[:], scalar1=scale, scalar2=None,
                                op0=ALU.mult), reads=[pab], writes=[ogb])
                    k.dma(k.pool, dst[cbase + ci], og[:], reads=[ogb])
            else:
                name, cbase = dest
                for t in range(T):
                    pa, pab = pacc.next()
                    for kc in range(KC):
                        k.op(k.pe, lambda kc=kc: nc.tensor.matmul(
                            pa[:, 0:ncol], lhsT=hT[:, kc, t * 128:(t + 1) * 128], rhs=wb[:, kc, 0:ncol],
                            start=(kc == 0), stop=(kc == KC - 1)),
                            reads=[wbb, hTb[t]], writes=[pab])
                    flip ^= 1
                    if name == "V":
                        vs, vsb = vstg.next()
                        if flip:
                            k.op(k.act, lambda: nc.scalar.copy(out=vs[:], in_=pa[:]), reads=[pab], writes=[vsb])
                        else:
                            k.op(k.dve, lambda: nc.vector.tensor_copy(out=vs[:], in_=pa[:]),
                                 reads=[pab], writes=[vsb])
                        k.dma(k.pool, C.V_d[cbase:cbase + 4, :, t, :].rearrange("c p n -> p c n"),
                              vs[:].rearrange("p (c n) -> p c n", c=4), reads=[vsb])
                    else:
                        vs, vsb = kwstg.next()
                        k.op(k.dve, lambda: nc.vector.tensor_copy(out=vs[:], in_=pa[:, 0:80]),
                             reads=[pab], writes=[vsb])
                        k.dma(k.pool, C.KW_d[:, t, :], vs[:], reads=[vsb])


def phase_tables(k, cfg, C):
    nc = k.nc
    with Phase(k, "T") as ph:
        negc = ph.sb([128, 16], F32, "negc")
        negb = Buf()
        k.op(k.dve, lambda: nc.vector.tensor_scalar(out=negc[:], in0=C.c31[:], scalar1=-1.0, scalar2=None,
                                                    op0=ALU.mult), reads=[C.c31b], writes=[negb])
        caus = ph.sb([128, 1024], F32, "caus")
        cdil = ph.sb([128, 1024], F32, "cdil")
        cb_, db_ = Buf(), Buf()
        k.dma(k.sp, caus[:], C.caus_in[:, :], writes=[cb_])
        k.dma(k.sp, cdil[:], C.cdil_in[:, :], writes=[db_])
        gr = Ring(ph, 2, [128, 1024], F32, "g")
        er = Ring(ph, 2, [128, 1024], F32, "e")
        mnr = Ring(ph, 2, [128, 1024], BF16, "mn")
        mdr = Ring(ph, 2, [128, 1024], BF16, "md")
        for h in range(16):
            g, gb = gr.next()
            k.dma(k.sp, g[:], C.G_in[h], writes=[gb])
            e, eb = er.next()
            k.op(k.act, lambda: nc.scalar.activation(out=e[:], in_=g[:], func=AF.Exp, bias=negc[:, h:h + 1],
                                                     scale=1.0), reads=[gb, negb], writes=[eb])
            mn, mnb = mnr.next()
            k.op(k.dve, lambda: nc.vector.tensor_tensor(out=mn[:], in0=e[:], in1=caus[:], op=ALU.mult),
                 reads=[eb, cb_], writes=[mnb])
            md, mdb = mdr.next()
            k.op(k.dve, lambda: nc.vector.tensor_tensor(out=md[:], in0=e[:], in1=cdil[:], op=ALU.mult),
                 reads=[eb, db_], writes=[mdb])
            k.dma(k.pool, C.Mnear_d[h], mn[:], reads=[mnb])
            k.dma(k.pool, C.Mdil_d[h], md[:], reads=[mdb])


def moba_prepass(k, cfg, C):
    nc = k.nc
    S, T = cfg.S, cfg.T
    NB = S // 256
    with Phase(k, "G") as ph:
        pm = ph.sb([128, 16, 16], F32, "pm")
        o3 = ph.sb([128, 16, 16], F32, "o3")
        pmb, o3b = Buf(), Buf()
        k.dma(k.sp, pm[:], C.pastmask_in[:, :, :], writes=[pmb])
        k.dma(k.sp, o3[:], C.own30k_in[:, :, :], writes=[o3b])
        qr = Ring(ph, 2, [128, S], BF16, "q")
        kr = Ring(ph, 2, [128, S], BF16, "k")
        ksr = Ring(ph, 2, [128, 16], F32, "ks")
        kmr = Ring(ph, 2, [128, 16], BF16, "km")
        qmr = Ring(ph, 2, [128, S], BF16, "qm")
        tpr = Ring(ph, 2, [128, 512], F32, "tp", psum=True)
        gpr = [Ring(ph, 2, [128, 512], F32, "gpa", psum=True), Ring(ph, 2, [128, 512], F32, "gpb", psum=True)]
        gmr = Ring(ph, 3, [128, 2, 16], F32, "gm")
        t8r = Ring(ph, 3, [128, 2, 8], F32, "t8")
        thr = Ring(ph, 3, [128, 2], F32, "th")
        t1r = Ring(ph, 3, [128, 2, 16], F32, "t1")
        mvr = Ring(ph, 3, [128, 128], BF16, "mv")
        for i in range(3):
            k.op(k.dve, lambda i=i: nc.vector.memset(mvr.t[i][:], 0.0), writes=[mvr.b[i]])
        for c in range(4):
            qt, qb = qr.next()
            kt, kb = kr.next()
            k.dma(k.sp, qt[:], C.QT_d[c], writes=[qb])
            k.dma(k.sp, kt[:], C.KT_d[c], writes=[kb])
            ks, ksb = ksr.next()
            km, kmb = kmr.next()
            k.op(k.dve, lambda: nc.vector.memset(ks[:], 0.0), writes=[ksb])
            k.op(k.dve, lambda: nc.vector.tensor_reduce(out=ks[:, 0:NB], in_=kt[:].rearrange("p (n b) -> p n b", b=256),
                                                        axis=AX.X, op=ALU.add), reads=[kb], writes=[ksb])
            k.op(k.dve, lambda: nc.vector.tensor_scalar(out=km[:], in0=ks[:], scalar1=1.0 / 256, scalar2=None,
                                                        op0=ALU.mult), reads=[ksb], writes=[kmb])
            qm, qmb = qmr.next()
            LV = getattr(cfg, "lv", 9)
            for t in range(T):
                if LV < 1:
                    break
                own = t // 2
                gps = [gpr[0].next(), gpr[1].next()]
                for hh in range(2):
                    k.op(k.pe, lambda hh=hh: nc.tensor.matmul(
                        gps[hh][0][:, 0:16], lhsT=qt[hh * 64:(hh + 1) * 64, t * 128:(t + 1) * 128],
                        rhs=km[hh * 64:(hh + 1) * 64, :], start=True, stop=True),
                        reads=[qb, kmb], writes=[gps[hh][1]])
                if LV < 2:
                    continue
                gm, gmb = gmr.next()
                t8, t8b = t8r.next()
                th, thb = thr.next()
                t1, t1b = t1r.next()
                mv, mvb = mvr.next()
                for hh in range(2):
                    k.op(k.dve, lambda hh=hh: nc.vector.tensor_tensor(
                        out=gm[:, hh, :], in0=gps[hh][0][:, 0:16], in1=pm[:, own, :], op=ALU.add),
                        reads=[gps[hh][1], pmb], writes=[gmb])
                for hh in range(2):
                    k.op(k.dve, lambda hh=hh: nc.vector.max(out=t8[:, hh, :], in_=gm[:, hh, :]),
                         reads=[gmb], writes=[t8b])
                k.op(k.dve, lambda: nc.vector.tensor_scalar(out=th[:], in0=t8[:, :, 2], scalar1=-1e29, scalar2=None,
                                                            op0=ALU.max), reads=[t8b], writes=[thb])
                for hh in range(2):
                    k.op(k.dve, lambda hh=hh: nc.vector.tensor_scalar(
                        out=t1[:, hh, :], in0=gm[:, hh, :], scalar1=th[:, hh:hh + 1], scalar2=1.0,
                        op0=ALU.is_ge, op1=ALU.subtract), reads=[gmb, thb], writes=[t1b])
                for hh in range(2):
                    k.op(k.dve, lambda hh=hh: nc.vector.scalar_tensor_tensor(
                        out=mv[:, 64 * hh:64 * hh + 16], in0=t1[:, hh, :], scalar=30000.0, in1=o3[:, own, :],
                        op0=ALU.mult, op1=ALU.add), reads=[t1b, o3b], writes=[mvb])
                if LV < 3:
                    continue
                tp, tpb = tpr.next()
                k.op(k.pe, lambda: nc.tensor.matmul(tp[:, 0:128], lhsT=mv[:], rhs=C.ident[:],
                                                    start=True, stop=True), reads=[mvb], writes=[tpb])
                if LV == 3:
                    continue
                k.op(k.act, lambda: nc.scalar.copy(out=qm[:, t * 128:(t + 1) * 128], in_=tp[:, 0:128]),
                     reads=[tpb], writes=[qmb])
            if LV >= 4:
                k.dma(k.pool, C.QM_d[c], qm[:], reads=[qmb])


def dsa_prepass(k, cfg, C, layer):
    nc = k.nc
    S, T, TB = cfg.S, cfg.T, cfg.TB
    KEEP = min(256, S // 4)
    with Phase(k, "I") as ph:
        kiT = ph.sb([128, S], BF16, "kiT")
        kiTb = [Buf() for _ in range(T)]
        kw = ph.sb([128, T, 80], F32, "kw")
        kwb = Buf()
        k.dma(k.sp, kw[:], C.KW_d[:, :, :], writes=[kwb])
        gk = ph.sb([128, 64], F32, "gk")
        gkb = Buf()
        k.dma(k.sp, gk[:], C.gk_in[layer // 2], writes=[gkb])
        tri = ph.sb([128, 128], F32, "tri")
        trib = Buf()
        k.dma(k.sp, tri[:], C.tri_in[:, :], writes=[trib])
        pw2 = ph.sb([128, NIT], F32, "pw2")
        pw2b = Buf()
        k.dma(k.sp, pw2[:], C.pow2_in[:, :], writes=[pw2b])
        wabs = ph.sb([128, T, 16], F32, "wabs")
        wsgn = ph.sb([128, T, 16], F32, "wsgn")
        wab, wsb_ = Buf(), Buf()
        k.op(k.act, lambda: nc.scalar.activation(out=wabs[:], in_=kw[:, :, 64:80], func=AF.Abs, scale=1.0 / 32),
             reads=[kwb], writes=[wab])
        k.op(k.act, lambda: nc.scalar.activation(out=wsgn[:], in_=kw[:, :, 64:80], func=AF.Sign),
             reads=[kwb], writes=[wsb_])
        ssr = Ring(ph, 4, [128, 4], F32, "ss")
        jkr = Ring(ph, 1, [128, 64], F32, "jk")
        kkr = Ring(ph, 2, [128, 128], BF16, "kk")
        tpr = Ring(ph, 2, [128, 1024], BF16, "tp", psum=True)
        for t in range(T):
            ss, ssb = ssr.next()
            jk, jkb = jkr.next()
            rstd_chain(k, kw[:, t, 0:64], kwb, ss, ssb, jk[:], jkb, 64)
            kk, kkb = kkr.next()
            k.op(k.dve, lambda: nc.vector.scalar_tensor_tensor(out=kk[:, 0:64], in0=kw[:, t, 0:64], scalar=ss[:, 3:4],
                                                               in1=gk[:], op0=ALU.mult, op1=ALU.mult),
                 reads=[kwb, ssb, gkb], writes=[kkb])
            k.op(k.dve, lambda: nc.vector.tensor_copy(out=kk[:, 64:128], in_=kk[:, 0:64]), reads=[kkb], writes=[kkb])
            tp, tpb = tpr.next()
            k.op(k.pe, lambda: nc.tensor.transpose(out=tp[:, 0:128], in_=kk[:], identity=C.ident[:]),
                 reads=[kkb], writes=[tpb])
            k.op(k.act, lambda: nc.scalar.copy(out=kiT[:, t * 128:(t + 1) * 128], in_=tp[:, 0:128]),
                 reads=[tpb], writes=[kiTb[t]])
        qir = Ring(ph, 2, [128, 8, 128], BF16, "qi")
        dsr = Ring(ph, 2, [128, 16, 128], BF16, "ds")
        spr = Ring(ph, 4, [128, 512], F32, "sp", psum=True)
        apr = Ring(ph, 2, [128, 512], F32, "ap", psum=True)
        rr = Ring(ph, 4, [128, 512], BF16, "r")
        scr = Ring(ph, 2, [128, S], F32, "sc")
        cjr = Ring(ph, 1, [128, S], BF16, "cj")
        m01r = Ring(ph, 2, [128, S], BF16, "m01")
        mtsr = Ring(ph, 2, [128, T, 128], BF16, "mts")
        smr = Ring(ph, 2, [128, 8], F32, "sm")
        hwr = Ring(ph, 2, [128, NIT], F32, "hw")
        for i in range(mtsr.n):
            k.op(k.pool, lambda i=i: nc.gpsimd.memset(mtsr.t[i][:], 0.0), writes=[mtsr.b[i]])
        ilag = Lag(3)
        prev_post = None
        for t in range(T):
            L = (t + 1) * 128
            qi, qib = qir.next()
            k.dma(k.sp, qi[:], C.QI_d[:, :, t * 128:(t + 1) * 128].rearrange("c p n -> p c n"), writes=[qib])
            ds, dsb = dsr.next()
            for h in range(16):
                k.op(k.pool, lambda h=h: nc.gpsimd.tensor_scalar(out=ds[:, h, :], in0=C.ident[:],
                                                                 scalar1=wsgn[:, t, h:h + 1], scalar2=None,
                                                                 op0=ALU.mult), reads=[wsb_], writes=[dsb])
            sc, scb = scr.next()
            nkb = (L + 511) // 512
            for kb in range(nkb):
                kw_ = min(512, L - kb * 512)
                ap_, apb = apr.next()
                for h in range(16):
                    c, hh = h // 2, h % 2
                    sp_, spb = spr.next()
                    k.op(k.pe, lambda: nc.tensor.matmul(
                        sp_[:, 0:kw_], lhsT=qi[hh * 64:(hh + 1) * 64, c, :],
                        rhs=kiT[hh * 64:(hh + 1) * 64, kb * 512:kb * 512 + kw_], start=True, stop=True),
                        reads=[qib] + kiTb[kb * 4:kb * 4 + (kw_ // 128)], writes=[spb])
                    r, rb = rr.next()
                    k.op(k.act, lambda: nc.scalar.activation(out=r[:, 0:kw_], in_=sp_[:, 0:kw_], func=AF.Relu,
                                                             scale=wabs[:, t, h:h + 1]),
                         reads=[spb, wab], writes=[rb])

                    def sgn(ap_=ap_, apb=apb, ds=ds, dsb=dsb, r=r, rb=rb, h=h, kw_=kw_):
                        k.op(k.pe, lambda: nc.tensor.matmul(ap_[:, 0:kw_], lhsT=ds[:, h, :], rhs=r[:, 0:kw_],
                                                            start=(h == 0), stop=(h == 15)),
                             reads=[dsb, rb], writes=[apb])
                    ilag.push(sgn)

                def cp(sc=sc, scb=scb, ap_=ap_, apb=apb, kb=kb, kw_=kw_):
                    k.op(k.act, lambda: nc.scalar.copy(out=sc[:, kb * 512:kb * 512 + kw_], in_=ap_[:, 0:kw_]),
                         reads=[apb], writes=[scb])
                ilag.push(cp)
            ilag.flush()
            sm, smb = smr.next()
            hw, hwb = hwr.next()
            k.op(k.dve, lambda: nc.vector.tensor_reduce(out=sm[:, 0:1], in_=sc[:, 0:L], axis=AX.X, op=ALU.max,
                                                        apply_absolute_value=True), reads=[scb], writes=[smb])
            k.op(k.dve, lambda: nc.vector.tensor_tensor(out=sc[:, L - 128:L], in0=sc[:, L - 128:L], in1=tri[:],
                                                        op=ALU.add), reads=[scb, trib], writes=[scb])
            k.op(k.dve, lambda: nc.vector.tensor_scalar(out=sm[:, 1:2], in0=sm[:, 0:1], scalar1=-1.0, scalar2=None,
                                                        op0=ALU.mult), reads=[smb], writes=[smb])
            k.op(k.dve, lambda: nc.vector.tensor_scalar(out=hw[:], in0=pw2[:], scalar1=sm[:, 0:1], scalar2=None,
                                                        op0=ALU.mult), reads=[smb, pw2b], writes=[hwb])
            cj, cjb = cjr.next()
            for it in range(NIT):
                k.op(k.dve, lambda: nc.vector.tensor_tensor(out=sm[:, 2:3], in0=sm[:, 1:2], in1=hw[:, it:it + 1],
                                                            op=ALU.add), reads=[smb, hwb], writes=[smb])
                k.op(k.dve, lambda: nc.vector.tensor_scalar(out=cj[:, 0:L], in0=sc[:, 0:L], scalar1=sm[:, 2:3],
                                                            scalar2=0.0, op0=ALU.is_ge, op1=ALU.add,
                                                            accum_out=sm[:, 3:4]),
                     reads=[scb, smb], writes=[cjb, smb])
                k.op(k.dve, lambda: nc.vector.scalar_tensor_tensor(out=sm[:, 4:5], in0=sm[:, 3:4],
                                                                   scalar=KEEP - 0.5, in1=hw[:, it:it + 1],
                                                                   op0=ALU.is_ge, op1=ALU.mult),
                     reads=[smb, hwb], writes=[smb])
                k.op(k.dve, lambda: nc.vector.tensor_tensor(out=sm[:, 1:2], in0=sm[:, 1:2], in1=sm[:, 4:5],
                                                            op=ALU.add), reads=[smb], writes=[smb])
            m01, m01b = m01r.next()
            k.op(k.dve, lambda: nc.vector.tensor_scalar(out=m01[:, 0:L], in0=sc[:, 0:L], scalar1=sm[:, 1:2],
                                                        scalar2=None, op0=ALU.is_ge), reads=[scb, smb], writes=[m01b])
            def post(t=t, m01=m01, m01b=m01b):
                mts, mtsb = mtsr.next()
                for j0 in range(0, t + 1, 8):
                    nj = min(8, t + 1 - j0)
                    tp, tpb = tpr.next()
                    for jj in range(nj):
                        k.op(k.pe, lambda jj=jj: nc.tensor.transpose(
                            out=tp[:, jj * 128:(jj + 1) * 128], in_=m01[:, (j0 + jj) * 128:(j0 + jj + 1) * 128],
                            identity=C.ident[:]), reads=[m01b], writes=[tpb])
                    k.op(k.act, lambda: nc.scalar.copy(out=mts[:, j0:j0 + nj, :],
                                                       in_=tp[:, 0:nj * 128].rearrange("p (j n) -> p j n", n=128)),
                         reads=[tpb], writes=[mtsb])
                i_q, sub = t // 4, t % 4
                nkt = 4 * i_q + 4
                k.dma(k.pool, C.MK_d[i_q, :, 0:nkt, sub * 128:(sub + 1) * 128], mts[:, 0:nkt, :], reads=[mtsb])
            if prev_post is not None:
                prev_post()
            prev_post = post
        prev_post()


def attention(k, cfg, C, mode, pairs, tag):
    nc = k.nc
    S, T, TB = cfg.S, cfg.T, cfg.TB
    with Phase(k, "B" + tag) as ph:
        qr = Ring(ph, 2, [128, S], BF16, "q")
        kr = Ring(ph, 2, [128, S], BF16, "k")
        ver = Ring(ph, 2, [128, T, 128], BF16, "ve")
        vor = Ring(ph, 2, [128, T, 128], BF16, "vo")
        for i in range(2):
            k.op(k.pool, lambda i=i: nc.gpsimd.memset(ver.t[i][:], 0.0), writes=[ver.b[i]])
            k.op(k.pool, lambda i=i: nc.gpsimd.memset(vor.t[i][:], 0.0), writes=[vor.b[i]])
        mnr = Ring(ph, 2, [128, 2, 1024], BF16, "mn")
        ptr = Ring(ph, 6, [128, 512], BF16, "pt")
        mor = Ring(ph, 2, [128, S], BF16, "mo")
        recr = Ring(ph, 2, [128, 512], F32, "rec")
        psr = Ring(ph, 4, [128, 512], F32, "ps", psum=True)
        por = Ring(ph, 2, [128, 512], F32, "po", psum=True)
        pdr = Ring(ph, 2, [128, 512], F32, "pd", psum=True)
        Mx_d = C.Mdil_d if mode == "dil" else C.Mnear_d
        if mode == "moba":
            oh = ph.sb([128, S], BF16, "oh")
            ohb = Buf()
            k.dma(k.sp, oh[:], C.onehot_in[:, :], writes=[ohb])
            qmr = Ring(ph, 2, [128, S], BF16, "qm")
        if mode == "dsa":
            mkr = Ring(ph, 2, [128, 16, 512], BF16, "mk")
        if mode == "dil":
            cf = ph.sb([128, 2432], BF16, "cf")
            cfb = Buf()
            k.dma(k.sp, cf[:], C.cfar_in[:, :], writes=[cfb])
        lag = Lag(3)
        tile_no = [0]
        for c in pairs:
            qt, qb = qr.next()
            kt, kb = kr.next()
            ve, veb = ver.next()
            vo, vob = vor.next()
            mn, mnb = mnr.next()
            k.dma(k.sp, qt[:], C.QT_d[c], writes=[qb])
            k.dma(k.sp, kt[:], C.KT_d[c], writes=[kb])
            k.dma(k.sp, ve[:, :, 0:64], C.V_d[c, :, :, 0:64], writes=[veb])
            k.dma(k.sp, vo[:, :, 64:128], C.V_d[c, :, :, 64:128], writes=[vob])
            k.dma(k.sp, mn[:], Mx_d[2 * c:2 * c + 2].rearrange("h p n -> p h n"), writes=[mnb])
            qm = qmb = None
            if mode == "moba":
                qm, qmb = qmr.next()
                k.dma(k.sp, qm[:], C.QM_d[c], writes=[qmb])
            mo, mob = mor.next()
            for i in range(TB):
                q0 = i * 512
                jhi = 4 * i + 3
                jlo = max(0, 4 * i - 16) if mode == "dil" else 0
                po, pob = por.next()
                pd, pdb = pdr.next()
                seq = [(j, hh) for j in range(jlo, jhi + 1) for hh in range(2)]
                mk = mkb = None
                for idx, (j, hh) in enumerate(seq):
                    if mode == "dsa" and hh == 0 and (j % 16 == 0):
                        mk, mkb = mkr.next()
                        g1 = min(jhi + 1, j + 16)
                        k.dma(k.sp, mk[:, 0:g1 - j, :], C.MK_d[i, :, j:g1, :], writes=[mkb])
                    rel = q0 - 128 * j
                    near = rel <= 128
                    hd = 2 * c + hh
                    ps, psb = psr.next()
                    k.op(k.pe, lambda: nc.tensor.matmul(
                        ps[:], lhsT=kt[hh * 64:(hh + 1) * 64, j * 128:(j + 1) * 128],
                        rhs=qt[hh * 64:(hh + 1) * 64, q0:q0 + 512], start=True, stop=(mode != "moba")),
                        reads=[kb, qb], writes=[psb])
                    if mode == "moba":
                        k.op(k.pe, lambda: nc.tensor.matmul(
                            ps[:], lhsT=oh[64 * hh:64 * hh + 16, j * 128:(j + 1) * 128],
                            rhs=qm[64 * hh:64 * hh + 16, q0:q0 + 512],
                            start=False, stop=True), reads=[ohb, qmb], writes=[psb])
                    pt, ptb = ptr.next()
                    k.op(k.act, lambda: nc.scalar.activation(out=pt[:], in_=ps[:], func=AF.Exp,
                                                             bias=C.c31[:, hd:hd + 1], scale=1.0),
                         reads=[psb, C.c31b], writes=[ptb])
                    tile_no[0] += 1
                    if tile_no[0] % 2 == 0:
                        ME, mfn = k.pool, nc.gpsimd.tensor_tensor
                    else:
                        ME, mfn = k.dve, nc.vector.tensor_tensor
                    if near:
                        o = rel + 384
                        k.op(ME, lambda: mfn(out=pt[:], in0=pt[:], in1=mn[:, hh, o:o + 512], op=ALU.mult),
                             reads=[ptb, mnb], writes=[ptb])
                    elif mode == "dil":
                        o = rel - 129
                        k.op(ME, lambda: mfn(out=pt[:], in0=pt[:], in1=cf[:, o:o + 512], op=ALU.mult),
                             reads=[ptb, cfb], writes=[ptb])
                    if mode == "dsa":
                        jj = j % 16
                        k.op(ME, lambda: mfn(out=pt[:], in0=pt[:], in1=mk[:, jj, :], op=ALU.mult),
                             reads=[ptb, mkb], writes=[ptb])
                    first, last = idx == 0, idx == len(seq) - 1

                    def pv(pt=pt, ptb=ptb, j=j, hh=hh, first=first, last=last, po=po, pob=pob, pd=pd, pdb=pdb,
                           ve=ve, veb=veb, vo=vo, vob=vob):
                        vv, vvb = (ve, veb) if hh == 0 else (vo, vob)
                        k.op(k.pe, lambda: nc.tensor.matmul(po[:], lhsT=vv[:, j, :], rhs=pt[:], start=first,
                                                            stop=last), reads=[vvb, ptb], writes=[pob])
                        on = C.onesE if hh == 0 else C.onesO
                        k.op(k.pe, lambda: nc.tensor.matmul(pd[:], lhsT=on, rhs=pt[:], start=first, stop=last),
                             reads=[ptb, C.onesb], writes=[pdb])
                    lag.push(pv)

                def fin(po=po, pob=pob, pd=pd, pdb=pdb, mo=mo, mob=mob, q0=q0):
                    rec, recb = recr.next()
                    k.op(k.dve, lambda: nc.vector.reciprocal(out=rec[:], in_=pd[:]), reads=[pdb], writes=[recb])
                    k.op(k.dve, lambda: nc.vector.tensor_tensor(out=mo[:, q0:q0 + 512], in0=po[:], in1=rec[:],
                                                                op=ALU.mult), reads=[pob, recb], writes=[mob])
                lag.push(fin)

            def st(mo=mo, mob=mob, c=c):
                k.dma(k.pool, C.MT_d[c], mo[:], reads=[mob])
            lag.push(st)
        lag.flush()


def phase_C(k, cfg, C, hT, hTb, layer):
    nc = k.nc
    S, T = cfg.S, cfg.T
    with Phase(k, "C") as ph:
        wo = ph.sb([128, KC, D], BF16, "wo")
        wob = Buf()
        wst = Ring(ph, 2, [128, KC, 512], F32, "wst")
        Wv = C.w_o[layer].rearrange("(c p) n -> p c n", p=128)
        for nb in range(2):
            ws, wsb = wst.next()
            k.dma(k.sp, ws[:], Wv[:, :, nb * 512:(nb + 1) * 512], writes=[wsb])
            k.op(k.pool, lambda: nc.gpsimd.tensor_copy(out=wo[:, :, nb * 512:(nb + 1) * 512], in_=ws[:]),
                 reads=[wsb], writes=[wob])
        g_bc = ph.sb([128, D], F32, "g")
        gb = Buf()
        k.dma(k.sp, g_bc[:], C.g_ffn[layer], writes=[gb])
        rings = norm_rings(ph)
        mtr = Ring(ph, 2, [128, KC, 128], BF16, "mt")
        xr = Ring(ph, 3, [128, D], F32, "x")
        xmr = Ring(ph, 2, [128, D], F32, "xm")
        pacc = Ring(ph, 4, [128, 512], F32, "pacc", psum=True)
        xsrc = C.x_in if layer == 0 else C.xs
        pend = None
        for t in range(T):
            mt, mtb = mtr.next()
            k.dma(k.sp, mt[:], C.MT_d[:, :, t * 128:(t + 1) * 128].rearrange("c p n -> p c n"), writes=[mtb])
            xt, xb = xr.next()
            k.dma(k.sp, xt[:], xsrc[t * 128:(t + 1) * 128, :], writes=[xb])
            xm, xmb = xmr.next()
            for nb in range(2):
                pa, pab = pacc.next()
                for c in range(KC):
                    k.op(k.pe, lambda c=c: nc.tensor.matmul(pa[:], lhsT=mt[:, c, :],
                                                            rhs=wo[:, c, nb * 512:(nb + 1) * 512],
                                                            start=(c == 0), stop=(c == KC - 1)),
                         reads=[mtb, wob], writes=[pab])
                k.op(k.dve, lambda: nc.vector.tensor_tensor(out=xm[:, nb * 512:(nb + 1) * 512], in0=pa[:],
                                                            in1=xt[:, nb * 512:(nb + 1) * 512], op=ALU.add),
                     reads=[pab, xb], writes=[xmb])
            k.dma(k.pool, C.xs[t * 128:(t + 1) * 128, :], xm[:], reads=[xmb])
            hb, hbb = norm_tile_a(k, C, xm, xmb, g_bc, gb, rings)
            if pend is not None:
                norm_tile_b(k, C, pend[0], pend[1], hT, hTb[pend[2]], pend[2], rings)
            pend = (hb, hbb, t)
        norm_tile_b(k, C, pend[0], pend[1], hT, hTb[pend[2]], pend[2], rings)


def phase_F1(k, cfg, C, hT, hTb, layer):
    nc = k.nc
    S, T, TB = cfg.S, cfg.T, cfg.TB
    with Phase(k, "F1") as ph:
        cp = ph.sb([128, 4, 44], F32, "cp")
        cpb = Buf()
        k.dma(k.sp, cp[:], C.convp[layer], writes=[cpb])
        Wv = C.w_up[layer].rearrange("(c p) n -> p c n", p=128)
        wst = Ring(ph, 2, [128, KC, 256], F32, "wst")
        wbf = Ring(ph, 2, [128, KC, 256], BF16, "wbf")
        pacc = Ring(ph, 4, [128, 512], F32, "pacc", psum=True)
        ubr = [Ring(ph, 2, [128, 514], F32, "ubv"), Ring(ph, 2, [128, 514], F32, "ubg")]
        car = Ring(ph, 3, [128, 512], F32, "ca")
        cbr = Ring(ph, 3, [128, 512], F32, "cb")
        sgr = Ring(ph, 2, [128, 512], F32, "sg")
        asr = Ring(ph, 2, [128, S], BF16, "as")
        for f in range(NF):
            ws, wsb = wst.next()
            k.dma(k.sp, ws[:, :, 0:128], Wv[:, :, f * 128:(f + 1) * 128], writes=[wsb])
            k.dma(k.sp, ws[:, :, 128:256], Wv[:, :, DFF + f * 128:DFF + (f + 1) * 128], writes=[wsb])
            wb, wbb = wbf.next()
            k.op(k.pool, lambda: nc.gpsimd.tensor_copy(out=wb[:], in_=ws[:]), reads=[wsb], writes=[wbb])
            a_s, asb = asr.next()
            prev = [None, None]
            for tb in range(TB):
                cv = [None, None]
                for half in range(2):
                    col = f if half == 0 else NF + f
                    pa, pab = pacc.next()
                    for kc in range(KC):
                        k.op(k.pe, lambda kc=kc: nc.tensor.matmul(
                            pa[:], lhsT=wb[:, kc, half * 128:(half + 1) * 128],
                            rhs=hT[:, kc, tb * 512:(tb + 1) * 512], start=(kc == 0), stop=(kc == KC - 1)),
                            reads=[wbb] + hTb[tb * 4:(tb + 1) * 4], writes=[pab])
                    ub, ubb = ubr[half].next()
                    if tb == 0:
                        k.op(k.pool, lambda: nc.gpsimd.memset(ub[:, 0:2], 0.0), writes=[ubb])
                    else:
                        pu, pub = prev[half]
                        k.op(k.pool, lambda: nc.gpsimd.tensor_copy(out=ub[:, 0:2], in_=pu[:, 512:514]),
                             reads=[pub], writes=[ubb])
                    k.op(k.act, lambda: nc.scalar.copy(out=ub[:, 2:514], in_=pa[:]), reads=[pab], writes=[ubb])
                    prev[half] = (ub, ubb)
                    r = car if half == 0 else cbr
                    c1, c1b = r.next()
                    k.op(k.dve, lambda: nc.vector.tensor_scalar(out=c1[:], in0=ub[:, 2:514],
                                                                scalar1=cp[:, 2, col:col + 1],
                                                                scalar2=cp[:, 3, col:col + 1],
                                                                op0=ALU.mult, op1=ALU.add),
                         reads=[ubb, cpb], writes=[c1b])
                    c2, c2b = r.next()
                    k.op(k.dve, lambda: nc.vector.scalar_tensor_tensor(out=c2[:], in0=ub[:, 1:513],
                                                                       scalar=cp[:, 1, col:col + 1], in1=c1[:],
                                                                       op0=ALU.mult, op1=ALU.add),
                         reads=[ubb, cpb, c1b], writes=[c2b])
                    c3, c3b = r.next()
                    k.op(k.dve, lambda: nc.vector.scalar_tensor_tensor(out=c3[:], in0=ub[:, 0:512],
                                                                       scalar=cp[:, 0, col:col + 1], in1=c2[:],
                                                                       op0=ALU.mult, op1=ALU.add),
                         reads=[ubb, cpb, c2b], writes=[c3b])
                    cv[half] = (c3, c3b)
                sg, sgb = sgr.next()
                k.op(k.act, lambda: nc.scalar.activation(out=sg[:], in_=cv[1][0][:], func=AF.Silu),
                     reads=[cv[1][1]], writes=[sgb])
                k.op(k.dve, lambda: nc.vector.tensor_tensor(out=a_s[:, tb * 512:(tb + 1) * 512], in0=sg[:],
                                                            in1=cv[0][0][:], op=ALU.mult),
                     reads=[sgb, cv[0][1]], writes=[asb])
            k.dma(k.pool, C.AT_d[f], a_s[:], reads=[asb])


def phase_F2(k, cfg, C, hT, hTb, layer, last):
    nc = k.nc
    S, T = cfg.S, cfg.T
    with Phase(k, "F2") as ph:
        wd = ph.sb([128, NF, D], BF16, "wd")
        wdb = Buf()
        wst = Ring(ph, 2, [128, NF, 128], F32, "wst")
        Wv = C.w_down[layer].rearrange("(f p) n -> p f n", p=128)
        for cb in range(8):
            ws, wsb = wst.next()
            k.dma(k.sp, ws[:], Wv[:, :, cb * 128:(cb + 1) * 128], writes=[wsb])
            k.op(k.pool, lambda: nc.gpsimd.tensor_copy(out=wd[:, :, cb * 128:(cb + 1) * 128], in_=ws[:]),
                 reads=[wsb], writes=[wdb])
        g_bc = ph.sb([128, D], F32, "g")
        gb = Buf()
        k.dma(k.sp, g_bc[:], C.g_fin[:, :] if last else C.g_attn[layer + 1], writes=[gb])
        rings = norm_rings(ph)
        atr = Ring(ph, 2, [128, NF, 128], BF16, "at")
        xr = Ring(ph, 3, [128, D], F32, "x")
        xnr = Ring(ph, 2, [128, D], F32, "xn")
        yor = Ring(ph, 2, [128, D], F32, "yo")
        pacc = Ring(ph, 4, [128, 512], F32, "pacc", psum=True)
        pend = None
        for t in range(T):
            at, atb = atr.next()
            k.dma(k.sp, at[:], C.AT_d[:, :, t * 128:(t + 1) * 128].rearrange("f p n -> p f n"), writes=[atb])
            xt, xb = xr.next()
            k.dma(k.sp, xt[:], C.xs[t * 128:(t + 1) * 128, :], writes=[xb])
            xn, xnb = xnr.next()
            for nb in range(2):
                pa, pab = pacc.next()
                for f in range(NF):
                    k.op(k.pe, lambda f=f: nc.tensor.matmul(pa[:], lhsT=at[:, f, :],
                                                            rhs=wd[:, f, nb * 512:(nb + 1) * 512],
                                                            start=(f == 0), stop=(f == NF - 1)),
                         reads=[atb, wdb], writes=[pab])
                k.op(k.dve, lambda: nc.vector.tensor_tensor(out=xn[:, nb * 512:(nb + 1) * 512], in0=pa[:],
                                                            in1=xt[:, nb * 512:(nb + 1) * 512], op=ALU.add),
                     reads=[pab, xb], writes=[xnb])
            if last:
                junk, junkb = rings["junk"].next()
                ss, ssb = rings["ss"].next()
                rstd_chain(k, xn[:], xnb, ss, ssb, junk[:], junkb, D)
                yo, yob = yor.next()
                k.op(k.dve, lambda: nc.vector.scalar_tensor_tensor(out=yo[:], in0=xn[:], scalar=ss[:, 3:4],
                                                                   in1=g_bc[:], op0=ALU.mult, op1=ALU.mult),
                     reads=[xnb, ssb, gb], writes=[yob])
                k.dma(k.pool, C.out[t * 128:(t + 1) * 128, :], yo[:], reads=[yob])
            else:
                k.dma(k.pool, C.xs[t * 128:(t + 1) * 128, :], xn[:], reads=[xnb])
                hb, hbb = norm_tile_a(k, C, xn, xnb, g_bc, gb, rings)
                if pend is not None:
                    norm_tile_b(k, C, pend[0], pend[1], hT, hTb[pend[2]], pend[2], rings)
                pend = (hb, hbb, t)
        if pend is not None:
            norm_tile_b(k, C, pend[0], pend[1], hT, hTb[pend[2]], pend[2], rings)


def build(cfg):
    S, T, TB = cfg.S, cfg.T, cfg.TB
    nc = bass.Bass("TRN2", target_bir_lowering=False)
    k = K(nc)
    C = NS()

    def din(name, shape, dt=F32):
        return nc.dram_tensor(name, list(shape), dt, kind="ExternalInput").ap()

    def dscr(name, shape, dt):
        kind = "ExternalOutput" if (cfg.debug and name in cfg.debug) else "Internal"
        return nc.dram_tensor(name, list(shape), dt, kind=kind).ap()

    C.x_in = din("x", [S, D])
    C.w_in_even = din("w_in_even", [2, D, 4176])
    C.w_in_odd = din("w_in_odd", [2, D, 3072])
    C.w_o = din("w_o", [4, D, D])
    C.w_up = din("w_up", [4, D, 2 * DFF])
    C.w_down = din("w_down", [4, DFF, D])
    C.g_attn = din("g_attn", [4, 128, D])
    C.g_ffn = din("g_ffn", [4, 128, D])
    C.g_fin = din("g_fin", [128, D])
    C.gk_in = din("gk", [2, 128, 64])
    C.convp = din("convp", [4, 128, 4, 44])
    C.G_in = din("G", [16, 128, 1024])
    c31_in = din("c31", [128, 16])
    ident_in = din("ident", [128, 128], BF16)
    ones_in = din("ones2", [128, 2, 128], BF16)
    C.caus_in = din("caus", [128, 1024])
    C.cdil_in = din("cdil", [128, 1024])
    C.cfar_in = din("cfar", [128, 2432], BF16)
    C.onehot_in = din("onehot", [128, S], BF16)
    C.pastmask_in = din("pastmask", [128, 16, 16])
    C.own30k_in = din("own30k", [128, 16, 16])
    C.tri_in = din("tri", [128, 128])
    C.pow2_in = din("pow2", [128, NIT])

    C.QT_d = dscr("QT_d", [8, 128, S], BF16)
    C.KT_d = dscr("KT_d", [8, 128, S], BF16)
    C.V_d = dscr("V_d", [8, 128, T, 128], BF16)
    C.QI_d = dscr("QI_d", [8, 128, S], BF16)
    C.KW_d = dscr("KW_d", [128, T, 80], F32)
    C.QM_d = dscr("QM_d", [4, 128, S], BF16)
    C.MK_d = dscr("MK_d", [TB, 128, T, 512], BF16)
    C.MT_d = dscr("MT_d", [8, 128, S], BF16)
    C.AT_d = dscr("AT_d", [NF, 128, S], BF16)
    C.xs = dscr("xs", [S, D], F32)
    C.Mnear_d = dscr("Mnear_d", [16, 128, 1024], BF16)
    C.Mdil_d = dscr("Mdil_d", [16, 128, 1024], BF16)
    C.out = nc.dram_tensor("out", [S, D], F32, kind="ExternalOutput").ap()

    stop_after = cfg.stop_after

    with ExitStack() as top:
        C.ident = top.enter_context(nc.sbuf_tensor("ident_sb", [128, 128], BF16))
        C.c31 = top.enter_context(nc.sbuf_tensor("c31_sb", [128, 16], F32))
        ones2 = top.enter_context(nc.sbuf_tensor("ones_sb", [128, 2, 128], BF16))
        C.onesE = ones2[:, 0, :]
        C.onesO = ones2[:, 1, :]
        C.c31b, C.onesb, identb = Buf(), Buf(), Buf()
        k.dma(k.sp, C.ident[:], ident_in[:, :], writes=[identb])
        k.dma(k.sp, C.c31[:], c31_in[:, :], writes=[C.c31b])
        k.dma(k.sp, ones2[:], ones_in[:, :, :], writes=[C.onesb])
        k.barrier()
        phase_tables(k, cfg, C)

        def done(tag):
            return stop_after == tag

        fin = False
        for layer in range(cfg.depth):
            if fin:
                break
            if layer == 0:
                with Phase(k, "H0") as hp:
                    hT = hp.sb([128, KC, S], BF16, "hT")
                    hTb = [Buf() for _ in range(T)]
                    phase_A1(k, cfg, C, hT, hTb, 0)
                    phase_A2(k, cfg, C, hT, hTb, 0)
            if done("A%d" % layer):
                break
            if layer % 2 == 0:
                moba_prepass(k, cfg, C)
                if done("G%d" % layer):
                    break
                attention(k, cfg, C, "moba", [0, 1, 2, 3], "m")
                if done("M%d" % layer):
                    break
                dsa_prepass(k, cfg, C, layer)
                attention(k, cfg, C, "dsa", [4, 5, 6, 7], "d")
            else:
                attention(k, cfg, C, "dil", list(range(8)), "l")
            if done("B%d" % layer):
                break
            with Phase(k, "HC") as hp:
                hT = hp.sb([128, KC, S], BF16, "hT")
                hTb = [Buf() for _ in range(T)]
                phase_C(k, cfg, C, hT, hTb, layer)
                if done("C%d" % layer):
                    fin = True
                else:
                    phase_F1(k, cfg, C, hT, hTb, layer)
            if fin or done("F1%d" % layer):
                break
            last = layer == cfg.depth - 1
            with Phase(k, "HF") as hp:
                hT = hp.sb([128, KC, S], BF16, "hT")
                hTb = [Buf() for _ in range(T)]
                phase_F2(k, cfg, C, hT, hTb, layer, last)
                if not last and not done("F2%d" % layer):
                    phase_A2(k, cfg, C, hT, hTb, layer + 1)
            if done("F2%d" % layer):
                break
        k.final_wait()
    k.stack.close()
    return nc


def host_inputs(cfg, b, x, w_in_even, idx_k_norm, w_in_odd, w_o, rel_bias, attn_norm, ffn_norm,
                w_up, conv_w, conv_b, w_down, final_norm):
    S = cfg.S
    bf = ml_dtypes.bfloat16
    m = {}
    m["x"] = np.ascontiguousarray(x[b], dtype=np.float32)
    m["w_in_even"] = w_in_even
    m["w_in_odd"] = w_in_odd
    m["w_o"] = w_o
    m["w_up"] = w_up
    m["w_down"] = w_down
    m["g_attn"] = np.ascontiguousarray(np.broadcast_to(attn_norm[:, None, :], (4, 128, D)))
    m["g_ffn"] = np.ascontiguousarray(np.broadcast_to(ffn_norm[:, None, :], (4, 128, D)))
    m["g_fin"] = np.ascontiguousarray(np.broadcast_to(final_norm[None, :], (128, D)))
    m["gk"] = np.ascontiguousarray(np.broadcast_to(idx_k_norm[:, None, :], (2, 128, 64)))
    cp = np.zeros((4, 128, 4, 44), np.float32)
    for l in range(4):
        for j in range(3):
            cp[l, :, j, :] = conv_w[l, j].reshape(44, 128).T
        cp[l, :, 3, :] = conv_b[l].reshape(44, 128).T
    m["convp"] = cp
    p = np.arange(128)[:, None]
    j = np.arange(1024)[None, :]
    d = j - 384 - p
    bk = t5_bucket_np(d)
    m["G"] = np.ascontiguousarray(np.transpose(rel_bias[bk, :], (2, 0, 1))).astype(np.float32)
    m["c31"] = np.ascontiguousarray(np.broadcast_to(rel_bias[31][None, :], (128, 16))).astype(np.float32)
    m["ident"] = np.eye(128, dtype=np.float32).astype(bf)
    o2 = np.zeros((128, 2, 128), np.float32)
    o2[:, 0, 0:64] = 1.0
    o2[:, 1, 64:128] = 1.0
    m["ones2"] = o2.astype(bf)
    m["caus"] = (d >= 0).astype(np.float32)

    def cmul(dd):
        return (((dd >= 0) & (dd <= 128)).astype(np.float32)
                + ((dd >= 0) & (dd <= 512) & (dd % 4 == 0)).astype(np.float32)
                + ((dd >= 0) & (dd <= 2048) & (dd % 16 == 0)).astype(np.float32))
    m["cdil"] = cmul(d)
    jf = np.arange(2432)[None, :]
    m["cfar"] = cmul(jf - p + 129).astype(bf)
    kk = np.arange(S)[None, :]
    n128 = np.arange(128)[:, None]
    m["onehot"] = ((kk // 256 == (n128 % 64)) & ((n128 % 64) < 16)).astype(np.float32).astype(bf)
    own = np.arange(16)[:, None]
    nn = np.arange(16)[None, :]
    pmk = np.where(nn < own, 0.0, -1e30).astype(np.float32)
    m["pastmask"] = np.ascontiguousarray(np.broadcast_to(pmk[None], (128, 16, 16)))
    o3 = np.where(nn == own, 30000.0, 0.0).astype(np.float32)
    m["own30k"] = np.ascontiguousarray(np.broadcast_to(o3[None], (128, 16, 16)))
    q = np.arange(128)[:, None]
    kx = np.arange(128)[None, :]
    m["tri"] = np.where(kx <= q, 0.0, -1e30).astype(np.float32)
    m["pow2"] = np.ascontiguousarray(np.broadcast_to((2.0 ** -np.arange(NIT))[None, :], (128, NIT))).astype(np.float32)
    return m


_CACHE = {}


def kernel(**inputs):
    inputs = {k_: np.asarray(v) for k_, v in inputs.items()}
    x = inputs["x"]
    B, S, _ = x.shape
    cfg = Cfg(S=S, depth=4)
    if "nc" not in _CACHE:
        _CACHE["nc"] = build(cfg)
    nc = _CACHE["nc"]
    in_maps = []
    for core in range(8):
        in_maps.append(host_inputs(cfg, core // 2, **inputs))
    res = run_bass_kernel_spmd(nc, in_maps, core_ids=list(range(8)))
    outs = [res.results[2 * b]["out"] for b in range(B)]
    return np.stack(outs, axis=0).astype(np.float32)
```

```python
import math
from contextlib import ExitStack

import numpy as np
import ml_dtypes
import concourse.bass as bass
import concourse.mybir as mybir
from concourse.bass_utils import run_bass_kernel_spmd

F32 = mybir.dt.float32
BF16 = mybir.dt.bfloat16
AF = mybir.ActivationFunctionType
ALU = mybir.AluOpType
AX = mybir.AxisListType

D = 1024
KC = 8
DFF = 2816
NF = 22
NEG = -30000.0
SAME_ENGINE_SYNC = True


class Buf:
    __slots__ = ("name", "w", "r")

    def __init__(self, name=""):
        self.name = name
        self.w = {}
        self.r = {}


class Eng:
    def __init__(self, K, name, e, is_pe=False):
        self.K = K
        self.name = name
        self.e = e
        self.is_pe = is_pe
        self.sem = K.new_sem("pg_" + name)
        self.n = 0
        self.waited = {}

    def wait(self, sem, val):
        key = id(sem)
        if self.waited.get(key, 0) >= val:
            return
        self.waited[key] = val
        self.e.wait_ge(sem, val)


class K:
    def __init__(self, nc):
        self.nc = nc
        self.stack = ExitStack()
        self.sems = []
        self.pe = Eng(self, "pe", nc.tensor, is_pe=True)
        self.act = Eng(self, "act", nc.scalar)
        self.dve = Eng(self, "dve", nc.vector)
        self.pool = Eng(self, "pool", nc.gpsimd)
        self.sp = Eng(self, "sp", nc.sync)
        self.engs = [self.pe, self.act, self.dve, self.pool, self.sp]
        self.dq = {}
        for q in (self.sp, self.pool, self.act):
            self.dq[q.name] = dict(sems=[self.new_sem("dq_%s%d" % (q.name, i)) for i in range(8)],
                                   tot=[0] * 8, i=0)

    def new_sem(self, name):
        s = self.stack.enter_context(self.nc.semaphore(name))
        self.sems.append(s)
        return s

    def _deps(self, X, reads, writes, acc=False):
        for b in reads:
            for sem, val in b.w.values():
                if (sem is X.sem) and (X.is_pe or not SAME_ENGINE_SYNC):
                    continue
                X.wait(sem, val)
        for b in writes:
            for sem, val in list(b.w.values()) + list(b.r.values()):
                if (sem is X.sem) and (X.is_pe or not SAME_ENGINE_SYNC):
                    continue
                X.wait(sem, val)

    def _mark(self, ev, reads, writes):
        sem, val = ev
        for b in reads:
            b.r[id(sem)] = ev
        for b in writes:
            b.w = {id(sem): ev}
            b.r = {}

    def op(self, X, ins_fn, reads=(), writes=()):
        self._deps(X, reads, writes)
        ins = ins_fn()
        ins.then_inc(X.sem, 1)
        X.n += 1
        self._mark((X.sem, X.n), reads, writes)
        return ins

    def dma(self, Q, out, in_, reads=(), writes=(), **kw):
        self._deps(Q, reads, writes)
        dq = self.dq[Q.name]
        i = dq["i"]
        dq["i"] = (i + 1) % len(dq["sems"])
        sem = dq["sems"][i]
        if dq["tot"][i] > 0:
            Q.wait(sem, dq["tot"][i])
        Q.e.dma_start(out=out, in_=in_, **kw).then_inc(sem, 16)
        dq["tot"][i] += 16
        self._mark((sem, dq["tot"][i]), reads, writes)

    def barrier(self):
        for X in self.engs:
            for Y in self.engs:
                if Y is not X and Y.n > 0:
                    X.wait(Y.sem, Y.n)
            for dq in self.dq.values():
                for sem, tot in zip(dq["sems"], dq["tot"]):
                    if tot > 0:
                        X.wait(sem, tot)

    def final_wait(self):
        X = self.sp
        for Y in self.engs:
            if Y is not X and Y.n > 0:
                X.wait(Y.sem, Y.n)
        for dq in self.dq.values():
            for sem, tot in zip(dq["sems"], dq["tot"]):
                if tot > 0:
                    X.wait(sem, tot)


class Phase:
    _uid = [0]

    def __init__(self, k, name):
        self.k = k
        Phase._uid[0] += 1
        self.name = "%s%d" % (name, Phase._uid[0])
        self.stack = ExitStack()
        self.cnt = 0

    def __enter__(self):
        return self

    def __exit__(self, *a):
        self.k.barrier()
        self.stack.close()
        return False

    def sb(self, shape, dt, name=None):
        self.cnt += 1
        t = self.stack.enter_context(
            self.k.nc.sbuf_tensor("%s_%s%d" % (self.name, name or "t", self.cnt), list(shape), dt))
        return t

    def ps(self, shape, dt, name=None):
        self.cnt += 1
        t = self.stack.enter_context(
            self.k.nc.psum_tensor("%s_%s%d" % (self.name, name or "p", self.cnt), list(shape), dt))
        return t


class Ring:
    def __init__(self, ph, n, shape, dt, name, psum=False):
        self.t = [(ph.ps if psum else ph.sb)(shape, dt, name) for _ in range(n)]
        self.b = [Buf("%s%d" % (name, i)) for i in range(n)]
        self.i = -1
        self.n = n

    def next(self):
        self.i = (self.i + 1) % self.n
        return self.t[self.i], self.b[self.i]


def t5_bucket_np(dist):
    n = np.maximum(dist, 0)
    max_exact = 16
    nf = np.maximum(n, max_exact).astype(np.float32)
    large = max_exact + (np.log(nf / max_exact) / math.log(128 / max_exact) * (32 - max_exact)).astype(np.int32)
    large = np.minimum(large, 31)
    return np.where(n < max_exact, n, large)


class Cfg:
    def __init__(self, S=4096, depth=4, debug=None, stop_after=None):
        self.S = S
        self.T = S // 128
        self.TB = S // 512
        self.depth = depth
        self.debug = debug
        self.stop_after = stop_after


NIT = 18


class NS:
    pass


class Lag:
    def __init__(self, n):
        self.q = []
        self.n = n

    def push(self, fn):
        self.q.append(fn)
        while len(self.q) > self.n:
            self.q.pop(0)()

    def flush(self):
        while self.q:
            self.q.pop(0)()


def norm_rings(ph):
    return dict(
        junk=Ring(ph, 1, [128, D], BF16, "junk"),
        ss=Ring(ph, 4, [128, 4], F32, "ss"),
        hb=Ring(ph, 2, [128, D], BF16, "hb"),
        pT=Ring(ph, 2, [128, D], BF16, "pT", psum=True),
    )


def rstd_chain(k, xt, xb, ss, ssb, junk, junkb, n):
    nc = k.nc
    k.op(k.act, lambda: nc.scalar.activation(out=junk, in_=xt, func=AF.Square, accum_out=ss[:, 0:1]),
         reads=[xb], writes=[junkb, ssb])
    k.op(k.dve, lambda: nc.vector.tensor_scalar(out=ss[:, 1:2], in0=ss[:, 0:1], scalar1=1.0 / n, scalar2=1e-6,
                                                op0=ALU.mult, op1=ALU.add), reads=[ssb], writes=[ssb])
    k.op(k.act, lambda: nc.scalar.activation(out=ss[:, 2:3], in_=ss[:, 1:2], func=AF.Sqrt),
         reads=[ssb], writes=[ssb])
    k.op(k.dve, lambda: nc.vector.reciprocal(out=ss[:, 3:4], in_=ss[:, 2:3]), reads=[ssb], writes=[ssb])


def norm_tile_a(k, C, xt, xb, g_bc, gb, rings):
    nc = k.nc
    junk, junkb = rings["junk"].next()
    ss, ssb = rings["ss"].next()
    rstd_chain(k, xt[:], xb, ss, ssb, junk[:], junkb, D)
    hb, hbb = rings["hb"].next()
    k.op(k.dve, lambda: nc.vector.scalar_tensor_tensor(out=hb[:], in0=xt[:], scalar=ss[:, 3:4], in1=g_bc[:],
                                                       op0=ALU.mult, op1=ALU.mult),
         reads=[xb, ssb, gb], writes=[hbb])
    return hb, hbb


def norm_tile_b(k, C, hb, hbb, hT, hTb_tile, t, rings):
    nc = k.nc
    pT, pTb = rings["pT"].next()
    for kc in range(KC):
        k.op(k.pe, lambda kc=kc: nc.tensor.transpose(out=pT[:, kc * 128:(kc + 1) * 128],
                                                     in_=hb[:, kc * 128:(kc + 1) * 128], identity=C.ident[:]),
             reads=[hbb], writes=[pTb])
    k.op(k.act, lambda: nc.scalar.copy(out=hT[:, :, t * 128:(t + 1) * 128],
                                       in_=pT[:].rearrange("p (c n) -> p c n", c=KC)),
         reads=[pTb], writes=[hTb_tile])


def norm_tile(k, C, xt, xb, g_bc, gb, hT, hTb_tile, t, rings):
    hb, hbb = norm_tile_a(k, C, xt, xb, g_bc, gb, rings)
    norm_tile_b(k, C, hb, hbb, hT, hTb_tile, t, rings)


def phase_A1(k, cfg, C, hT, hTb, layer):
    nc = k.nc
    with Phase(k, "A1") as ph:
        g_bc = ph.sb([128, D], F32, "g")
        gb = Buf("g")
        k.dma(k.sp, g_bc[:], C.g_attn[layer], writes=[gb])
        rings = norm_rings(ph)
        xr = Ring(ph, 3, [128, D], F32, "x")
        for t in range(cfg.T):
            xt, xb = xr.next()
            k.dma(k.sp, xt[:], C.x_in[t * 128:(t + 1) * 128, :], writes=[xb])
            norm_tile(k, C, xt, xb, g_bc, gb, hT, hTb[t], t, rings)


def phase_A2(k, cfg, C, hT, hTb, layer):
    nc = k.nc
    S, T, TB = cfg.S, cfg.T, cfg.TB
    with Phase(k, "A2") as ph:
        W = C.w_in_even[layer // 2] if layer % 2 == 0 else C.w_in_odd[layer // 2]
        Wv = W.rearrange("(c p) n -> p c n", p=128)
        if layer % 2 == 0:
            blocks = [(0, 512, "fm", ("Q", 0, 0.125)), (512, 512, "fm", ("K", 0, 1.0)),
                      (1024, 512, "tm", ("V", 0)),
                      (1536, 512, "fm", ("Q", 4, 0.125)), (2048, 512, "fm", ("K", 4, 1.0)),
                      (2560, 512, "tm", ("V", 4)),
                      (3072, 512, "fm", ("QI", 0, 1.0)), (3584, 512, "fm", ("QI", 4, 1.0)),
                      (4096, 80, "tm", ("KW", 0))]
        else:
            blocks = [(0, 512, "fm", ("Q", 0, 0.125)), (512, 512, "fm", ("Q", 4, 0.125)),
                      (1024, 512, "fm", ("K", 0, 1.0)), (1536, 512, "fm", ("K", 4, 1.0)),
                      (2048, 512, "tm", ("V", 0)), (2560, 512, "tm", ("V", 4))]
        wst = Ring(ph, 2, [128, KC, 512], F32, "wst")
        wbf = Ring(ph, 2, [128, KC, 512], BF16, "wbf")
        pacc = Ring(ph, 4, [128, 512], F32, "pacc", psum=True)
        ostg = Ring(ph, 2, [128, S], BF16, "ostg")
        vstg = Ring(ph, 3, [128, 512], BF16, "vstg")
        kwstg = Ring(ph, 3, [128, 80], F32, "kwstg")
        flip = 0
        for (c0, ncol, kind, dest) in blocks:
            ws, wsb = wst.next()
            k.dma(k.sp, ws[:, :, 0:ncol], Wv[:, :, c0:c0 + ncol], writes=[wsb])
            wb, wbb = wbf.next()
            k.op(k.pool, lambda: nc.gpsimd.tensor_copy(out=wb[:, :, 0:ncol], in_=ws[:, :, 0:ncol]),
                 reads=[wsb], writes=[wbb])
            if kind == "fm":
                name, cbase, scale = dest
                dst = {"Q": C.QT_d, "K": C.KT_d, "QI": C.QI_d}[name]
                for ci in range(ncol // 128):
                    og, ogb = ostg.next()
                    for tb in range(TB):
                        pa, pab = pacc.next()
                        for kc in range(KC):
                            k.op(k.pe, lambda kc=kc: nc.tensor.matmul(
                                pa[:], lhsT=wb[:, kc, ci * 128:(ci + 1) * 128],
                                rhs=hT[:, kc, tb * 512:(tb + 1) * 512],
                                start=(kc == 0), stop=(kc == KC - 1)),
                                reads=[wbb] + hTb[tb * 4:(tb + 1) * 4], writes=[pab])
                        flip ^= 1
                        if flip:
                            k.op(k.act, lambda: nc.scalar.mul(out=og[:, tb * 512:(tb + 1) * 512], in_=pa[:],
                                                              mul=scale), reads=[pab], writes=[ogb])
                        else:
                            k.op(k.dve, lambda: nc.vector.tensor_scalar(
                                out=og[:, tb * 512:(tb + 1) * 512], in0=pa[:], scalar1=scale, scalar2=None,
                                op0=ALU.mult), reads=[pab], writes=[ogb])
                    k.dma(k.pool, dst[cbase + ci], og[:], reads=[ogb])
            else:
                name, cbase = dest
                for t in range(T):
                    pa, pab = pacc.next()
                    for kc in range(KC):
                        k.op(k.pe, lambda kc=kc: nc.tensor.matmul(
                            pa[:, 0:ncol], lhsT=hT[:, kc, t * 128:(t + 1) * 128], rhs=wb[:, kc, 0:ncol],
                            start=(kc == 0), stop=(kc == KC - 1)),
                            reads=[wbb, hTb[t]], writes=[pab])
                    flip ^= 1
                    if name == "V":
                        vs, vsb = vstg.next()
                        if flip:
                            k.op(k.act, lambda: nc.scalar.copy(out=vs[:], in_=pa[:]), reads=[pab], writes=[vsb])
                        else:
                            k.op(k.dve, lambda: nc.vector.tensor_copy(out=vs[:], in_=pa[:]),
                                 reads=[pab], writes=[vsb])
                        k.dma(k.pool, C.V_d[cbase:cbase + 4, :, t, :].rearrange("c p n -> p c n"),
                              vs[:].rearrange("p (c n) -> p c n", c=4), reads=[vsb])
                    else:
                        vs, vsb = kwstg.next()
                        k.op(k.dve, lambda: nc.vector.tensor_copy(out=vs[:], in_=pa[:, 0:80]),
                             reads=[pab], writes=[vsb])
                        k.dma(k.pool, C.KW_d[:, t, :], vs[:], reads=[vsb])


def phase_tables(k, cfg, C):
    nc = k.nc
    with Phase(k, "T") as ph:
        negc = ph.sb([128, 16], F32, "negc")
        negb = Buf()
        k.op(k.dve, lambda: nc.vector.tensor_scalar(out=negc[:], in0=C.c31[:], scalar1=-1.0, scalar2=None,
                                                    op0=ALU.mult), reads=[C.c31b], writes=[negb])
        caus = ph.sb([128, 1024], F32, "caus")
        cdil = ph.sb([128, 1024], F32, "cdil")
        cb_, db_ = Buf(), Buf()
        k.dma(k.sp, caus[:], C.caus_in[:, :], writes=[cb_])
        k.dma(k.sp, cdil[:], C.cdil_in[:, :], writes=[db_])
        gr = Ring(ph, 2, [128, 1024], F32, "g")
        er = Ring(ph, 2, [128, 1024], F32, "e")
        mnr = Ring(ph, 2, [128, 1024], BF16, "mn")
        mdr = Ring(ph, 2, [128, 1024], BF16, "md")
        for h in range(16):
            g, gb = gr.next()
            k.dma(k.sp, g[:], C.G_in[h], writes=[gb])
            e, eb = er.next()
            k.op(k.act, lambda: nc.scalar.activation(out=e[:], in_=g[:], func=AF.Exp, bias=negc[:, h:h + 1],
                                                     scale=1.0), reads=[gb, negb], writes=[eb])
            mn, mnb = mnr.next()
            k.op(k.dve, lambda: nc.vector.tensor_tensor(out=mn[:], in0=e[:], in1=caus[:], op=ALU.mult),
                 reads=[eb, cb_], writes=[mnb])
            md, mdb = mdr.next()
            k.op(k.dve, lambda: nc.vector.tensor_tensor(out=md[:], in0=e[:], in1=cdil[:], op=ALU.mult),
                 reads=[eb, db_], writes=[mdb])
            k.dma(k.pool, C.Mnear_d[h], mn[:], reads=[mnb])
            k.dma(k.pool, C.Mdil_d[h], md[:], reads=[mdb])


def moba_prepass(k, cfg, C):
    nc = k.nc
    S, T = cfg.S, cfg.T
    NB = S // 256
    with Phase(k, "G") as ph:
        pm = ph.sb([128, 16, 16], F32, "pm")
        o3 = ph.sb([128, 16, 16], F32, "o3")
        pmb, o3b = Buf(), Buf()
        k.dma(k.sp, pm[:], C.pastmask_in[:, :, :], writes=[pmb])
        k.dma(k.sp, o3[:], C.own30k_in[:, :, :], writes=[o3b])
        qr = Ring(ph, 2, [128, S], BF16, "q")
        kr = Ring(ph, 2, [128, S], BF16, "k")
        ksr = Ring(ph, 2, [128, 16], F32, "ks")
        kmr = Ring(ph, 2, [128, 16], BF16, "km")
        qmr = Ring(ph, 2, [128, S], BF16, "qm")
        tpr = Ring(ph, 2, [128, 512], F32, "tp", psum=True)
        gpr = [Ring(ph, 2, [128, 512], F32, "gpa", psum=True), Ring(ph, 2, [128, 512], F32, "gpb", psum=True)]
        gmr = Ring(ph, 3, [128, 2, 16], F32, "gm")
        t8r = Ring(ph, 3, [128, 2, 8], F32, "t8")
        thr = Ring(ph, 3, [128, 2], F32, "th")
        t1r = Ring(ph, 3, [128, 2, 16], F32, "t1")
        mvr = Ring(ph, 3, [128, 128], BF16, "mv")
        for i in range(3):
            k.op(k.dve, lambda i=i: nc.vector.memset(mvr.t[i][:], 0.0), writes=[mvr.b[i]])
        for c in range(4):
            qt, qb = qr.next()
            kt, kb = kr.next()
            k.dma(k.sp, qt[:], C.QT_d[c], writes=[qb])
            k.dma(k.sp, kt[:], C.KT_d[c], writes=[kb])
            ks, ksb = ksr.next()
            km, kmb = kmr.next()
            k.op(k.dve, lambda: nc.vector.memset(ks[:], 0.0), writes=[ksb])
            k.op(k.dve, lambda: nc.vector.tensor_reduce(out=ks[:, 0:NB], in_=kt[:].rearrange("p (n b) -> p n b", b=256),
                                                        axis=AX.X, op=ALU.add), reads=[kb], writes=[ksb])
            k.op(k.dve, lambda: nc.vector.tensor_scalar(out=km[:], in0=ks[:], scalar1=1.0 / 256, scalar2=None,
                                                        op0=ALU.mult), reads=[ksb], writes=[kmb])
            qm, qmb = qmr.next()
            LV = getattr(cfg, "lv", 9)
            gpend = None
            for t in range(T):
                if LV < 1:
                    break
                own = t // 2
                gps = [gpr[0].next(), gpr[1].next()]
                for hh in range(2):
                    k.op(k.pe, lambda hh=hh: nc.tensor.matmul(
                        gps[hh][0][:, 0:16], lhsT=qt[hh * 64:(hh + 1) * 64, t * 128:(t + 1) * 128],
                        rhs=km[hh * 64:(hh + 1) * 64, :], start=True, stop=True),
                        reads=[qb, kmb], writes=[gps[hh][1]])
                if LV < 2:
                    continue
                gm, gmb = gmr.next()
                t8, t8b = t8r.next()
                th, thb = thr.next()
                t1, t1b = t1r.next()
                mv, mvb = mvr.next()
                for hh in range(2):
                    k.op(k.dve, lambda hh=hh: nc.vector.tensor_tensor(
                        out=gm[:, hh, :], in0=gps[hh][0][:, 0:16], in1=pm[:, own, :], op=ALU.add),
                        reads=[gps[hh][1], pmb], writes=[gmb])
                for hh in range(2):
                    k.op(k.dve, lambda hh=hh: nc.vector.max(out=t8[:, hh, :], in_=gm[:, hh, :]),
                         reads=[gmb], writes=[t8b])
                k.op(k.dve, lambda: nc.vector.tensor_scalar(out=th[:], in0=t8[:, :, 2], scalar1=-1e29, scalar2=None,
                                                            op0=ALU.max), reads=[t8b], writes=[thb])
                for hh in range(2):
                    k.op(k.dve, lambda hh=hh: nc.vector.tensor_scalar(
                        out=t1[:, hh, :], in0=gm[:, hh, :], scalar1=th[:, hh:hh + 1], scalar2=1.0,
                        op0=ALU.is_ge, op1=ALU.subtract), reads=[gmb, thb], writes=[t1b])
                for hh in range(2):
                    k.op(k.dve, lambda hh=hh: nc.vector.scalar_tensor_tensor(
                        out=mv[:, 64 * hh:64 * hh + 16], in0=t1[:, hh, :], scalar=30000.0, in1=o3[:, own, :],
                        op0=ALU.mult, op1=ALU.add), reads=[t1b, o3b], writes=[mvb])
                def post(mv=mv, mvb=mvb, qm=qm, qmb=qmb, t=t):
                    tp, tpb = tpr.next()
                    k.op(k.pe, lambda: nc.tensor.matmul(tp[:, 0:128], lhsT=mv[:], rhs=C.ident[:],
                                                        start=True, stop=True), reads=[mvb], writes=[tpb])
                    k.op(k.act, lambda: nc.scalar.copy(out=qm[:, t * 128:(t + 1) * 128], in_=tp[:, 0:128]),
                         reads=[tpb], writes=[qmb])
                if gpend is not None:
                    gpend()
                gpend = post
            if gpend is not None:
                gpend()
                gpend = None
            k.dma(k.pool, C.QM_d[c], qm[:], reads=[qmb])


def dsa_prepass(k, cfg, C, layer):
    nc = k.nc
    S, T, TB = cfg.S, cfg.T, cfg.TB
    KEEP = min(256, S // 4)
    with Phase(k, "I") as ph:
        kiT = ph.sb([128, S], BF16, "kiT")
        kiTb = [Buf() for _ in range(T)]
        kw = ph.sb([128, T, 80], F32, "kw")
        kwb = Buf()
        k.dma(k.sp, kw[:], C.KW_d[:, :, :], writes=[kwb])
        gk = ph.sb([128, 64], F32, "gk")
        gkb = Buf()
        k.dma(k.sp, gk[:], C.gk_in[layer // 2], writes=[gkb])
        tri = ph.sb([128, 128], F32, "tri")
        trib = Buf()
        k.dma(k.sp, tri[:], C.tri_in[:, :], writes=[trib])
        pw2 = ph.sb([128, NIT], F32, "pw2")
        pw2b = Buf()
        k.dma(k.sp, pw2[:], C.pow2_in[:, :], writes=[pw2b])
        wabs = ph.sb([128, T, 16], F32, "wabs")
        wsgn = ph.sb([128, T, 16], F32, "wsgn")
        wab, wsb_ = Buf(), Buf()
        k.op(k.act, lambda: nc.scalar.activation(out=wabs[:], in_=kw[:, :, 64:80], func=AF.Abs, scale=1.0 / 32),
             reads=[kwb], writes=[wab])
        k.op(k.act, lambda: nc.scalar.activation(out=wsgn[:], in_=kw[:, :, 64:80], func=AF.Sign),
             reads=[kwb], writes=[wsb_])
        ssr = Ring(ph, 4, [128, 4], F32, "ss")
        jkr = Ring(ph, 1, [128, 64], F32, "jk")
        kkr = Ring(ph, 2, [128, 128], BF16, "kk")
        tpr = Ring(ph, 2, [128, 1024], BF16, "tp", psum=True)
        for t in range(T):
            ss, ssb = ssr.next()
            jk, jkb = jkr.next()
            rstd_chain(k, kw[:, t, 0:64], kwb, ss, ssb, jk[:], jkb, 64)
            kk, kkb = kkr.next()
            k.op(k.dve, lambda: nc.vector.scalar_tensor_tensor(out=kk[:, 0:64], in0=kw[:, t, 0:64], scalar=ss[:, 3:4],
                                                               in1=gk[:], op0=ALU.mult, op1=ALU.mult),
                 reads=[kwb, ssb, gkb], writes=[kkb])
            k.op(k.dve, lambda: nc.vector.tensor_copy(out=kk[:, 64:128], in_=kk[:, 0:64]), reads=[kkb], writes=[kkb])
            tp, tpb = tpr.next()
            k.op(k.pe, lambda: nc.tensor.transpose(out=tp[:, 0:128], in_=kk[:], identity=C.ident[:]),
                 reads=[kkb], writes=[tpb])
            k.op(k.act, lambda: nc.scalar.copy(out=kiT[:, t * 128:(t + 1) * 128], in_=tp[:, 0:128]),
                 reads=[tpb], writes=[kiTb[t]])
        qir = Ring(ph, 2, [128, 8, 128], BF16, "qi")
        dsr = Ring(ph, 2, [128, 16, 128], BF16, "ds")
        spr = Ring(ph, 4, [128, 512], F32, "sp", psum=True)
        apr = Ring(ph, 2, [128, 512], F32, "ap", psum=True)
        rr = Ring(ph, 4, [128, 512], BF16, "r")
        scr = Ring(ph, 2, [128, S], F32, "sc")
        cjr = Ring(ph, 1, [128, S], BF16, "cj")
        m01r = Ring(ph, 2, [128, S], BF16, "m01")
        mtsr = Ring(ph, 2, [128, T, 128], BF16, "mts")
        smr = Ring(ph, 2, [128, 8], F32, "sm")
        hwr = Ring(ph, 2, [128, NIT], F32, "hw")
        for i in range(mtsr.n):
            k.op(k.pool, lambda i=i: nc.gpsimd.memset(mtsr.t[i][:], 0.0), writes=[mtsr.b[i]])
        ilag = Lag(3)
        prev_post = None
        for t in range(T):
            L = (t + 1) * 128
            qi, qib = qir.next()
            k.dma(k.sp, qi[:], C.QI_d[:, :, t * 128:(t + 1) * 128].rearrange("c p n -> p c n"), writes=[qib])
            ds, dsb = dsr.next()
            for h in range(16):
                k.op(k.pool, lambda h=h: nc.gpsimd.tensor_scalar(out=ds[:, h, :], in0=C.ident[:],
                                                                 scalar1=wsgn[:, t, h:h + 1], scalar2=None,
                                                                 op0=ALU.mult), reads=[wsb_], writes=[dsb])
            sc, scb = scr.next()
            nkb = (L + 511) // 512
            for kb in range(nkb):
                kw_ = min(512, L - kb * 512)
                ap_, apb = apr.next()
                for h in range(16):
                    c, hh = h // 2, h % 2
                    sp_, spb = spr.next()
                    k.op(k.pe, lambda: nc.tensor.matmul(
                        sp_[:, 0:kw_], lhsT=qi[hh * 64:(hh + 1) * 64, c, :],
                        rhs=kiT[hh * 64:(hh + 1) * 64, kb * 512:kb * 512 + kw_], start=True, stop=True),
                        reads=[qib] + kiTb[kb * 4:kb * 4 + (kw_ // 128)], writes=[spb])
                    r, rb = rr.next()
                    k.op(k.act, lambda: nc.scalar.activation(out=r[:, 0:kw_], in_=sp_[:, 0:kw_], func=AF.Relu,
                                                             scale=wabs[:, t, h:h + 1]),
                         reads=[spb, wab], writes=[rb])

                    def sgn(ap_=ap_, apb=apb, ds=ds, dsb=dsb, r=r, rb=rb, h=h, kw_=kw_):
                        k.op(k.pe, lambda: nc.tensor.matmul(ap_[:, 0:kw_], lhsT=ds[:, h, :], rhs=r[:, 0:kw_],
                                                            start=(h == 0), stop=(h == 15)),
                             reads=[dsb, rb], writes=[apb])
                    ilag.push(sgn)

                def cp(sc=sc, scb=scb, ap_=ap_, apb=apb, kb=kb, kw_=kw_):
                    k.op(k.act, lambda: nc.scalar.copy(out=sc[:, kb * 512:kb * 512 + kw_], in_=ap_[:, 0:kw_]),
                         reads=[apb], writes=[scb])
                ilag.push(cp)
            ilag.flush()
            sm, smb = smr.next()
            hw, hwb = hwr.next()
            k.op(k.dve, lambda: nc.vector.tensor_reduce(out=sm[:, 0:1], in_=sc[:, 0:L], axis=AX.X, op=ALU.max,
                                                        apply_absolute_value=True), reads=[scb], writes=[smb])
            k.op(k.dve, lambda: nc.vector.tensor_tensor(out=sc[:, L - 128:L], in0=sc[:, L - 128:L], in1=tri[:],
                                                        op=ALU.add), reads=[scb, trib], writes=[scb])
            k.op(k.dve, lambda: nc.vector.tensor_scalar(out=sm[:, 1:2], in0=sm[:, 0:1], scalar1=-1.0, scalar2=None,
                                                        op0=ALU.mult), reads=[smb], writes=[smb])
            k.op(k.dve, lambda: nc.vector.tensor_scalar(out=hw[:], in0=pw2[:], scalar1=sm[:, 0:1], scalar2=None,
                                                        op0=ALU.mult), reads=[smb, pw2b], writes=[hwb])
            cj, cjb = cjr.next()
            for it in range(NIT):
                k.op(k.dve, lambda: nc.vector.tensor_tensor(out=sm[:, 2:3], in0=sm[:, 1:2], in1=hw[:, it:it + 1],
                                                            op=ALU.add), reads=[smb, hwb], writes=[smb])
                k.op(k.dve, lambda: nc.vector.tensor_scalar(out=cj[:, 0:L], in0=sc[:, 0:L], scalar1=sm[:, 2:3],
                                                            scalar2=0.0, op0=ALU.is_ge, op1=ALU.add,
                                                            accum_out=sm[:, 3:4]),
                     reads=[scb, smb], writes=[cjb, smb])
                k.op(k.dve, lambda: nc.vector.scalar_tensor_tensor(out=sm[:, 4:5], in0=sm[:, 3:4],
                                                                   scalar=KEEP - 0.5, in1=hw[:, it:it + 1],
                                                                   op0=ALU.is_ge, op1=ALU.mult),
                     reads=[smb, hwb], writes=[smb])
                k.op(k.dve, lambda: nc.vector.tensor_tensor(out=sm[:, 1:2], in0=sm[:, 1:2], in1=sm[:, 4:5],
                                                            op=ALU.add), reads=[smb], writes=[smb])
            m01, m01b = m01r.next()
            k.op(k.dve, lambda: nc.vector.tensor_scalar(out=m01[:, 0:L], in0=sc[:, 0:L], scalar1=sm[:, 1:2],
                                                        scalar2=None, op0=ALU.is_ge), reads=[scb, smb], writes=[m01b])
            def post(t=t, m01=m01, m01b=m01b):
                mts, mtsb = mtsr.next()
                for j0 in range(0, t + 1, 8):
                    nj = min(8, t + 1 - j0)
                    tp, tpb = tpr.next()
                    for jj in range(nj):
                        k.op(k.pe, lambda jj=jj: nc.tensor.transpose(
                            out=tp[:, jj * 128:(jj + 1) * 128], in_=m01[:, (j0 + jj) * 128:(j0 + jj + 1) * 128],
                            identity=C.ident[:]), reads=[m01b], writes=[tpb])
                    k.op(k.act, lambda: nc.scalar.copy(out=mts[:, j0:j0 + nj, :],
                                                       in_=tp[:, 0:nj * 128].rearrange("p (j n) -> p j n", n=128)),
                         reads=[tpb], writes=[mtsb])
                i_q, sub = t // 4, t % 4
                nkt = 4 * i_q + 4
                k.dma(k.pool, C.MK_d[i_q, :, 0:nkt, sub * 128:(sub + 1) * 128], mts[:, 0:nkt, :], reads=[mtsb])
            if prev_post is not None:
                prev_post()
            prev_post = post
        prev_post()


def attention(k, cfg, C, mode, pairs, tag):
    nc = k.nc
    S, T, TB = cfg.S, cfg.T, cfg.TB
    with Phase(k, "B" + tag) as ph:
        qr = Ring(ph, 2, [128, S], BF16, "q")
        kr = Ring(ph, 2, [128, S], BF16, "k")
        ver = Ring(ph, 2, [128, T, 128], BF16, "ve")
        vor = Ring(ph, 2, [128, T, 128], BF16, "vo")
        for i in range(2):
            k.op(k.pool, lambda i=i: nc.gpsimd.memset(ver.t[i][:], 0.0), writes=[ver.b[i]])
            k.op(k.pool, lambda i=i: nc.gpsimd.memset(vor.t[i][:], 0.0), writes=[vor.b[i]])
        mnr = Ring(ph, 2, [128, 2, 1024], BF16, "mn")
        ptr = Ring(ph, 6, [128, 512], BF16, "pt")
        mor = Ring(ph, 2, [128, S], BF16, "mo")
        recr = Ring(ph, 2, [128, 512], F32, "rec")
        psr = Ring(ph, 4, [128, 512], F32, "ps", psum=True)
        por = Ring(ph, 2, [128, 512], F32, "po", psum=True)
        pdr = Ring(ph, 2, [128, 512], F32, "pd", psum=True)
        Mx_d = C.Mdil_d if mode == "dil" else C.Mnear_d
        if mode == "moba":
            oh = ph.sb([128, S], BF16, "oh")
            ohb = Buf()
            k.dma(k.sp, oh[:], C.onehot_in[:, :], writes=[ohb])
            qmr = Ring(ph, 2, [128, S], BF16, "qm")
        if mode == "dsa":
            mkr = Ring(ph, 2, [128, 16, 512], BF16, "mk")
        if mode == "dil":
            cf = ph.sb([128, 2432], BF16, "cf")
            cfb = Buf()
            k.dma(k.sp, cf[:], C.cfar_in[:, :], writes=[cfb])
        lag = Lag(3)
        tile_no = [0]
        for c in pairs:
            qt, qb = qr.next()
            kt, kb = kr.next()
            ve, veb = ver.next()
            vo, vob = vor.next()
            mn, mnb = mnr.next()
            k.dma(k.sp, qt[:], C.QT_d[c], writes=[qb])
            k.dma(k.sp, kt[:], C.KT_d[c], writes=[kb])
            k.dma(k.sp, ve[:, :, 0:64], C.V_d[c, :, :, 0:64], writes=[veb])
            k.dma(k.sp, vo[:, :, 64:128], C.V_d[c, :, :, 64:128], writes=[vob])
            k.dma(k.sp, mn[:], Mx_d[2 * c:2 * c + 2].rearrange("h p n -> p h n"), writes=[mnb])
            qm = qmb = None
            if mode == "moba":
                qm, qmb = qmr.next()
                k.dma(k.sp, qm[:], C.QM_d[c], writes=[qmb])
            mo, mob = mor.next()
            for i in range(TB):
                q0 = i * 512
                jhi = 4 * i + 3
                jlo = max(0, 4 * i - 16) if mode == "dil" else 0
                po, pob = por.next()
                pd, pdb = pdr.next()
                seq = [(j, hh) for j in range(jlo, jhi + 1) for hh in range(2)]
                mk = mkb = None
                for idx, (j, hh) in enumerate(seq):
                    if mode == "dsa" and hh == 0 and (j % 16 == 0):
                        mk, mkb = mkr.next()
                        g1 = min(jhi + 1, j + 16)
                        k.dma(k.sp, mk[:, 0:g1 - j, :], C.MK_d[i, :, j:g1, :], writes=[mkb])
                    rel = q0 - 128 * j
                    near = rel <= 128
                    hd = 2 * c + hh
                    ps, psb = psr.next()
                    k.op(k.pe, lambda: nc.tensor.matmul(
                        ps[:], lhsT=kt[hh * 64:(hh + 1) * 64, j * 128:(j + 1) * 128],
                        rhs=qt[hh * 64:(hh + 1) * 64, q0:q0 + 512], start=True, stop=(mode != "moba")),
                        reads=[kb, qb], writes=[psb])
                    if mode == "moba":
                        k.op(k.pe, lambda: nc.tensor.matmul(
                            ps[:], lhsT=oh[64 * hh:64 * hh + 16, j * 128:(j + 1) * 128],
                            rhs=qm[64 * hh:64 * hh + 16, q0:q0 + 512],
                            start=False, stop=True), reads=[ohb, qmb], writes=[psb])
                    pt, ptb = ptr.next()
                    k.op(k.act, lambda: nc.scalar.activation(out=pt[:], in_=ps[:], func=AF.Exp,
                                                             bias=C.c31[:, hd:hd + 1], scale=1.0),
                         reads=[psb, C.c31b], writes=[ptb])
                    tile_no[0] += 1
                    if tile_no[0] % 2 == 0:
                        ME, mfn = k.pool, nc.gpsimd.tensor_tensor
                    else:
                        ME, mfn = k.dve, nc.vector.tensor_tensor
                    if near:
                        o = rel + 384
                        k.op(ME, lambda: mfn(out=pt[:], in0=pt[:], in1=mn[:, hh, o:o + 512], op=ALU.mult),
                             reads=[ptb, mnb], writes=[ptb])
                    elif mode == "dil":
                        o = rel - 129
                        k.op(ME, lambda: mfn(out=pt[:], in0=pt[:], in1=cf[:, o:o + 512], op=ALU.mult),
                             reads=[ptb, cfb], writes=[ptb])
                    if mode == "dsa":
                        jj = j % 16
                        k.op(ME, lambda: mfn(out=pt[:], in0=pt[:], in1=mk[:, jj, :], op=ALU.mult),
                             reads=[ptb, mkb], writes=[ptb])
                    first, last = idx == 0, idx == len(seq) - 1

                    def pv(pt=pt, ptb=ptb, j=j, hh=hh, first=first, last=last, po=po, pob=pob, pd=pd, pdb=pdb,
                           ve=ve, veb=veb, vo=vo, vob=vob):
                        vv, vvb = (ve, veb) if hh == 0 else (vo, vob)
                        k.op(k.pe, lambda: nc.tensor.matmul(po[:], lhsT=vv[:, j, :], rhs=pt[:], start=first,
                                                            stop=last), reads=[vvb, ptb], writes=[pob])
                        on = C.onesE if hh == 0 else C.onesO
                        k.op(k.pe, lambda: nc.tensor.matmul(pd[:], lhsT=on, rhs=pt[:], start=first, stop=last),
                             reads=[ptb, C.onesb], writes=[pdb])
                    lag.push(pv)

                def fin(po=po, pob=pob, pd=pd, pdb=pdb, mo=mo, mob=mob, q0=q0):
                    rec, recb = recr.next()
                    k.op(k.dve, lambda: nc.vector.reciprocal(out=rec[:], in_=pd[:]), reads=[pdb], writes=[recb])
                    k.op(k.dve, lambda: nc.vector.tensor_tensor(out=mo[:, q0:q0 + 512], in0=po[:], in1=rec[:],
                                                                op=ALU.mult), reads=[pob, recb], writes=[mob])
                lag.push(fin)

            def st(mo=mo, mob=mob, c=c):
                k.dma(k.pool, C.MT_d[c], mo[:], reads=[mob])
            lag.push(st)
        lag.flush()


def phase_C(k, cfg, C, hT, hTb, layer):
    nc = k.nc
    S, T = cfg.S, cfg.T
    with Phase(k, "C") as ph:
        wo = ph.sb([128, KC, D], BF16, "wo")
        wob = Buf()
        wst = Ring(ph, 2, [128, KC, 512], F32, "wst")
        Wv = C.w_o[layer].rearrange("(c p) n -> p c n", p=128)
        for nb in range(2):
            ws, wsb = wst.next()
            k.dma(k.sp, ws[:], Wv[:, :, nb * 512:(nb + 1) * 512], writes=[wsb])
            k.op(k.pool, lambda: nc.gpsimd.tensor_copy(out=wo[:, :, nb * 512:(nb + 1) * 512], in_=ws[:]),
                 reads=[wsb], writes=[wob])
        g_bc = ph.sb([128, D], F32, "g")
        gb = Buf()
        k.dma(k.sp, g_bc[:], C.g_ffn[layer], writes=[gb])
        rings = norm_rings(ph)
        mtr = Ring(ph, 2, [128, KC, 128], BF16, "mt")
        xr = Ring(ph, 3, [128, D], F32, "x")
        xmr = Ring(ph, 2, [128, D], F32, "xm")
        pacc = Ring(ph, 4, [128, 512], F32, "pacc", psum=True)
        xsrc = C.x_in if layer == 0 else C.xs
        pend = None
        for t in range(T):
            mt, mtb = mtr.next()
            k.dma(k.sp, mt[:], C.MT_d[:, :, t * 128:(t + 1) * 128].rearrange("c p n -> p c n"), writes=[mtb])
            xt, xb = xr.next()
            k.dma(k.sp, xt[:], xsrc[t * 128:(t + 1) * 128, :], writes=[xb])
            xm, xmb = xmr.next()
            for nb in range(2):
                pa, pab = pacc.next()
                for c in range(KC):
                    k.op(k.pe, lambda c=c: nc.tensor.matmul(pa[:], lhsT=mt[:, c, :],
                                                            rhs=wo[:, c, nb * 512:(nb + 1) * 512],
                                                            start=(c == 0), stop=(c == KC - 1)),
                         reads=[mtb, wob], writes=[pab])
                k.op(k.dve, lambda: nc.vector.tensor_tensor(out=xm[:, nb * 512:(nb + 1) * 512], in0=pa[:],
                                                            in1=xt[:, nb * 512:(nb + 1) * 512], op=ALU.add),
                     reads=[pab, xb], writes=[xmb])
            k.dma(k.pool, C.xs[t * 128:(t + 1) * 128, :], xm[:], reads=[xmb])
            hb, hbb = norm_tile_a(k, C, xm, xmb, g_bc, gb, rings)
            if pend is not None:
                norm_tile_b(k, C, pend[0], pend[1], hT, hTb[pend[2]], pend[2], rings)
            pend = (hb, hbb, t)
        norm_tile_b(k, C, pend[0], pend[1], hT, hTb[pend[2]], pend[2], rings)


def phase_F1(k, cfg, C, hT, hTb, layer):
    nc = k.nc
    S, T, TB = cfg.S, cfg.T, cfg.TB
    with Phase(k, "F1") as ph:
        cp = ph.sb([128, 4, 44], F32, "cp")
        cpb = Buf()
        k.dma(k.sp, cp[:], C.convp[layer], writes=[cpb])
        Wv = C.w_up[layer].rearrange("(c p) n -> p c n", p=128)
        wst = Ring(ph, 2, [128, KC, 256], F32, "wst")
        wbf = Ring(ph, 2, [128, KC, 256], BF16, "wbf")
        pacc = Ring(ph, 4, [128, 512], F32, "pacc", psum=True)
        ubr = [Ring(ph, 2, [128, 514], F32, "ubv"), Ring(ph, 2, [128, 514], F32, "ubg")]
        car = Ring(ph, 3, [128, 512], F32, "ca")
        cbr = Ring(ph, 3, [128, 512], F32, "cb")
        sgr = Ring(ph, 2, [128, 512], F32, "sg")
        asr = Ring(ph, 2, [128, S], BF16, "as")
        for f in range(NF):
            ws, wsb = wst.next()
            k.dma(k.sp, ws[:, :, 0:128], Wv[:, :, f * 128:(f + 1) * 128], writes=[wsb])
            k.dma(k.sp, ws[:, :, 128:256], Wv[:, :, DFF + f * 128:DFF + (f + 1) * 128], writes=[wsb])
            wb, wbb = wbf.next()
            k.op(k.pool, lambda: nc.gpsimd.tensor_copy(out=wb[:], in_=ws[:]), reads=[wsb], writes=[wbb])
            a_s, asb = asr.next()
            prev = [None, None]
            for tb in range(TB):
                cv = [None, None]
                for half in range(2):
                    col = f if half == 0 else NF + f
                    pa, pab = pacc.next()
                    for kc in range(KC):
                        k.op(k.pe, lambda kc=kc: nc.tensor.matmul(
                            pa[:], lhsT=wb[:, kc, half * 128:(half + 1) * 128],
                            rhs=hT[:, kc, tb * 512:(tb + 1) * 512], start=(kc == 0), stop=(kc == KC - 1)),
                            reads=[wbb] + hTb[tb * 4:(tb + 1) * 4], writes=[pab])
                    ub, ubb = ubr[half].next()
                    if tb == 0:
                        k.op(k.pool, lambda: nc.gpsimd.memset(ub[:, 0:2], 0.0), writes=[ubb])
                    else:
                        pu, pub = prev[half]
                        k.op(k.pool, lambda: nc.gpsimd.tensor_copy(out=ub[:, 0:2], in_=pu[:, 512:514]),
                             reads=[pub], writes=[ubb])
                    k.op(k.act, lambda: nc.scalar.copy(out=ub[:, 2:514], in_=pa[:]), reads=[pab], writes=[ubb])
                    prev[half] = (ub, ubb)
                    r = car if half == 0 else cbr
                    c1, c1b = r.next()
                    k.op(k.dve, lambda: nc.vector.tensor_scalar(out=c1[:], in0=ub[:, 2:514],
                                                                scalar1=cp[:, 2, col:col + 1],
                                                                scalar2=cp[:, 3, col:col + 1],
                                                                op0=ALU.mult, op1=ALU.add),
                         reads=[ubb, cpb], writes=[c1b])
                    c2, c2b = r.next()
                    k.op(k.dve, lambda: nc.vector.scalar_tensor_tensor(out=c2[:], in0=ub[:, 1:513],
                                                                       scalar=cp[:, 1, col:col + 1], in1=c1[:],
                                                                       op0=ALU.mult, op1=ALU.add),
                         reads=[ubb, cpb, c1b], writes=[c2b])
                    c3, c3b = r.next()
                    k.op(k.dve, lambda: nc.vector.scalar_tensor_tensor(out=c3[:], in0=ub[:, 0:512],
                                                                       scalar=cp[:, 0, col:col + 1], in1=c2[:],
                                                                       op0=ALU.mult, op1=ALU.add),
                         reads=[ubb, cpb, c2b], writes=[c3b])
                    cv[half] = (c3, c3b)
                sg, sgb = sgr.next()
                k.op(k.act, lambda: nc.scalar.activation(out=sg[:], in_=cv[1][0][:], func=AF.Silu),
                     reads=[cv[1][1]], writes=[sgb])
                k.op(k.dve, lambda: nc.vector.tensor_tensor(out=a_s[:, tb * 512:(tb + 1) * 512], in0=sg[:],
                                                            in1=cv[0][0][:], op=ALU.mult),
                     reads=[sgb, cv[0][1]], writes=[asb])
            k.dma(k.pool, C.AT_d[f], a_s[:], reads=[asb])


def phase_F2(k, cfg, C, hT, hTb, layer, last):
    nc = k.nc
    S, T = cfg.S, cfg.T
    with Phase(k, "F2") as ph:
        wd = ph.sb([128, NF, D], BF16, "wd")
        wdb = Buf()
        wst = Ring(ph, 2, [128, NF, 128], F32, "wst")
        Wv = C.w_down[layer].rearrange("(f p) n -> p f n", p=128)
        for cb in range(8):
            ws, wsb = wst.next()
            k.dma(k.sp, ws[:], Wv[:, :, cb * 128:(cb + 1) * 128], writes=[wsb])
            k.op(k.pool, lambda: nc.gpsimd.tensor_copy(out=wd[:, :, cb * 128:(cb + 1) * 128], in_=ws[:]),
                 reads=[wsb], writes=[wdb])
        g_bc = ph.sb([128, D], F32, "g")
        gb = Buf()
        k.dma(k.sp, g_bc[:], C.g_fin[:, :] if last else C.g_attn[layer + 1], writes=[gb])
        rings = norm_rings(ph)
        atr = Ring(ph, 2, [128, NF, 128], BF16, "at")
        xr = Ring(ph, 3, [128, D], F32, "x")
        xnr = Ring(ph, 2, [128, D], F32, "xn")
        yor = Ring(ph, 2, [128, D], F32, "yo")
        pacc = Ring(ph, 4, [128, 512], F32, "pacc", psum=True)
        pend = None
        for t in range(T):
            at, atb = atr.next()
            k.dma(k.sp, at[:], C.AT_d[:, :, t * 128:(t + 1) * 128].rearrange("f p n -> p f n"), writes=[atb])
            xt, xb = xr.next()
            k.dma(k.sp, xt[:], C.xs[t * 128:(t + 1) * 128, :], writes=[xb])
            xn, xnb = xnr.next()
            for nb in range(2):
                pa, pab = pacc.next()
                for f in range(NF):
                    k.op(k.pe, lambda f=f: nc.tensor.matmul(pa[:], lhsT=at[:, f, :],
                                                            rhs=wd[:, f, nb * 512:(nb + 1) * 512],
                                                            start=(f == 0), stop=(f == NF - 1)),
                         reads=[atb, wdb], writes=[pab])
                k.op(k.dve, lambda: nc.vector.tensor_tensor(out=xn[:, nb * 512:(nb + 1) * 512], in0=pa[:],
                                                            in1=xt[:, nb * 512:(nb + 1) * 512], op=ALU.add),
                     reads=[pab, xb], writes=[xnb])
            if last:
                junk, junkb = rings["junk"].next()
                ss, ssb = rings["ss"].next()
                rstd_chain(k, xn[:], xnb, ss, ssb, junk[:], junkb, D)
                yo, yob = yor.next()
                k.op(k.dve, lambda: nc.vector.scalar_tensor_tensor(out=yo[:], in0=xn[:], scalar=ss[:, 3:4],
                                                                   in1=g_bc[:], op0=ALU.mult, op1=ALU.mult),
                     reads=[xnb, ssb, gb], writes=[yob])
                k.dma(k.pool, C.out[t * 128:(t + 1) * 128, :], yo[:], reads=[yob])
            else:
                k.dma(k.pool, C.xs[t * 128:(t + 1) * 128, :], xn[:], reads=[xnb])
                hb, hbb = norm_tile_a(k, C, xn, xnb, g_bc, gb, rings)
                if pend is not None:
                    norm_tile_b(k, C, pend[0], pend[1], hT, hTb[pend[2]], pend[2], rings)
                pend = (hb, hbb, t)
        if pend is not None:
            norm_tile_b(k, C, pend[0], pend[1], hT, hTb[pend[2]], pend[2], rings)


def build(cfg):
    S, T, TB = cfg.S, cfg.T, cfg.TB
    nc = bass.Bass("TRN2", target_bir_lowering=False)
    k = K(nc)
    C = NS()

    def din(name, shape, dt=F32):
        return nc.dram_tensor(name, list(shape), dt, kind="ExternalInput").ap()

    def dscr(name, shape, dt):
        kind = "ExternalOutput" if (cfg.debug and name in cfg.debug) else "Internal"
        return nc.dram_tensor(name, list(shape), dt, kind=kind).ap()

    C.x_in = din("x", [S, D])
    C.w_in_even = din("w_in_even", [2, D, 4176])
    C.w_in_odd = din("w_in_odd", [2, D, 3072])
    C.w_o = din("w_o", [4, D, D])
    C.w_up = din("w_up", [4, D, 2 * DFF])
    C.w_down = din("w_down", [4, DFF, D])
    C.g_attn = din("g_attn", [4, 128, D])
    C.g_ffn = din("g_ffn", [4, 128, D])
    C.g_fin = din("g_fin", [128, D])
    C.gk_in = din("gk", [2, 128, 64])
    C.convp = din("convp", [4, 128, 4, 44])
    C.G_in = din("G", [16, 128, 1024])
    c31_in = din("c31", [128, 16])
    ident_in = din("ident", [128, 128], BF16)
    ones_in = din("ones2", [128, 2, 128], BF16)
    C.caus_in = din("caus", [128, 1024])
    C.cdil_in = din("cdil", [128, 1024])
    C.cfar_in = din("cfar", [128, 2432], BF16)
    C.onehot_in = din("onehot", [128, S], BF16)
    C.pastmask_in = din("pastmask", [128, 16, 16])
    C.own30k_in = din("own30k", [128, 16, 16])
    C.tri_in = din("tri", [128, 128])
    C.pow2_in = din("pow2", [128, NIT])

    C.QT_d = dscr("QT_d", [8, 128, S], BF16)
    C.KT_d = dscr("KT_d", [8, 128, S], BF16)
    C.V_d = dscr("V_d", [8, 128, T, 128], BF16)
    C.QI_d = dscr("QI_d", [8, 128, S], BF16)
    C.KW_d = dscr("KW_d", [128, T, 80], F32)
    C.QM_d = dscr("QM_d", [4, 128, S], BF16)
    C.MK_d = dscr("MK_d", [TB, 128, T, 512], BF16)
    C.MT_d = dscr("MT_d", [8, 128, S], BF16)
    C.AT_d = dscr("AT_d", [NF, 128, S], BF16)
    C.xs = dscr("xs", [S, D], F32)
    C.Mnear_d = dscr("Mnear_d", [16, 128, 1024], BF16)
    C.Mdil_d = dscr("Mdil_d", [16, 128, 1024], BF16)
    C.out = nc.dram_tensor("out", [S, D], F32, kind="ExternalOutput").ap()

    stop_after = cfg.stop_after

    with ExitStack() as top:
        C.ident = top.enter_context(nc.sbuf_tensor("ident_sb", [128, 128], BF16))
        C.c31 = top.enter_context(nc.sbuf_tensor("c31_sb", [128, 16], F32))
        ones2 = top.enter_context(nc.sbuf_tensor("ones_sb", [128, 2, 128], BF16))
        C.onesE = ones2[:, 0, :]
        C.onesO = ones2[:, 1, :]
        C.c31b, C.onesb, identb = Buf(), Buf(), Buf()
        k.dma(k.sp, C.ident[:], ident_in[:, :], writes=[identb])
        k.dma(k.sp, C.c31[:], c31_in[:, :], writes=[C.c31b])
        k.dma(k.sp, ones2[:], ones_in[:, :, :], writes=[C.onesb])
        k.barrier()
        phase_tables(k, cfg, C)

        def done(tag):
            return stop_after == tag

        fin = False
        for layer in range(cfg.depth):
            if fin:
                break
            if layer == 0:
                with Phase(k, "H0") as hp:
                    hT = hp.sb([128, KC, S], BF16, "hT")
                    hTb = [Buf() for _ in range(T)]
                    phase_A1(k, cfg, C, hT, hTb, 0)
                    phase_A2(k, cfg, C, hT, hTb, 0)
            if done("A%d" % layer):
                break
            if layer % 2 == 0:
                moba_prepass(k, cfg, C)
                if done("G%d" % layer):
                    break
                attention(k, cfg, C, "moba", [0, 1, 2, 3], "m")
                if done("M%d" % layer):
                    break
                dsa_prepass(k, cfg, C, layer)
                attention(k, cfg, C, "dsa", [4, 5, 6, 7], "d")
            else:
                attention(k, cfg, C, "dil", list(range(8)), "l")
            if done("B%d" % layer):
                break
            with Phase(k, "HC") as hp:
                hT = hp.sb([128, KC, S], BF16, "hT")
                hTb = [Buf() for _ in range(T)]
                phase_C(k, cfg, C, hT, hTb, layer)
                if done("C%d" % layer):
                    fin = True
                else:
                    phase_F1(k, cfg, C, hT, hTb, layer)
            if fin or done("F1%d" % layer):
                break
            last = layer == cfg.depth - 1
            with Phase(k, "HF") as hp:
                hT = hp.sb([128, KC, S], BF16, "hT")
                hTb = [Buf() for _ in range(T)]
                phase_F2(k, cfg, C, hT, hTb, layer, last)
                if not last and not done("F2%d" % layer):
                    phase_A2(k, cfg, C, hT, hTb, layer + 1)
            if done("F2%d" % layer):
                break
        k.final_wait()
    k.stack.close()
    return nc


def host_inputs(cfg, b, x, w_in_even, idx_k_norm, w_in_odd, w_o, rel_bias, attn_norm, ffn_norm,
                w_up, conv_w, conv_b, w_down, final_norm):
    S = cfg.S
    bf = ml_dtypes.bfloat16
    m = {}
    m["x"] = np.ascontiguousarray(x[b], dtype=np.float32)
    m["w_in_even"] = w_in_even
    m["w_in_odd"] = w_in_odd
    m["w_o"] = w_o
    m["w_up"] = w_up
    m["w_down"] = w_down
    m["g_attn"] = np.ascontiguousarray(np.broadcast_to(attn_norm[:, None, :], (4, 128, D)))
    m["g_ffn"] = np.ascontiguousarray(np.broadcast_to(ffn_norm[:, None, :], (4, 128, D)))
    m["g_fin"] = np.ascontiguousarray(np.broadcast_to(final_norm[None, :], (128, D)))
    m["gk"] = np.ascontiguousarray(np.broadcast_to(idx_k_norm[:, None, :], (2, 128, 64)))
    cp = np.zeros((4, 128, 4, 44), np.float32)
    for l in range(4):
        for j in range(3):
            cp[l, :, j, :] = conv_w[l, j].reshape(44, 128).T
        cp[l, :, 3, :] = conv_b[l].reshape(44, 128).T
    m["convp"] = cp
    p = np.arange(128)[:, None]
    j = np.arange(1024)[None, :]
    d = j - 384 - p
    bk = t5_bucket_np(d)
    m["G"] = np.ascontiguousarray(np.transpose(rel_bias[bk, :], (2, 0, 1))).astype(np.float32)
    m["c31"] = np.ascontiguousarray(np.broadcast_to(rel_bias[31][None, :], (128, 16))).astype(np.float32)
    m["ident"] = np.eye(128, dtype=np.float32).astype(bf)
    o2 = np.zeros((128, 2, 128), np.float32)
    o2[:, 0, 0:64] = 1.0
    o2[:, 1, 64:128] = 1.0
    m["ones2"] = o2.astype(bf)
    m["caus"] = (d >= 0).astype(np.float32)

    def cmul(dd):
        return (((dd >= 0) & (dd <= 128)).astype(np.float32)
                + ((dd >= 0) & (dd <= 512) & (dd % 4 == 0)).astype(np.float32)
                + ((dd >= 0) & (dd <= 2048) & (dd % 16 == 0)).astype(np.float32))
    m["cdil"] = cmul(d)
    jf = np.arange(2432)[None, :]
    m["cfar"] = cmul(jf - p + 129).astype(bf)
    kk = np.arange(S)[None, :]
    n128 = np.arange(128)[:, None]
    m["onehot"] = ((kk // 256 == (n128 % 64)) & ((n128 % 64) < 16)).astype(np.float32).astype(bf)
    own = np.arange(16)[:, None]
    nn = np.arange(16)[None, :]
    pmk = np.where(nn < own, 0.0, -1e30).astype(np.float32)
    m["pastmask"] = np.ascontiguousarray(np.broadcast_to(pmk[None], (128, 16, 16)))
    o3 = np.where(nn == own, 30000.0, 0.0).astype(np.float32)
    m["own30k"] = np.ascontiguousarray(np.broadcast_to(o3[None], (128, 16, 16)))
    q = np.arange(128)[:, None]
    kx = np.arange(128)[None, :]
    m["tri"] = np.where(kx <= q, 0.0, -1e30).astype(np.float32)
    m["pow2"] = np.ascontiguousarray(np.broadcast_to((2.0 ** -np.arange(NIT))[None, :], (128, NIT))).astype(np.float32)
    return m


_CACHE = {}


def kernel(**inputs):
    inputs = {k_: np.asarray(v) for k_, v in inputs.items()}
    x = inputs["x"]
    B, S, _ = x.shape
    cfg = Cfg(S=S, depth=4)
    if "nc" not in _CACHE:
        _CACHE["nc"] = build(cfg)
    nc = _CACHE["nc"]
    in_maps = []
    for core in range(8):
        in_maps.append(host_inputs(cfg, core // 2, **inputs))
    res = run_bass_kernel_spmd(nc, in_maps, core_ids=list(range(8)))
    outs = [res.results[2 * b]["out"] for b in range(B)]
    return np.stack(outs, axis=0).astype(np.float32)
```
